# Optimizing a Trainium2 kernel written in Bass

```python
import math
import jax, jax.numpy as jnp
from jax import lax
import numpy as np

D_MODEL = 1024
BATCH = 16
SEQ = 4096
DEPTH = 1

MLA_HEADS = 8
MLA_NOPE = 64
MLA_ROPE = 32
MLA_V = 64
Q_LORA = 256
KV_LORA = 128
ROPE_THETA = 10000.0
ATTN_Q_BLOCK = 128
MOBA_HEADS = 8
MOBA_HD = 64
MOBA_BLOCK = 256
MOBA_TOPK = 3
MOBA_Q_CHUNK = 16
D_FF = 2816
CONV_W = 3
MIX_W = MLA_HEADS * MLA_V + MOBA_HEADS * MOBA_HD
MOBA_W = MOBA_HEADS * MOBA_HD
IN_COLS = Q_LORA + KV_LORA + MLA_ROPE + 3 * MOBA_W
SPLITS = [Q_LORA, Q_LORA + KV_LORA, Q_LORA + KV_LORA + MLA_ROPE,
          Q_LORA + KV_LORA + MLA_ROPE + MOBA_W, Q_LORA + KV_LORA + MLA_ROPE + 2 * MOBA_W]
EPS = 1e-5

kernel_name = "hymba_mla_moba_deepnorm_convffn"


def layer_norm(x, g, b):
    xf = x.astype(jnp.float32)
    mu = jnp.mean(xf, -1, keepdims=True)
    var = jnp.mean(jnp.square(xf - mu), -1, keepdims=True)
    return ((xf - mu) * lax.rsqrt(var + EPS) * g.astype(jnp.float32) + b.astype(jnp.float32)).astype(x.dtype)


def rms_norm(x, g):
    xf = x.astype(jnp.float32)
    ms = jnp.mean(jnp.square(xf), -1, keepdims=True)
    return (xf * lax.rsqrt(ms + EPS) * g.astype(jnp.float32)).astype(x.dtype)


def rope(x):
    S, R = x.shape[1], x.shape[-1]
    half = R // 2
    inv = ROPE_THETA ** (-jnp.arange(half, dtype=jnp.float32) / half)
    ang = jnp.arange(S, dtype=jnp.float32)[:, None] * inv[None, :]
    cos = jnp.cos(ang)[None, :, None, :].astype(x.dtype)
    sin = jnp.sin(ang)[None, :, None, :].astype(x.dtype)
    x1, x2 = x[..., :half], x[..., half:]
    return jnp.concatenate([x1 * cos - x2 * sin, x1 * sin + x2 * cos], -1)


def mla_attention(c_q, c_kv, k_rope, q_norm_g, w_uq, kv_norm_g, w_ukv):
    B, S, _ = c_q.shape
    q = (rms_norm(c_q, q_norm_g) @ w_uq).reshape(B, S, MLA_HEADS, MLA_NOPE + MLA_ROPE)
    q_nope, q_pe = q[..., :MLA_NOPE], rope(q[..., MLA_NOPE:])
    kv = (rms_norm(c_kv, kv_norm_g) @ w_ukv).reshape(B, S, MLA_HEADS, MLA_NOPE + MLA_V)
    k_nope, v = kv[..., :MLA_NOPE], kv[..., MLA_NOPE:]
    k_pe = rope(k_rope[:, :, None, :])[:, :, 0]
    scale = (MLA_NOPE + MLA_ROPE) ** -0.5
    nq = S // ATTN_Q_BLOCK
    blk = lambda a: a.reshape(B, nq, ATTN_Q_BLOCK, *a.shape[2:]).swapaxes(0, 1)
    kpos = jnp.arange(S)

    def one(args):
        i, qn, qp = args
        s = (jnp.einsum('bthd,bshd->bhts', qn, k_nope)
             + jnp.einsum('bthr,bsr->bhts', qp, k_pe)).astype(jnp.float32) * scale
        qpos = i * ATTN_Q_BLOCK + jnp.arange(ATTN_Q_BLOCK)
        s = jnp.where(kpos[None, :] <= qpos[:, None], s, -jnp.inf)
        p = jax.nn.softmax(s, -1).astype(v.dtype)
        return jnp.einsum('bhts,bshd->bthd', p, v)

    out = lax.map(one, (jnp.arange(nq), blk(q_nope), blk(q_pe)))
    return out.swapaxes(0, 1).reshape(B, S, MLA_HEADS * MLA_V)


def moba_attention(q, k, v):
    B, S, H, D = q.shape
    NB = -(-S // MOBA_BLOCK)
    Sp = NB * MOBA_BLOCK
    pad = ((0, 0), (0, Sp - S), (0, 0), (0, 0))
    q, k, v = jnp.pad(q, pad), jnp.pad(k, pad), jnp.pad(v, pad)
    kb = k.reshape(B, NB, MOBA_BLOCK, H, D).transpose(0, 3, 1, 2, 4)
    vb = v.reshape(B, NB, MOBA_BLOCK, H, D).transpose(0, 3, 1, 2, 4)
    kmean = jnp.mean(kb.astype(jnp.float32), axis=3)
    topk = min(MOBA_TOPK, NB)
    L = MOBA_BLOCK
    Tq = MOBA_Q_CHUNK
    slopes = 2.0 ** (-8.0 * jnp.arange(1, H + 1, dtype=jnp.float32) / H)
    scale = D ** -0.5
    nc = Sp // Tq
    qc = q.reshape(B, nc, Tq, H, D).transpose(1, 0, 3, 2, 4)
    bi = jnp.arange(B)[:, None, None, None]
    hi = jnp.arange(H)[None, :, None, None]

    def one(args):
        i, qi = args
        t = i * Tq + jnp.arange(Tq)
        own = (i * Tq) // MOBA_BLOCK
        g = jnp.einsum('bhtd,bhnd->bhtn', qi.astype(jnp.float32), kmean)
        g = jnp.where(jnp.arange(NB) < own, g, -jnp.inf)
        _, sel = lax.top_k(g, topk)
        valid = jnp.arange(topk) < own
        k_sel = kb[bi, hi, sel]
        v_sel = vb[bi, hi, sel]
        s_sel = jnp.einsum('bhtd,bhtjld->bhtjl', qi, k_sel).astype(jnp.float32) * scale
        kpos_sel = sel[..., None] * MOBA_BLOCK + jnp.arange(L)
        dist_sel = (t[:, None, None] - kpos_sel).astype(jnp.float32)
        s_sel = jnp.where(valid[:, None], s_sel - slopes[:, None, None, None] * dist_sel, -jnp.inf)
        k_own = lax.dynamic_index_in_dim(kb, own, axis=2, keepdims=False)
        v_own = lax.dynamic_index_in_dim(vb, own, axis=2, keepdims=False)
        s_own = jnp.einsum('bhtd,bhld->bhtl', qi, k_own).astype(jnp.float32) * scale
        dist_own = (t[:, None] - (own * MOBA_BLOCK + jnp.arange(L))[None, :]).astype(jnp.float32)
        s_own = jnp.where(dist_own >= 0, s_own - slopes[:, None, None] * dist_own, -jnp.inf)
        s = jnp.concatenate([s_sel.reshape(B, H, Tq, topk * L), s_own], -1)
        p = jax.nn.softmax(s, -1).astype(v.dtype)
        p_sel = p[..., :topk * L].reshape(B, H, Tq, topk, L)
        return (jnp.einsum('bhtjl,bhtjld->bhtd', p_sel, v_sel)
                + jnp.einsum('bhtl,bhld->bhtd', p[..., topk * L:], v_own))

    out = lax.map(one, (jnp.arange(nc), qc))
    return out.transpose(1, 0, 3, 2, 4).reshape(B, Sp, H * D)[:, :S]


def conv_gated_ffn(x, w_up, conv_w, conv_b, w_down):
    h = x @ w_up
    C = h.shape[-1]
    h = lax.conv_general_dilated(h, conv_w[:, None, :].astype(h.dtype), window_strides=(1,),
                                 padding=[(CONV_W - 1, 0)],
                                 dimension_numbers=('NWC', 'WIO', 'NWC'),
                                 feature_group_count=C) + conv_b
    gate, up = jnp.split(h, 2, axis=-1)
    return (jax.nn.silu(gate) * up) @ w_down


def setup_inputs(seed: int = 0) -> dict:
    key = jax.random.key(seed)
    ks = jax.random.split(key, 24)
    beta = (8.0 * DEPTH) ** -0.25
    nrm = lambda k, shape, s: jax.random.normal(k, shape, jnp.float32) * s
    x = jax.random.normal(ks[0], (BATCH, SEQ, D_MODEL), jnp.float32)
    w_in = jnp.concatenate([
        nrm(ks[1], (DEPTH, D_MODEL, Q_LORA + KV_LORA + MLA_ROPE + 2 * MOBA_W), D_MODEL ** -0.5),
        nrm(ks[2], (DEPTH, D_MODEL, MOBA_W), beta * D_MODEL ** -0.5)], axis=-1)
    w_ukv = jnp.concatenate([
        nrm(ks[3], (DEPTH, KV_LORA, MLA_HEADS, MLA_NOPE), KV_LORA ** -0.5),
        nrm(ks[4], (DEPTH, KV_LORA, MLA_HEADS, MLA_V), beta * KV_LORA ** -0.5)],
        axis=-1).reshape(DEPTH, KV_LORA, MLA_HEADS * (MLA_NOPE + MLA_V))
    return {
        "x": x,
        "w_in": w_in,
        "q_norm_g": 1.0 + nrm(ks[5], (DEPTH, Q_LORA), 0.01),
        "w_uq": nrm(ks[6], (DEPTH, Q_LORA, MLA_HEADS * (MLA_NOPE + MLA_ROPE)), Q_LORA ** -0.5),
        "kv_norm_g": 1.0 + nrm(ks[7], (DEPTH, KV_LORA), 0.01),
        "w_ukv": w_ukv,
        "w_o": nrm(ks[8], (DEPTH, MIX_W, D_MODEL), beta * MIX_W ** -0.5),
        "ln1_g": 1.0 + nrm(ks[9], (DEPTH, D_MODEL), 0.01),
        "ln1_b": nrm(ks[10], (DEPTH, D_MODEL), 0.01),
        "w_up": nrm(ks[11], (DEPTH, D_MODEL, 2 * D_FF), D_MODEL ** -0.5),
        "conv_w": nrm(ks[12], (DEPTH, CONV_W, 2 * D_FF), CONV_W ** -0.5),
        "conv_b": nrm(ks[13], (DEPTH, 2 * D_FF), 0.01),
        "w_down": nrm(ks[14], (DEPTH, D_FF, D_MODEL), beta * D_FF ** -0.5),
        "ln2_g": 1.0 + nrm(ks[15], (DEPTH, D_MODEL), 0.01),
        "ln2_b": nrm(ks[16], (DEPTH, D_MODEL), 0.01),
    }


def reference(x, w_in, q_norm_g, w_uq, kv_norm_g, w_ukv, w_o, ln1_g, ln1_b,
              w_up, conv_w, conv_b, w_down, ln2_g, ln2_b):
    alpha = (2.0 * DEPTH) ** 0.25
    B, S, _ = x.shape
    for l in range(DEPTH):
        proj = x @ w_in[l]
        c_q, c_kv, k_rope, q_m, k_m, v_m = jnp.split(proj, SPLITS, axis=-1)
        a_out = mla_attention(c_q, c_kv, k_rope, q_norm_g[l], w_uq[l], kv_norm_g[l], w_ukv[l])
        b_out = moba_attention(q_m.reshape(B, S, MOBA_HEADS, MOBA_HD),
                               k_m.reshape(B, S, MOBA_HEADS, MOBA_HD),
                               v_m.reshape(B, S, MOBA_HEADS, MOBA_HD))
        mix = jnp.concatenate([a_out, b_out], axis=-1) @ w_o[l]
        x = layer_norm(alpha * x + mix, ln1_g[l], ln1_b[l])
        x = layer_norm(alpha * x + conv_gated_ffn(x, w_up[l], conv_w[l], conv_b[l], w_down[l]),
                       ln2_g[l], ln2_b[l])
    return x
```

```python
import math
from contextlib import ExitStack

import numpy as np
import ml_dtypes
import concourse.bass as bass
import concourse.mybir as mybir
from concourse.bass_utils import run_bass_kernel_spmd

F32 = mybir.dt.float32
BF16 = mybir.dt.bfloat16
AF = mybir.ActivationFunctionType
ALU = mybir.AluOpType
AX = mybir.AxisListType

D = 1024
NCORES = 8
EPS = 1e-5
NEGBIG = -30000.0
ALPHA = 2.0 ** 0.25
SC_MLA = 96.0 ** -0.5
SC_MOBA = 0.125
DFF = 2816
NPAIR = 22
WIN_COLS = 1984
import os as _os
ALL_INC = _os.environ.get("ALL_INC", "0") == "1"


class Res:
    __slots__ = ("w", "r", "scratch")

    def __init__(self, scratch=False):
        self.w = None
        self.r = []
        self.scratch = scratch


class DSem:
    def __init__(self, sem):
        self.sem = sem
        self.cnt = 0


class _Rec:
    def __getattr__(self, name):
        def f(*a, **k):
            self.call = (name, a, k)
        return f


class Sched:
    ENGS = ("pe", "act", "dve", "pool", "sp")
    CENGS = ("pe", "act", "dve", "pool")

    def __init__(self, nc, stack):
        self.nc = nc
        self.stack = stack
        self.prog = {e: [] for e in self.ENGS}
        self.sem = {}
        self.cnt = {}
        self.targets = {e: set() for e in self.CENGS}
        for e in self.CENGS:
            self.sem[e] = stack.enter_context(nc.semaphore("s_" + e))
            self.cnt[e] = 0
        self.seen = {e: {} for e in self.ENGS}
        self.dsems = []
        self.rank = {e: {} for e in self.CENGS}
        self.flushed = {e: 0 for e in self.CENGS}

    def dsem(self):
        s = self.stack.enter_context(self.nc.semaphore(f"d{len(self.dsems)}"))
        d = DSem(s)
        self.dsems.append(d)
        return d

    def _wait(self, eng, ev):
        if ev is None:
            return
        kind, obj, val = ev
        if kind == "e":
            if obj == eng and eng == "pe":
                return
            key = obj
        else:
            key = id(obj)
            if val is None:
                val = obj.cnt
        if val <= 0 or self.seen[eng].get(key, 0) >= val:
            return
        self.seen[eng][key] = val
        if kind == "e":
            assert val > self.flushed[obj] or val in self.rank[obj], (eng, obj, val)
            self.targets[obj].add(val)
        self.prog[eng].append(("w", kind, obj, val))

    def _deps(self, eng, reads, writes):
        for r in reads:
            self._wait(eng, r.w)
        for w in writes:
            if w.scratch:
                continue
            self._wait(eng, w.w)
            for ev in w.r:
                self._wait(eng, ev)

    def _commit(self, ev, reads, writes):
        for r in reads:
            if not r.scratch:
                r.r.append(ev)
        for w in writes:
            w.w = ev
            w.r = []

    def op(self, eng, fn, reads=(), writes=()):
        self._deps(eng, reads, writes)
        self.cnt[eng] += 1
        ev = ("e", eng, self.cnt[eng])
        rec = _Rec()
        fn(rec)
        if ALL_INC:
            self.targets[eng].add(self.cnt[eng])
        self.prog[eng].append(("i", rec.call, self.cnt[eng]))
        self._commit(ev, reads, writes)
        return ev

    def dma(self, out, in_, ds, reads=(), writes=(), live=False, nodeps=False, q="sp"):
        if not nodeps:
            self._deps(q, reads, writes)
        ds.cnt += 16
        assert ds.cnt < 65000
        ev = ("d", ds, None if live else ds.cnt)
        self.prog[q].append(("d", out, in_, ds.sem))
        self._commit(ev, reads, writes)
        return ev

    def barrier(self):
        for e in self.ENGS:
            for o in self.CENGS:
                self._wait(e, ("e", o, self.cnt[o]))
            for d in self.dsems:
                self._wait(e, ("d", d, d.cnt))

    def flush(self):
        self.barrier()
        rank = self.rank
        for e in self.CENGS:
            new = sorted(v for v in self.targets[e] if v not in rank[e])
            base = len(rank[e])
            for i, v in enumerate(new):
                assert v > self.flushed[e]
                rank[e][v] = base + i + 1
            assert len(rank[e]) < 65000, (e, len(rank[e]))

        def replay(name):
            def body(eng):
                for it in self.prog[name]:
                    if it[0] == "w":
                        _, kind, obj, val = it
                        if kind == "e":
                            eng.wait_ge(self.sem[obj], rank[obj][val])
                        else:
                            eng.wait_ge(obj.sem, val)
                    elif it[0] == "i":
                        nm, a, k = it[1]
                        ins = getattr(eng, nm)(*a, **k)
                        if it[2] in rank[name]:
                            ins.then_inc(self.sem[name], 1)
                    else:
                        eng.dma_start(out=it[1], in_=it[2]).then_inc(it[3], 16)
            return body

        with self.nc.Block() as block:
            block.tensor(replay("pe"))
            block.scalar(replay("act"))
            block.vector(replay("dve"))
            block.gpsimd(replay("pool"))
            block.sync(replay("sp"))
        for e in self.ENGS:
            self.prog[e] = []
        for e in self.CENGS:
            self.flushed[e] = self.cnt[e]


class _Stop(Exception):
    pass


def build(NSEQ, S, debug=False, stop=None):
    st_ = {}
    try:
        return _build(NSEQ, S, debug, stop, st_)
    except _Stop:
        return st_["nc"]


def _build(NSEQ, S, debug, stop, st_):
    assert S % 512 == 0
    NT = S // 512
    NKT = S // 128
    nc = bass.Bass("TRN2", target_bir_lowering=False)
    st_["nc"] = nc

    def din(name, shape, dt=F32):
        return nc.dram_tensor(name, list(shape), dt, kind="ExternalInput").ap()

    def dscr(name, shape, dt=BF16):
        kind = "ExternalOutput" if debug else "Internal"
        return nc.dram_tensor(name, list(shape), dt, kind=kind).ap()

    x = din("x", [NSEQ, S, D])
    out = nc.dram_tensor("out", [NSEQ, S, D], F32, kind="ExternalOutput").ap()
    w_in = din("w_in", [128, 8 * WIN_COLS])
    w_uq = din("w_uq", [128, 2 * 1024])
    w_ukv = din("w_ukv", [128, 1024])
    qg = din("qg", [128, 2])
    kvg = din("kvg", [128, 1])
    w_o = din("w_o", [128, 8 * 1024])
    w_dn = din("w_dn", [128, NPAIR * 1024])
    w_up = din("w_up", [NPAIR, 128, 2048])
    lnp = din("lnp", [128, 4 * 1024])
    convp = din("convp", [128, 44 * 4])
    c_ident = din("c_ident", [128, 128])
    c_identb = din("c_identb", [128, 128], BF16)
    c_cmask = din("c_cmask", [128, 4 * 512], BF16)
    c_rope = din("c_rope", [NT, 128, 2 * 512])
    c_kconst = din("c_kconst", [18, S], BF16)
    c_qalibi = din("c_qalibi", [8, 2, S], BF16)
    c_abias = din("c_abias", [128, 8 * 35])
    c_pastm = din("c_pastm", [128, 16 * 16])

    wo_s = dscr("wo_s", [128, 8 * 1024])
    wdn_s = dscr("wdn_s", [128, NPAIR * 1024])
    wup_s = dscr("wup_s", [NPAIR, 128, 2048])
    scr = []
    for s in range(NSEQ):
        scr.append(dict(
            qn=dscr(f"qn{s}", [4, 128, S]), qp=dscr(f"qp{s}", [2, 128, S]),
            kn=dscr(f"kn{s}", [4, 128, S]), kp=dscr(f"kp{s}", [32, S]),
            va=dscr(f"va{s}", [S, 512]),
            mq=dscr(f"mq{s}", [4, 128, S]), mk=dscr(f"mk{s}", [4, 128, S]),
            mv=dscr(f"mv{s}", [S, 512]), ms=dscr(f"ms{s}", [128, S]),
            at=dscr(f"at{s}", [8, 128, S]),
        ))

    with ExitStack() as top:
        SC = Sched(nc, top)
        op = SC.op
        uniq = {"n": 0}

        def sb(st, name, shape, dt):
            uniq["n"] += 1
            return st.enter_context(nc.sbuf_tensor(f"{name}_{uniq['n']}", list(shape), dt))

        banks = [top.enter_context(nc.psum_tensor(f"bank{i}", [128, 512], F32)) for i in range(8)]
        bres = [Res() for _ in range(8)]
        bstate = {"i": 0}

        def bank():
            i = bstate["i"]
            bstate["i"] = (i + 1) % 8
            return banks[i], bres[i]

        ident = sb(top, "ident", [128, 128], F32)
        identb = sb(top, "identb", [128, 128], BF16)
        ones = sb(top, "ones", [128, 128], BF16)
        r_const = Res()
        d_const = SC.dsem()
        SC.dma(ident[:], c_ident, d_const, writes=[r_const])
        SC.dma(identb[:], c_identb, d_const, writes=[r_const], nodeps=True)
        op("pool", lambda e: e.memset(ones[:], 1.0), writes=[r_const])

        d_wscr = SC.dsem()
        r_wscr = Res(scratch=True)

        def chk(tag):
            if stop == tag:
                SC.flush()
                raise _Stop()

        def make_stager(st, piece=2048):
            return dict(stgs=[sb(st, f"stg{i}", [128, piece], F32) for i in range(2)], rs=[Res(), Res()],
                        ds=[SC.dsem(), SC.dsem()], k=0, piece=piece)

        def cast_stream(sg_, src, ncols, dst_fn, piece=2048):
            for c0 in range(0, ncols, piece):
                c1 = min(ncols, c0 + piece)
                k = sg_["k"]
                sg_["k"] += 1
                b = k % 2
                SC.dma(sg_["stgs"][b][:, 0:c1 - c0], src[:, c0:c1], sg_["ds"][b], writes=[sg_["rs"][b]])
                dst_fn(c0, c1, sg_["stgs"][b][:, 0:c1 - c0], sg_["rs"][b], k)

        with ExitStack() as st:
            stb = [sb(st, f"stb{i}", [128, 2048], BF16) for i in range(2)]
            rsb = [Res(), Res()]
            cnt = {"k": 0}

            def to_scratch(dst):
                def f(c0, c1, stg, rstg, k):
                    kk = cnt["k"]
                    cnt["k"] += 1
                    b = kk % 2
                    eng = "act" if kk % 2 == 0 else "dve"
                    if eng == "act":
                        op("act", lambda e: e.activation(out=stb[b][:, 0:c1 - c0], in_=stg, func=AF.Copy),
                           reads=[rstg], writes=[rsb[b]])
                    else:
                        op("dve", lambda e: e.tensor_copy(out=stb[b][:, 0:c1 - c0], in_=stg),
                           reads=[rstg], writes=[rsb[b]])
                    SC.dma(dst[:, c0:c1], stb[b][:, 0:c1 - c0], d_wscr, reads=[rsb[b]], writes=[r_wscr], live=True)
                return f

            stager = make_stager(st)
            cast_stream(stager, w_o, 8 * 1024, to_scratch(wo_s))
            cast_stream(stager, w_dn, NPAIR * 1024, to_scratch(wdn_s))
            for p in range(NPAIR):
                cast_stream(stager, w_up[p], 2048, to_scratch(wup_s[p]))
            SC.flush()
        if stop == "pro":
            return nc

        d_out = SC.dsem()
        r_out = Res(scratch=True)

        for s in range(NSEQ):
            sc = scr[s]
            d_stA = SC.dsem()
            r_scrA = Res(scratch=True)
            d_stB = SC.dsem()
            r_scrB = Res(scratch=True)

            with ExitStack() as st:
                win = sb(st, "win", [128, 8, WIN_COLS], BF16)
                wuq = sb(st, "wuq", [128, 2, 1024], BF16)
                wukv = sb(st, "wukv", [128, 1024], BF16)
                qg_t = sb(st, "qg_t", [128, 2], F32)
                kvg_t = sb(st, "kvg_t", [128, 1], F32)
                r_w = Res()
                d_w = SC.dsem()
                SC.dma(qg_t[:], qg, d_w, writes=[r_w])
                SC.dma(kvg_t[:], kvg, d_w, writes=[r_w], nodeps=True)
                with ExitStack() as st2:
                    winf = win[:].rearrange("p c n -> p (c n)")
                    stager2 = make_stager(st2)

                    def f_win(c0, c1, stg, rstg, k):
                        op("act" if k % 2 == 0 else "dve",
                           (lambda e: e.activation(out=winf[:, c0:c1], in_=stg, func=AF.Copy)) if k % 2 == 0 else
                           (lambda e: e.tensor_copy(out=winf[:, c0:c1], in_=stg)),
                           reads=[rstg], writes=[r_w])
                    cast_stream(stager2, w_in, 8 * WIN_COLS, f_win)

                    def f_wuq(c0, c1, stg, rstg, k):
                        kc = c0 // 1024
                        op("dve", lambda e: e.tensor_scalar(out=wuq[:, kc, :], in0=stg, scalar1=qg_t[:, kc:kc + 1],
                                                            scalar2=None, op0=ALU.mult),
                           reads=[rstg, r_w], writes=[r_w])
                    cast_stream(stager2, w_uq, 2048, f_wuq, piece=1024)

                    def f_wukv(c0, c1, stg, rstg, k):
                        op("dve", lambda e: e.tensor_scalar(out=wukv[:, :], in0=stg, scalar1=kvg_t[:, 0:1],
                                                            scalar2=None, op0=ALU.mult),
                           reads=[rstg, r_w], writes=[r_w])
                    cast_stream(stager2, w_ukv, 1024, f_wukv, piece=1024)
                    SC.flush()

                xt = [sb(st, f"xt{i}", [128, 4, D], F32) for i in range(2)]
                r_xt = [Res(), Res()]
                d_xt = [SC.dsem(), SC.dsem()]
                rope_t = [sb(st, f"rope{i}", [128, 2, 512], F32) for i in range(2)]
                r_rope = [Res(), Res()]
                d_rope = [SC.dsem(), SC.dsem()]
                xT = sb(st, "xT", [128, 8, 512], BF16)
                r_xT = Res()
                cqf = sb(st, "cqf", [128, 3, 512], F32)
                r_cqf = Res()
                sq = sb(st, "sq", [128, 3, 512], BF16)
                r_sq = Res()
                rstd = sb(st, "rstd", [128, 2, 512], F32)
                r_rstd = Res()
                cqn = sb(st, "cqn", [128, 3, 512], BF16)
                r_cqn = Res()
                tmp1 = sb(st, "tmp1", [128, 512], F32)
                r_tmp1 = Res()
                tmp2 = sb(st, "tmp2", [128, 512], F32)
                r_tmp2 = Res()
                pastm = sb(st, "pastm", [128, 16, 16], F32)
                kmsum = sb(st, "kmsum", [128, 4, 16], F32)
                kmT = sb(st, "kmT", [128, 4, 32], BF16)
                r_km = Res()
                gm = sb(st, "gm", [128, 8, 16], F32)
                r_gm = Res()
                m8 = sb(st, "m8", [128, 8, 8], F32)
                r_m8 = Res()
                seln = sb(st, "seln", [128, 128], F32)
                r_seln = Res()
                tmpg = sb(st, "tmpg", [128, 8, 16], F32)
                r_tmpg = Res()
                d_pm = SC.dsem()
                r_pm = Res()
                SC.dma(pastm[:].rearrange("p a b -> p (a b)"), c_pastm, d_pm, writes=[r_pm])
                op("pool", lambda e: e.memset(kmT[:], 0.0), writes=[r_km])
                op("pool", lambda e: e.memset(kmsum[:], 0.0), writes=[r_km])

                def obuf(name, shape):
                    return sb(st, name, shape, BF16), Res()
                o_qn, r_qn = obuf("o_qn", [128, 4, 512])
                o_qp, r_qp = obuf("o_qp", [128, 2, 512])
                o_kn, r_kn = obuf("o_kn", [128, 4, 512])
                o_kp, r_kp = obuf("o_kp", [32, 512])
                o_va, r_va = obuf("o_va", [128, 4, 512])
                o_mq, r_mq = obuf("o_mq", [128, 4, 512])
                o_mk, r_mk = obuf("o_mk", [128, 4, 512])
                o_mv, r_mv = obuf("o_mv", [128, 4, 512])
                o_ms, r_ms = obuf("o_ms", [128, 512])

                evk = {"k": 0}

                def evac(outap, inap, reads, writes):
                    evk["k"] += 1
                    if evk["k"] % 2 == 0:
                        op("act", lambda e: e.activation(out=outap, in_=inap, func=AF.Copy), reads, writes)
                    else:
                        op("dve", lambda e: e.tensor_copy(out=outap, in_=inap), reads, writes)

                def load_tile(t):
                    b = t % 2
                    SC.dma(xt[b][:], x[s, t * 512:(t + 1) * 512, :].rearrange("(a p) d -> p a d", p=128),
                           d_xt[b], writes=[r_xt[b]])
                    SC.dma(rope_t[b][:].rearrange("p a n -> p (a n)"), c_rope[t], d_rope[b], writes=[r_rope[b]])

                load_tile(0)
                for t in range(NT):
                    b = t % 2
                    tsl = slice(t * 512, (t + 1) * 512)
                    if t + 1 < NT:
                        load_tile(t + 1)
                    for c in range(8):
                        bk, rb = bank()
                        for a in range(4):
                            op("pe", lambda e, a=a, c=c, bk=bk: e.transpose(
                                out=bk[:, a * 128:(a + 1) * 128], in_=xt[b][:, a, c * 128:(c + 1) * 128],
                                identity=ident[:]), reads=[r_xt[b], r_const], writes=[rb])
                        evac(xT[:, c, :], bk[:, :], [rb], [r_xT])

                    chk("A1")

                    def proj(col0, m, rhs_t=None):
                        bk, rb = bank()
                        for c in range(8):
                            op("pe", lambda e, c=c, bk=bk: e.matmul(bk[0:m, :], lhsT=win[:, c, col0:col0 + m],
                                                                     rhs=xT[:, c, :], start=(c == 0), stop=(c == 7)),
                               reads=[r_w, r_xT], writes=[rb])
                        return bk, rb

                    for m in range(3):
                        bk, rb = proj(m * 128, 128)
                        op("dve", lambda e, m=m, bk=bk: e.tensor_copy(out=cqf[:, m, :], in_=bk[:, :]),
                           reads=[rb], writes=[r_cqf])
                        op("act", lambda e, m=m: e.activation(out=sq[:, m, :], in_=cqf[:, m, :], func=AF.Square),
                           reads=[r_cqf], writes=[r_sq])
                    chk("A1b")
                    for g, (chs, n) in enumerate((((0, 1), 256.0), ((2,), 128.0))):
                        bk, rb = bank()
                        for i, m in enumerate(chs):
                            op("pe", lambda e, m=m, bk=bk, i=i, chs=chs: e.matmul(
                                bk[:, :], lhsT=ones[:], rhs=sq[:, m, :], start=(i == 0), stop=(i == len(chs) - 1)),
                               reads=[r_sq, r_const], writes=[rb])
                        op("dve", lambda e, bk=bk, n=n: e.tensor_scalar(out=tmp1[:], in0=bk[:, :], scalar1=1.0 / n,
                                                                        scalar2=EPS, op0=ALU.mult, op1=ALU.add),
                           reads=[rb], writes=[r_tmp1])
                        op("act", lambda e: e.activation(out=tmp2[:], in_=tmp1[:], func=AF.Sqrt),
                           reads=[r_tmp1], writes=[r_tmp2])
                        op("dve", lambda e, g=g: e.reciprocal(out=rstd[:, g, :], in_=tmp2[:]),
                           reads=[r_tmp2], writes=[r_rstd])
                        chk("A1c")
                        for m in chs:
                            op("pool", lambda e, m=m, g=g: e.tensor_tensor(out=cqn[:, m, :], in0=cqf[:, m, :],
                                                                           in1=rstd[:, g, :], op=ALU.mult),
                               reads=[r_cqf, r_rstd], writes=[r_cqn])

                    chk("A2")

                    def rope_comb(bkP, rbP, bkR, rbR, nrow, outap, rout):
                        op("dve", lambda e: e.tensor_tensor(out=tmp1[0:nrow, :], in0=bkP[0:nrow, :],
                                                            in1=rope_t[b][0:nrow, 0, :], op=ALU.mult),
                           reads=[rbP, r_rope[b]], writes=[r_tmp1])
                        op("dve", lambda e: e.tensor_tensor(out=tmp2[0:nrow, :], in0=bkR[0:nrow, :],
                                                            in1=rope_t[b][0:nrow, 1, :], op=ALU.mult),
                           reads=[rbR, r_rope[b]], writes=[r_tmp2])
                        op("pool", lambda e: e.tensor_tensor(out=outap, in0=tmp1[0:nrow, :], in1=tmp2[0:nrow, :],
                                                             op=ALU.add),
                           reads=[r_tmp1, r_tmp2], writes=[rout])

                    bkP, rbP = proj(384, 32)
                    bkR, rbR = proj(416, 32)
                    rope_comb(bkP, rbP, bkR, rbR, 32, o_kp[:, :], r_kp)

                    chk("A3")
                    for m in range(4):
                        bk, rb = proj(448 + m * 128, 128)
                        evac(o_mq[:, m, :], bk[:, :], [rb], [r_mq])
                    for m in range(4):
                        bk, rb = proj(960 + m * 128, 128)
                        op("dve", lambda e, m=m, bk=bk: e.tensor_copy(out=o_mk[:, m, :], in_=bk[:, :]),
                           reads=[rb], writes=[r_mk])
                        op("dve", lambda e, m=m, bk=bk: e.tensor_reduce(
                            out=kmsum[:, m, 2 * t:2 * t + 2], in_=bk[:, :].rearrange("p (b l) -> p b l", b=2),
                            op=ALU.add, axis=AX.X), reads=[rb], writes=[r_km])
                    for hp in range(2):
                        op("dve", lambda e, hp=hp: e.tensor_scalar(
                            out=kmT[64 * hp:64 * hp + 64, :, 16 * hp + 2 * t:16 * hp + 2 * t + 2],
                            in0=kmsum[64 * hp:64 * hp + 64, :, 2 * t:2 * t + 2],
                            scalar1=1.0 / 256.0, scalar2=None, op0=ALU.mult), reads=[r_km], writes=[r_km])
                    for a in range(4):
                        bk, rb = bank()
                        for c in range(8):
                            op("pe", lambda e, c=c, a=a, bk=bk: e.matmul(
                                bk[:, :], lhsT=xT[:, c, a * 128:(a + 1) * 128], rhs=win[:, c, 1472:1984],
                                start=(c == 0), stop=(c == 7)), reads=[r_w, r_xT], writes=[rb])
                        evac(o_mv[:, a, :], bk[:, :], [rb], [r_mv])

                    chk("A4")
                    for m in range(4):
                        bk, rb = bank()
                        for kc in range(2):
                            op("pe", lambda e, kc=kc, m=m, bk=bk: e.matmul(
                                bk[:, :], lhsT=wuq[:, kc, m * 128:(m + 1) * 128], rhs=cqn[:, kc, :],
                                start=(kc == 0), stop=(kc == 1)), reads=[r_w, r_cqn], writes=[rb])
                        evac(o_qn[:, m, :], bk[:, :], [rb], [r_qn])
                    for m in range(2):
                        pr = []
                        for off in (512, 768):
                            bk, rb = bank()
                            for kc in range(2):
                                op("pe", lambda e, kc=kc, m=m, bk=bk, off=off: e.matmul(
                                    bk[:, :], lhsT=wuq[:, kc, off + m * 128:off + (m + 1) * 128], rhs=cqn[:, kc, :],
                                    start=(kc == 0), stop=(kc == 1)), reads=[r_w, r_cqn], writes=[rb])
                            pr.append((bk, rb))
                        rope_comb(pr[0][0], pr[0][1], pr[1][0], pr[1][1], 128, o_qp[:, m, :], r_qp)
                    for m in range(4):
                        bk, rb = bank()
                        op("pe", lambda e, m=m, bk=bk: e.matmul(bk[:, :], lhsT=wukv[:, m * 128:(m + 1) * 128],
                                                                rhs=cqn[:, 2, :], start=True, stop=True),
                           reads=[r_w, r_cqn], writes=[rb])
                        evac(o_kn[:, m, :], bk[:, :], [rb], [r_kn])
                    for a in range(4):
                        bk, rb = bank()
                        op("pe", lambda e, a=a, bk=bk: e.matmul(bk[:, :], lhsT=cqn[:, 2, a * 128:(a + 1) * 128],
                                                                rhs=wukv[:, 512:1024], start=True, stop=True),
                           reads=[r_w, r_cqn], writes=[rb])
                        evac(o_va[:, a, :], bk[:, :], [rb], [r_va])

                    chk("A5")
                    bkT, rbT = bank()
                    for a in range(4):
                        own = 2 * t + a // 2
                        bk, rb = bank()
                        for m in range(4):
                            op("pe", lambda e, m=m, a=a, bk=bk: e.matmul(
                                bk[:, 32 * m:32 * m + 32], lhsT=o_mq[:, m, a * 128:(a + 1) * 128],
                                rhs=kmT[:, m, :], start=True, stop=True),
                               reads=[r_mq, r_km], writes=[rb])
                        op("dve", lambda e, bk=bk, own=own: e.tensor_tensor(
                            out=gm[:], in0=bk[:, 0:128].rearrange("p (h n) -> p h n", h=8),
                            in1=pastm[:, own:own + 1, :].broadcast_to([128, 8, 16]), op=ALU.add),
                           reads=[rb, r_pm], writes=[r_gm])
                        gm2 = seln[:].rearrange("p (h n) -> p h n", h=8)
                        op("dve", lambda e: e.tensor_copy(out=gm2, in_=gm[:]), reads=[r_gm], writes=[r_seln])
                        for rnd in range(3):
                            op("dve", lambda e: e.tensor_reduce(out=m8[:, :, 0:1], in_=gm2, op=ALU.max, axis=AX.X),
                               reads=[r_seln], writes=[r_m8])
                            op("dve", lambda e: e.tensor_tensor(out=m8[:, :, 1:2].broadcast_to([128, 8, 16]) if False else tmpg[:],
                                                                in0=gm2, in1=m8[:, :, 0:1].broadcast_to([128, 8, 16]),
                                                                op=ALU.is_ge), reads=[r_seln, r_m8], writes=[r_tmpg])
                            op("dve", lambda e: e.scalar_tensor_tensor(out=gm2, in0=tmpg[:], scalar=-2e30, in1=gm2,
                                                                       op0=ALU.mult, op1=ALU.add),
                               reads=[r_tmpg, r_seln], writes=[r_seln])
                        op("dve", lambda e: e.tensor_reduce(out=m8[:, :, 3:4], in_=gm2, op=ALU.max, axis=AX.X),
                           reads=[r_seln], writes=[r_m8])
                        op("dve", lambda e: e.tensor_tensor(
                            out=seln[:].rearrange("p (h n) -> p h n", h=8), in0=gm[:],
                            in1=m8[:, :, 3:4].broadcast_to([128, 8, 16]), op=ALU.is_lt),
                           reads=[r_gm, r_m8], writes=[r_seln])
                        op("pe", lambda e, a=a: e.transpose(out=bkT[:, a * 128:(a + 1) * 128], in_=seln[:],
                                                            identity=ident[:]),
                           reads=[r_seln, r_const], writes=[rbT])
                    evac(o_ms[:, :], bkT[:, :], [rbT], [r_ms])

                    chk("A6")
                    def store(dst, src, rsrc):
                        SC.dma(dst, src, d_stA, reads=[rsrc], writes=[r_scrA], live=True)
                    store(sc["qn"][:, :, tsl].rearrange("m p t -> p m t"), o_qn[:], r_qn)
                    store(sc["qp"][:, :, tsl].rearrange("m p t -> p m t"), o_qp[:], r_qp)
                    store(sc["kn"][:, :, tsl].rearrange("m p t -> p m t"), o_kn[:], r_kn)
                    store(sc["kp"][:, tsl], o_kp[:], r_kp)
                    store(sc["va"][tsl, :].rearrange("(a p) c -> p a c", p=128), o_va[:], r_va)
                    store(sc["mq"][:, :, tsl].rearrange("m p t -> p m t"), o_mq[:], r_mq)
                    store(sc["mk"][:, :, tsl].rearrange("m p t -> p m t"), o_mk[:], r_mk)
                    store(sc["mv"][tsl, :].rearrange("(a p) c -> p a c", p=128), o_mv[:], r_mv)
                    store(sc["ms"][:, tsl], o_ms[:], r_ms)
                SC.flush()

            if stop == "A":
                return nc
            with ExitStack() as st:
                QT = [sb(st, f"QT{i}", [128, S], BF16) for i in range(2)]
                KT = [sb(st, f"KT{i}", [128, S], BF16) for i in range(2)]
                VA = [sb(st, f"VA{i}", [128, NKT, 128], BF16) for i in range(2)]
                r_QT = [Res(), Res()]
                r_KT = [Res(), Res()]
                r_VA = [Res(), Res()]
                d_QT = [SC.dsem(), SC.dsem()]
                d_KT = [SC.dsem(), SC.dsem()]
                d_VA = [SC.dsem(), SC.dsem()]
                PT = [sb(st, f"PT{i}", [128, 512], BF16) for i in range(4)]
                r_PT = [Res() for _ in range(4)]
                aT = [sb(st, f"aT{i}", [128, S], BF16) for i in range(2)]
                r_aT = [Res(), Res()]
                rd = sb(st, "rd", [128, 512], F32)
                r_rd = Res()
                cmask = sb(st, "cmask", [128, 4, 512], BF16)
                abias = sb(st, "abias", [128, 8, 35], F32)
                r_cB = Res()
                d_cB = SC.dsem()
                SC.dma(cmask[:].rearrange("p a n -> p (a n)"), c_cmask, d_cB, writes=[r_cB])
                SC.dma(abias[:].rearrange("p a n -> p (a n)"), c_abias, d_cB, writes=[r_cB], nodeps=True)
                op("pool", lambda e: e.memset(VA[0][:, :, 64:128], 1.0), writes=[r_VA[0]])
                op("pool", lambda e: e.memset(VA[1][:, :, 0:64], 1.0), writes=[r_VA[1]])

                sbank = [(banks[i], bres[i]) for i in range(4)]
                obank = [(banks[4 + i], bres[4 + i]) for i in range(2)]

                def load_head(hh):
                    b = hh % 2
                    h = hh % 8
                    if hh < 8:
                        R = 96
                        SC.dma(QT[b][0:64, :], sc["qn"][h // 2, 64 * (h % 2):64 * (h % 2) + 64, :], d_QT[b],
                               reads=[r_scrA], writes=[r_QT[b]])
                        SC.dma(QT[b][64:96, :], sc["qp"][h // 4, 32 * (h % 4):32 * (h % 4) + 32, :], d_QT[b],
                               reads=[r_scrA], writes=[r_QT[b]], nodeps=True)
                        SC.dma(KT[b][0:64, :], sc["kn"][h // 2, 64 * (h % 2):64 * (h % 2) + 64, :], d_KT[b],
                               reads=[r_scrA], writes=[r_KT[b]])
                        SC.dma(KT[b][64:96, :], sc["kp"][:, :], d_KT[b], reads=[r_scrA], writes=[r_KT[b]], nodeps=True)
                        vsrc = sc["va"]
                    else:
                        SC.dma(QT[b][0:64, :], sc["mq"][h // 2, 64 * (h % 2):64 * (h % 2) + 64, :], d_QT[b],
                               reads=[r_scrA], writes=[r_QT[b]])
                        SC.dma(QT[b][64:66, :], c_qalibi[h], d_QT[b], writes=[r_QT[b]], nodeps=True)
                        SC.dma(QT[b][66:82, :], sc["ms"][16 * h:16 * h + 16, :], d_QT[b], writes=[r_QT[b]], nodeps=True)
                        SC.dma(KT[b][0:64, :], sc["mk"][h // 2, 64 * (h % 2):64 * (h % 2) + 64, :], d_KT[b],
                               reads=[r_scrA], writes=[r_KT[b]])
                        SC.dma(KT[b][64:82, :], c_kconst, d_KT[b], writes=[r_KT[b]], nodeps=True)
                        vsrc = sc["mv"]
                    c0 = 0 if b == 0 else 64
                    for k0 in range(0, NKT, 8):
                        SC.dma(VA[b][:, k0:k0 + 8, c0:c0 + 64],
                               vsrc[k0 * 128:(k0 + 8) * 128, 64 * h:64 * h + 64].rearrange("(k p) d -> p k d", p=128),
                               d_VA[b], reads=[r_scrA], writes=[r_VA[b]], nodeps=(k0 > 0))

                steps = []
                for hh in range(16):
                    for j in range(NT):
                        nk = 4 * j + 4
                        for kt in range(nk):
                            steps.append((hh, j, kt, kt == 0, kt == nk - 1))

                def emit_score(i):
                    hh, j, kt, first, last = steps[i]
                    b = hh % 2
                    R = 96 if hh < 8 else 82
                    r = kt - 4 * j
                    q0 = 128 * r if r > 0 else 0
                    bk, rb = sbank[i % 4]
                    diag = r >= 0
                    op("pe", lambda e: e.matmul(bk[:, q0:512], lhsT=KT[b][0:R, kt * 128:(kt + 1) * 128],
                                                rhs=QT[b][0:R, j * 512 + q0:(j + 1) * 512], start=True, stop=not diag),
                       reads=[r_KT[b], r_QT[b]], writes=[rb])
                    if diag:
                        op("pe", lambda e: e.matmul(bk[:, q0:512], lhsT=identb[:], rhs=cmask[:, r, q0:512],
                                                    start=False, stop=True), reads=[r_cB, r_const], writes=[rb])

                def emit_rest(i):
                    hh, j, kt, first, last = steps[i]
                    b = hh % 2
                    h = hh % 8
                    r = kt - 4 * j
                    q0 = 128 * r if r > 0 else 0
                    bk, rb = sbank[i % 4]
                    pt, rpt = PT[i % 4], r_PT[i % 4]
                    ob, rob = obank[(hh * NT + j) % 2]
                    if hh < 8:
                        op("act", lambda e: e.activation(out=pt[:, q0:512], in_=bk[:, q0:512], func=AF.Exp,
                                                         scale=SC_MLA), reads=[rb], writes=[rpt])
                    else:
                        di = (512 * j - 128 * kt + 384) // 128
                        op("act", lambda e: e.activation(out=pt[:, q0:512], in_=bk[:, q0:512], func=AF.Exp,
                                                         scale=SC_MOBA, bias=abias[:, h, di:di + 1]),
                           reads=[rb, r_cB], writes=[rpt])
                    op("pe", lambda e: e.matmul(ob[:, q0:512], lhsT=VA[b][:, kt, :], rhs=pt[:, q0:512],
                                                start=first, stop=last), reads=[r_VA[b], rpt], writes=[rob])
                    if last:
                        u0, d0 = (0, 64) if b == 0 else (64, 0)
                        pair = hh // 2
                        ab = pair % 2
                        op("dve", lambda e: e.reciprocal(out=rd[u0:u0 + 64, :], in_=ob[d0:d0 + 64, :]),
                           reads=[rob], writes=[r_rd])
                        op("dve", lambda e: e.tensor_tensor(out=aT[ab][u0:u0 + 64, j * 512:(j + 1) * 512],
                                                            in0=ob[u0:u0 + 64, :], in1=rd[u0:u0 + 64, :], op=ALU.mult),
                           reads=[rob, r_rd], writes=[r_aT[ab]])
                        if j == NT - 1 and b == 1:
                            SC.dma(sc["at"][pair], aT[ab][:], d_stB, reads=[r_aT[ab]], writes=[r_scrB], live=True)

                load_head(0)
                LOOK = 2
                nst = len(steps)
                for i in range(min(LOOK, nst)):
                    emit_score(i)
                for i in range(nst):
                    hh, j, kt, first, last = steps[i]
                    if first and j == 0 and hh + 1 < 16:
                        load_head(hh + 1)
                    if i + LOOK < nst:
                        emit_score(i + LOOK)
                    emit_rest(i)
                SC.flush()

            if stop == "B":
                return nc
            with ExitStack() as st:
                wo = sb(st, "wo", [128, 8, 1024], BF16)
                wdn = sb(st, "wdn", [128, NPAIR, 1024], BF16)
                lnt = sb(st, "lnt", [128, 4, 1024], F32)
                cvp = sb(st, "cvp", [128, 44, 4], F32)
                r_cw = Res()
                d_cw = SC.dsem()
                SC.dma(wo[:].rearrange("p a n -> p (a n)"), wo_s, d_cw, reads=[r_wscr], writes=[r_cw])
                SC.dma(wdn[:].rearrange("p a n -> p (a n)"), wdn_s, d_cw, writes=[r_cw], nodeps=True)
                SC.dma(lnt[:].rearrange("p a n -> p (a n)"), lnp, d_cw, writes=[r_cw], nodeps=True)
                SC.dma(cvp[:].rearrange("p a n -> p (a n)"), convp, d_cw, writes=[r_cw], nodeps=True)
                aTt = sb(st, "aTt", [128, 8, 512], BF16)
                r_aTt = Res()
                d_aTt = SC.dsem()
                xr = [sb(st, f"xr{i}", [128, D], F32) for i in range(2)]
                r_xr = [Res(), Res()]
                d_xr = [SC.dsem(), SC.dsem()]
                yb = sb(st, "yb", [128, D], F32)
                r_yb = Res()
                x1f = sb(st, "x1f", [128, 4, D], F32)
                r_x1f = Res()
                x1T = sb(st, "x1T", [128, 8, 512], BF16)
                r_x1T = Res()
                stats = sb(st, "stats", [128, 2, 6], F32)
                mv_ = sb(st, "mv_", [128, 2], F32)
                lns = sb(st, "lns", [128, 4], F32)
                r_ln = Res()
                wupt = [sb(st, f"wupt{i}", [128, 8, 256], BF16) for i in range(3)]
                r_wupt = [Res() for _ in range(3)]
                d_wupt = [SC.dsem() for _ in range(3)]
                hraw = [sb(st, f"hraw{i}", [128, 514], F32) for i in range(2)]
                r_hraw = [Res(), Res()]
                cacc = [sb(st, f"cacc{i}", [128, 512], F32) for i in range(2)]
                r_cacc = [Res(), Res()]
                sg = sb(st, "sg", [128, 512], F32)
                r_sg = Res()
                carry = sb(st, "carry", [128, 44, 2], F32)
                r_carry = Res()
                actT = sb(st, "actT", [128, NPAIR, 512], BF16)
                r_actT = Res()
                ot = [sb(st, f"ot{i}", [128, D], F32) for i in range(2)]
                r_ot = [Res(), Res()]
                op("pool", lambda e: e.memset(carry[:], 0.0), writes=[r_carry])

                def layer_norm(src, rsrc, gi, dst, rdst):
                    for hf in range(2):
                        op("dve", lambda e, hf=hf: e.bn_stats(out=stats[:, hf, :], in_=src[:, hf * 512:(hf + 1) * 512]),
                           reads=[rsrc], writes=[r_ln])
                    op("dve", lambda e: e.bn_aggr(out=mv_[:], in_=stats[:].rearrange("p a n -> p (a n)")),
                       reads=[r_ln], writes=[r_ln])
                    op("dve", lambda e: e.tensor_scalar(out=lns[:, 0:1], in0=mv_[:, 1:2], scalar1=EPS, scalar2=None,
                                                        op0=ALU.add), reads=[r_ln], writes=[r_ln])
                    op("act", lambda e: e.activation(out=lns[:, 1:2], in_=lns[:, 0:1], func=AF.Sqrt),
                       reads=[r_ln], writes=[r_ln])
                    op("dve", lambda e: e.reciprocal(out=lns[:, 2:3], in_=lns[:, 1:2]), reads=[r_ln], writes=[r_ln])
                    op("dve", lambda e: e.tensor_scalar(out=lns[:, 3:4], in0=mv_[:, 0:1], scalar1=-1.0,
                                                        scalar2=lns[:, 2:3], op0=ALU.mult, op1=ALU.mult),
                       reads=[r_ln], writes=[r_ln])
                    op("act", lambda e: e.activation(out=src[:], in_=src[:], func=AF.Identity, scale=lns[:, 2:3],
                                                     bias=lns[:, 3:4]), reads=[rsrc, r_ln], writes=[rsrc])
                    op("pool", lambda e: e.tensor_tensor(out=src[:], in0=src[:], in1=lnt[:, gi, :], op=ALU.mult),
                       reads=[rsrc, r_cw], writes=[rsrc])
                    op("pool", lambda e: e.tensor_tensor(out=dst, in0=src[:], in1=lnt[:, gi + 1, :], op=ALU.add),
                       reads=[rsrc, r_cw], writes=[rdst])

                def load_x(t, a):
                    k = (t * 4 + a) % 2
                    SC.dma(xr[k][:], x[s, t * 512 + a * 128:t * 512 + (a + 1) * 128, :], d_xr[k], writes=[r_xr[k]])

                wk = {"k": 0}

                def load_wup(p):
                    k = wk["k"] % 3
                    wk["k"] += 1
                    SC.dma(wupt[k][:].rearrange("p a n -> p (a n)"), wup_s[p], d_wupt[k], reads=[r_wscr],
                           writes=[r_wupt[k]])
                    return k

                for t in range(NT):
                    tsl = slice(t * 512, (t + 1) * 512)
                    SC.dma(aTt[:], sc["at"][:, :, tsl].rearrange("c p t -> p c t"), d_aTt, reads=[r_scrB],
                           writes=[r_aTt])
                    load_x(t, 0)
                    wq = [load_wup(0), load_wup(1)]
                    for a in range(4):
                        k = (t * 4 + a) % 2
                        if a + 1 < 4:
                            load_x(t, a + 1)
                        bks = []
                        for n in range(2):
                            bk, rb = bank()
                            for c in range(8):
                                op("pe", lambda e, c=c, n=n, bk=bk: e.matmul(
                                    bk[:, :], lhsT=aTt[:, c, a * 128:(a + 1) * 128], rhs=wo[:, c, n * 512:(n + 1) * 512],
                                    start=(c == 0), stop=(c == 7)), reads=[r_aTt, r_cw], writes=[rb])
                            bks.append((bk, rb))
                        for n in range(2):
                            bk, rb = bks[n]
                            op("dve", lambda e, n=n, bk=bk: e.scalar_tensor_tensor(
                                out=yb[:, n * 512:(n + 1) * 512], in0=xr[k][:, n * 512:(n + 1) * 512], scalar=ALPHA,
                                in1=bk[:, :], op0=ALU.mult, op1=ALU.add), reads=[r_xr[k], rb], writes=[r_yb])
                        layer_norm(yb, r_yb, 0, x1f[:, a, :], r_x1f)
                    for c in range(8):
                        bk, rb = bank()
                        for a in range(4):
                            op("pe", lambda e, a=a, c=c, bk=bk: e.transpose(
                                out=bk[:, a * 128:(a + 1) * 128], in_=x1f[:, a, c * 128:(c + 1) * 128],
                                identity=ident[:]), reads=[r_x1f, r_const], writes=[rb])
                        if c % 2 == 0:
                            op("act", lambda e, c=c, bk=bk: e.activation(out=x1T[:, c, :], in_=bk[:, :], func=AF.Copy),
                               reads=[rb], writes=[r_x1T])
                        else:
                            op("dve", lambda e, c=c, bk=bk: e.tensor_copy(out=x1T[:, c, :], in_=bk[:, :]),
                               reads=[rb], writes=[r_x1T])
                    for p in range(NPAIR):
                        kw = wq.pop(0)
                        if p + 2 < NPAIR:
                            wq.append(load_wup(p + 2))
                        for gu in range(2):
                            ch = p + 22 * gu
                            bk, rb = bank()
                            for c in range(8):
                                op("pe", lambda e, c=c, bk=bk, gu=gu: e.matmul(
                                    bk[:, :], lhsT=wupt[kw][:, c, gu * 128:(gu + 1) * 128], rhs=x1T[:, c, :],
                                    start=(c == 0), stop=(c == 7)), reads=[r_wupt[kw], r_x1T], writes=[rb])
                            hr, rhr = hraw[gu], r_hraw[gu]
                            ca, rca = cacc[gu], r_cacc[gu]
                            op("pool", lambda e, hr=hr, ch=ch: e.tensor_copy(out=hr[:, 0:2], in_=carry[:, ch, :]),
                               reads=[r_carry], writes=[rhr])
                            op("act", lambda e, hr=hr, bk=bk: e.activation(out=hr[:, 2:514], in_=bk[:, :], func=AF.Copy),
                               reads=[rb], writes=[rhr])
                            op("pool", lambda e, hr=hr, ch=ch: e.tensor_copy(out=carry[:, ch, :], in_=hr[:, 512:514]),
                               reads=[rhr], writes=[r_carry])
                            op("dve", lambda e, ca=ca, hr=hr, ch=ch: e.tensor_scalar(
                                out=ca[:], in0=hr[:, 2:514], scalar1=cvp[:, ch, 2:3], scalar2=cvp[:, ch, 3:4],
                                op0=ALU.mult, op1=ALU.add), reads=[rhr, r_cw], writes=[rca])
                            op("dve", lambda e, ca=ca, hr=hr, ch=ch: e.scalar_tensor_tensor(
                                out=ca[:], in0=hr[:, 1:513], scalar=cvp[:, ch, 1:2], in1=ca[:],
                                op0=ALU.mult, op1=ALU.add), reads=[rhr, rca, r_cw], writes=[rca])
                            op("dve", lambda e, ca=ca, hr=hr, ch=ch: e.scalar_tensor_tensor(
                                out=ca[:], in0=hr[:, 0:512], scalar=cvp[:, ch, 0:1], in1=ca[:],
                                op0=ALU.mult, op1=ALU.add), reads=[rhr, rca, r_cw], writes=[rca])
                        op("act", lambda e: e.activation(out=sg[:], in_=cacc[0][:], func=AF.Silu),
                           reads=[r_cacc[0]], writes=[r_sg])
                        op("pool", lambda e, p=p: e.tensor_tensor(out=actT[:, p, :], in0=sg[:], in1=cacc[1][:],
                                                                  op=ALU.mult),
                           reads=[r_sg, r_cacc[1]], writes=[r_actT])
                    for a in range(4):
                        ko = (t * 4 + a) % 2
                        bks = []
                        for n in range(2):
                            bk, rb = bank()
                            for p in range(NPAIR):
                                op("pe", lambda e, p=p, n=n, bk=bk: e.matmul(
                                    bk[:, :], lhsT=actT[:, p, a * 128:(a + 1) * 128], rhs=wdn[:, p, n * 512:(n + 1) * 512],
                                    start=(p == 0), stop=(p == NPAIR - 1)), reads=[r_actT, r_cw], writes=[rb])
                            bks.append((bk, rb))
                        for n in range(2):
                            bk, rb = bks[n]
                            op("dve", lambda e, n=n, bk=bk: e.scalar_tensor_tensor(
                                out=yb[:, n * 512:(n + 1) * 512], in0=x1f[:, a, n * 512:(n + 1) * 512], scalar=ALPHA,
                                in1=bk[:, :], op0=ALU.mult, op1=ALU.add), reads=[r_x1f, rb], writes=[r_yb])
                        layer_norm(yb, r_yb, 2, ot[ko][:], r_ot[ko])
                        SC.dma(out[s, t * 512 + a * 128:t * 512 + (a + 1) * 128, :], ot[ko][:], d_out,
                               reads=[r_ot[ko]], writes=[r_out], live=True)
                SC.flush()

        SC._wait("sp", ("d", d_out, d_out.cnt))
        SC.flush()
    return nc


def _bf(a):
    return np.ascontiguousarray(a.astype(ml_dtypes.bfloat16))


def make_consts(S):
    NT = S // 512
    c = {}
    c["c_ident"] = np.eye(128, dtype=np.float32)
    c["c_identb"] = _bf(np.eye(128, dtype=np.float32))
    k = np.arange(128)[:, None, None]
    r = np.arange(4)[None, :, None]
    q = np.arange(512)[None, None, :]
    c["c_cmask"] = _bf(np.where(128 * r + k > q, NEGBIG, 0.0).astype(np.float32).reshape(128, 2048))
    half = 16
    inv = (np.float32(10000.0) ** (-np.arange(half, dtype=np.float32) / np.float32(half))).astype(np.float32)
    pos = np.arange(S, dtype=np.float32)
    ang = (pos[:, None] * inv[None, :]).astype(np.float32)
    cos = np.cos(ang).astype(np.float32)
    sin = np.sin(ang).astype(np.float32)
    d = np.arange(128) % 32
    cosT = cos[:, d % 16].T
    sinT = sin[:, d % 16].T * np.where(d < 16, -1.0, 1.0)[:, None]
    rope = np.stack([cosT.reshape(128, NT, 512), sinT.reshape(128, NT, 512)], axis=2)
    c["c_rope"] = np.ascontiguousarray(rope.transpose(1, 0, 2, 3).reshape(NT, 128, 1024).astype(np.float32))
    kc = np.zeros((18, S), np.float32)
    kc[0:2] = 1.0
    blk = np.arange(S) // 256
    for n in range(16):
        kc[2 + n] = np.where(blk == n, NEGBIG, 0.0)
    c["c_kconst"] = _bf(kc)
    slopes = (2.0 ** (-8.0 * np.arange(1, 9, dtype=np.float32) / 8.0)).astype(np.float32)
    dq = (np.arange(S) % 512).astype(np.float32)
    v = (-slopes[:, None] * dq[None, :] / np.float32(SC_MOBA)).astype(np.float32)
    hi = v.astype(ml_dtypes.bfloat16)
    lo = (v - hi.astype(np.float32)).astype(ml_dtypes.bfloat16)
    c["c_qalibi"] = np.ascontiguousarray(np.stack([hi, lo], axis=1))
    i = np.arange(35)
    Dd = (128 * i - 384).astype(np.float32)
    kk = np.arange(128, dtype=np.float32)
    ab = -slopes[None, :, None] * (Dd[None, None, :] - kk[:, None, None])
    c["c_abias"] = np.ascontiguousarray(ab.astype(np.float32).reshape(128, 8 * 35))
    own = np.arange(16)[:, None]
    n = np.arange(16)[None, :]
    pm = np.where(n < own, 0.0, np.where(n == own, 1e30, -1e30)).astype(np.float32)
    c["c_pastm"] = np.ascontiguousarray(np.broadcast_to(pm.reshape(1, 256), (128, 256)))
    return c


def prep_weights(w_in, q_norm_g, w_uq, kv_norm_g, w_ukv, w_o, ln1_g, ln1_b, w_up, conv_w, conv_b, w_down,
                 ln2_g, ln2_b):
    f = lambda a: np.ascontiguousarray(np.asarray(a, dtype=np.float32))
    w_in, w_uq, w_ukv, w_o, w_up, w_down = (f(a[0]) for a in (w_in, w_uq, w_ukv, w_o, w_up, w_down))
    r = np.arange(32)
    kr_sw = w_in[:, 384 + ((r + 16) % 32)]
    win = np.concatenate([w_in[:, 0:416], kr_sw, w_in[:, 416:1952]], axis=1)
    win = win.reshape(8, 128, WIN_COLS).transpose(1, 0, 2).reshape(128, 8 * WIN_COLS)
    h = np.arange(8)
    nope = (h[:, None] * 96 + np.arange(64)[None, :]).reshape(-1)
    pe = (h[:, None] * 96 + 64 + r[None, :]).reshape(-1)
    pesw = (h[:, None] * 96 + 64 + ((r + 16) % 32)[None, :]).reshape(-1)
    wuq = w_uq[:, np.concatenate([nope, pe, pesw])]
    wuq = wuq.reshape(2, 128, 1024).transpose(1, 0, 2).reshape(128, 2048)
    kcols = (h[:, None] * 128 + np.arange(64)[None, :]).reshape(-1)
    vcols = kcols + 64
    wukv = w_ukv[:, np.concatenate([kcols, vcols])]
    wo = w_o.reshape(8, 128, 1024).transpose(1, 0, 2).reshape(128, 8192)
    wdn = w_down.reshape(NPAIR, 128, 1024).transpose(1, 0, 2).reshape(128, NPAIR * 1024)
    wu = w_up.reshape(8, 128, 2, NPAIR, 128)
    wu = wu.transpose(3, 1, 0, 2, 4).reshape(NPAIR, 128, 8 * 256)
    lnp = np.stack([f(ln1_g[0]), f(ln1_b[0]), f(ln2_g[0]), f(ln2_b[0])], axis=0).reshape(1, 4096)
    lnp = np.broadcast_to(lnp, (128, 4096))
    cw = f(conv_w[0])
    cb = f(conv_b[0])
    cv = np.concatenate([cw, cb[None, :]], axis=0)
    cv = cv.reshape(4, 44, 128).transpose(2, 1, 0).reshape(128, 176)
    return {
        "w_in": f(win), "w_uq": f(wuq), "w_ukv": f(wukv), "w_o": f(wo), "w_dn": f(wdn), "w_up": f(wu),
        "qg": f(f(q_norm_g[0]).reshape(2, 128).T), "kvg": f(f(kv_norm_g[0]).reshape(128, 1)),
        "lnp": f(lnp), "convp": f(cv),
    }


_NC_CACHE = {}


def run(x, params, ncores, debug=False, stop=None):
    B, S, _ = x.shape
    nseq = B // ncores
    key = (nseq, S, debug)
    if key not in _NC_CACHE:
        _NC_CACHE[key] = build(nseq, S, debug, stop)
    nc = _NC_CACHE[key]
    shared = dict(prep_weights(**params))
    shared.update(make_consts(S))
    xs = np.ascontiguousarray(np.asarray(x, dtype=np.float32)).reshape(ncores, nseq, S, D)
    in_maps = [dict(shared, x=xs[i]) for i in range(ncores)]
    res = run_bass_kernel_spmd(nc, in_maps, core_ids=list(range(ncores)))
    return res


def kernel(x, w_in, q_norm_g, w_uq, kv_norm_g, w_ukv, w_o, ln1_g, ln1_b, w_up, conv_w, conv_b, w_down,
           ln2_g, ln2_b):
    params = dict(w_in=w_in, q_norm_g=q_norm_g, w_uq=w_uq, kv_norm_g=kv_norm_g, w_ukv=w_ukv, w_o=w_o,
                  ln1_g=ln1_g, ln1_b=ln1_b, w_up=w_up, conv_w=conv_w, conv_b=conv_b, w_down=w_down,
                  ln2_g=ln2_g, ln2_b=ln2_b)
    x = np.asarray(x)
    res = run(x, params, NCORES)
    outs = [np.asarray(r["out"]) for r in res.results]
    return np.concatenate(outs, axis=0).astype(np.float32)
```

```python
import math
from contextlib import ExitStack

import numpy as np
import ml_dtypes
import concourse.bass as bass
import concourse.mybir as mybir
from concourse.bass_utils import run_bass_kernel_spmd

F32 = mybir.dt.float32
BF16 = mybir.dt.bfloat16
AF = mybir.ActivationFunctionType
ALU = mybir.AluOpType
AX = mybir.AxisListType

D = 1024
NCORES = 8
EPS = 1e-5
NEGBIG = -30000.0
ALPHA = 2.0 ** 0.25
SC_MLA = 96.0 ** -0.5
SC_MOBA = 0.125
DFF = 2816
NPAIR = 22
WIN_COLS = 1984
import os as _os
ALL_INC = _os.environ.get("ALL_INC", "0") == "1"


class Res:
    __slots__ = ("w", "r", "scratch")

    def __init__(self, scratch=False):
        self.w = None
        self.r = []
        self.scratch = scratch


class DSem:
    def __init__(self, sem):
        self.sem = sem
        self.cnt = 0


class _Rec:
    def __getattr__(self, name):
        def f(*a, **k):
            self.call = (name, a, k)
        return f


class Sched:
    ENGS = ("pe", "act", "dve", "pool", "sp")
    CENGS = ("pe", "act", "dve", "pool")

    def __init__(self, nc, stack):
        self.nc = nc
        self.stack = stack
        self.prog = {e: [] for e in self.ENGS}
        self.sem = {}
        self.cnt = {}
        self.targets = {e: set() for e in self.CENGS}
        for e in self.CENGS:
            self.sem[e] = stack.enter_context(nc.semaphore("s_" + e))
            self.cnt[e] = 0
        self.seen = {e: {} for e in self.ENGS}
        self.dsems = []
        self.rank = {e: {} for e in self.CENGS}
        self.flushed = {e: 0 for e in self.CENGS}

    def dsem(self):
        s = self.stack.enter_context(self.nc.semaphore(f"d{len(self.dsems)}"))
        d = DSem(s)
        self.dsems.append(d)
        return d

    def _wait(self, eng, ev):
        if ev is None:
            return
        kind, obj, val = ev
        if kind == "e":
            if obj == eng and eng == "pe":
                return
            key = obj
        else:
            key = id(obj)
            if val is None:
                val = obj.cnt
        if val <= 0 or self.seen[eng].get(key, 0) >= val:
            return
        self.seen[eng][key] = val
        if kind == "e":
            assert val > self.flushed[obj] or val in self.rank[obj], (eng, obj, val)
            self.targets[obj].add(val)
        self.prog[eng].append(("w", kind, obj, val))

    def _deps(self, eng, reads, writes):
        for r in reads:
            self._wait(eng, r.w)
        for w in writes:
            if w.scratch:
                continue
            self._wait(eng, w.w)
            for ev in w.r:
                self._wait(eng, ev)

    def _commit(self, ev, reads, writes):
        for r in reads:
            if not r.scratch:
                r.r.append(ev)
        for w in writes:
            w.w = ev
            w.r = []

    def op(self, eng, fn, reads=(), writes=()):
        self._deps(eng, reads, writes)
        self.cnt[eng] += 1
        ev = ("e", eng, self.cnt[eng])
        rec = _Rec()
        fn(rec)
        if ALL_INC:
            self.targets[eng].add(self.cnt[eng])
        self.prog[eng].append(("i", rec.call, self.cnt[eng]))
        self._commit(ev, reads, writes)
        return ev

    def dma(self, out, in_, ds, reads=(), writes=(), live=False, nodeps=False, q="sp"):
        if not nodeps:
            self._deps(q, reads, writes)
        ds.cnt += 16
        assert ds.cnt < 65000
        ev = ("d", ds, None if live else ds.cnt)
        self.prog[q].append(("d", out, in_, ds.sem))
        self._commit(ev, reads, writes)
        return ev

    def barrier(self):
        for e in self.ENGS:
            for o in self.CENGS:
                self._wait(e, ("e", o, self.cnt[o]))
            for d in self.dsems:
                self._wait(e, ("d", d, d.cnt))

    def flush(self):
        self.barrier()
        rank = self.rank
        for e in self.CENGS:
            new = sorted(v for v in self.targets[e] if v not in rank[e])
            base = len(rank[e])
            for i, v in enumerate(new):
                assert v > self.flushed[e]
                rank[e][v] = base + i + 1
            assert len(rank[e]) < 65000, (e, len(rank[e]))

        def replay(name):
            def body(eng):
                for it in self.prog[name]:
                    if it[0] == "w":
                        _, kind, obj, val = it
                        if kind == "e":
                            eng.wait_ge(self.sem[obj], rank[obj][val])
                        else:
                            eng.wait_ge(obj.sem, val)
                    elif it[0] == "i":
                        nm, a, k = it[1]
                        ins = getattr(eng, nm)(*a, **k)
                        if it[2] in rank[name]:
                            ins.then_inc(self.sem[name], 1)
                    else:
                        eng.dma_start(out=it[1], in_=it[2]).then_inc(it[3], 16)
            return body

        with self.nc.Block() as block:
            block.tensor(replay("pe"))
            block.scalar(replay("act"))
            block.vector(replay("dve"))
            block.gpsimd(replay("pool"))
            block.sync(replay("sp"))
        for e in self.ENGS:
            self.prog[e] = []
        for e in self.CENGS:
            self.flushed[e] = self.cnt[e]


class _Stop(Exception):
    pass


def build(NSEQ, S, debug=False, stop=None):
    st_ = {}
    try:
        return _build(NSEQ, S, debug, stop, st_)
    except _Stop:
        return st_["nc"]


def _build(NSEQ, S, debug, stop, st_):
    assert S % 512 == 0
    NT = S // 512
    NKT = S // 128
    nc = bass.Bass("TRN2", target_bir_lowering=False)
    st_["nc"] = nc

    def din(name, shape, dt=F32):
        return nc.dram_tensor(name, list(shape), dt, kind="ExternalInput").ap()

    def dscr(name, shape, dt=BF16):
        kind = "ExternalOutput" if debug else "Internal"
        return nc.dram_tensor(name, list(shape), dt, kind=kind).ap()

    x = din("x", [NSEQ, S, D])
    out = nc.dram_tensor("out", [NSEQ, S, D], F32, kind="ExternalOutput").ap()
    w_in = din("w_in", [128, 8 * WIN_COLS])
    w_uq = din("w_uq", [128, 2 * 1024])
    w_ukv = din("w_ukv", [128, 1024])
    qg = din("qg", [128, 2])
    kvg = din("kvg", [128, 1])
    w_o = din("w_o", [128, 8 * 1024])
    w_dn = din("w_dn", [128, NPAIR * 1024])
    w_up = din("w_up", [NPAIR, 128, 2048])
    lnp = din("lnp", [128, 4 * 1024])
    convp = din("convp", [128, 44 * 4])
    c_ident = din("c_ident", [128, 128])
    c_identb = din("c_identb", [128, 128], BF16)
    c_cmask = din("c_cmask", [128, 4 * 512], BF16)
    c_rope = din("c_rope", [NT, 128, 2 * 512])
    c_kconst = din("c_kconst", [18, S], BF16)
    c_qalibi = din("c_qalibi", [8, 2, S], BF16)
    c_abias = din("c_abias", [128, 8 * 35])
    c_pastm = din("c_pastm", [128, 16 * 16])

    wo_s = dscr("wo_s", [128, 8 * 1024])
    wdn_s = dscr("wdn_s", [128, NPAIR * 1024])
    wup_s = dscr("wup_s", [NPAIR, 128, 2048])
    scr = []
    for s in range(NSEQ):
        scr.append(dict(
            qn=dscr(f"qn{s}", [4, 128, S]), qp=dscr(f"qp{s}", [2, 128, S]),
            kn=dscr(f"kn{s}", [4, 128, S]), kp=dscr(f"kp{s}", [32, S]),
            va=dscr(f"va{s}", [S, 512]),
            mq=dscr(f"mq{s}", [4, 128, S]), mk=dscr(f"mk{s}", [4, 128, S]),
            mv=dscr(f"mv{s}", [S, 512]), ms=dscr(f"ms{s}", [128, S]),
            at=dscr(f"at{s}", [8, 128, S]),
        ))

    with ExitStack() as top:
        SC = Sched(nc, top)
        op = SC.op
        uniq = {"n": 0}

        def sb(st, name, shape, dt):
            uniq["n"] += 1
            return st.enter_context(nc.sbuf_tensor(f"{name}_{uniq['n']}", list(shape), dt))

        banks = [top.enter_context(nc.psum_tensor(f"bank{i}", [128, 512], F32)) for i in range(8)]
        bres = [Res() for _ in range(8)]
        bstate = {"i": 0}

        def bank():
            i = bstate["i"]
            bstate["i"] = (i + 1) % 8
            return banks[i], bres[i]

        ident = sb(top, "ident", [128, 128], F32)
        identb = sb(top, "identb", [128, 128], BF16)
        ones = sb(top, "ones", [128, 128], BF16)
        r_const = Res()
        d_const = SC.dsem()
        SC.dma(ident[:], c_ident, d_const, writes=[r_const])
        SC.dma(identb[:], c_identb, d_const, writes=[r_const], nodeps=True)
        op("pool", lambda e: e.memset(ones[:], 1.0), writes=[r_const])

        d_wscr = SC.dsem()
        r_wscr = Res(scratch=True)

        def chk(tag):
            if stop == tag:
                SC.flush()
                raise _Stop()

        def make_stager(st, piece=2048):
            return dict(stgs=[sb(st, f"stg{i}", [128, piece], F32) for i in range(2)], rs=[Res(), Res()],
                        ds=[SC.dsem(), SC.dsem()], k=0, piece=piece)

        def cast_stream(sg_, src, ncols, dst_fn, piece=2048):
            for c0 in range(0, ncols, piece):
                c1 = min(ncols, c0 + piece)
                k = sg_["k"]
                sg_["k"] += 1
                b = k % 2
                SC.dma(sg_["stgs"][b][:, 0:c1 - c0], src[:, c0:c1], sg_["ds"][b], writes=[sg_["rs"][b]])
                dst_fn(c0, c1, sg_["stgs"][b][:, 0:c1 - c0], sg_["rs"][b], k)

        with ExitStack() as st:
            stb = [sb(st, f"stb{i}", [128, 2048], BF16) for i in range(2)]
            rsb = [Res(), Res()]
            cnt = {"k": 0}

            def to_scratch(dst):
                def f(c0, c1, stg, rstg, k):
                    kk = cnt["k"]
                    cnt["k"] += 1
                    b = kk % 2
                    eng = "act" if kk % 2 == 0 else "dve"
                    if eng == "act":
                        op("act", lambda e: e.activation(out=stb[b][:, 0:c1 - c0], in_=stg, func=AF.Copy),
                           reads=[rstg], writes=[rsb[b]])
                    else:
                        op("dve", lambda e: e.tensor_copy(out=stb[b][:, 0:c1 - c0], in_=stg),
                           reads=[rstg], writes=[rsb[b]])
                    SC.dma(dst[:, c0:c1], stb[b][:, 0:c1 - c0], d_wscr, reads=[rsb[b]], writes=[r_wscr], live=True)
                return f

            stager = make_stager(st)
            cast_stream(stager, w_o, 8 * 1024, to_scratch(wo_s))
            cast_stream(stager, w_dn, NPAIR * 1024, to_scratch(wdn_s))
            for p in range(NPAIR):
                cast_stream(stager, w_up[p], 2048, to_scratch(wup_s[p]))
            SC.flush()
        if stop == "pro":
            return nc

        d_out = SC.dsem()
        r_out = Res(scratch=True)

        for s in range(NSEQ):
            sc = scr[s]
            d_stA = SC.dsem()
            r_scrA = Res(scratch=True)
            d_stB = SC.dsem()
            r_scrB = Res(scratch=True)

            with ExitStack() as st:
                win = sb(st, "win", [128, 8, WIN_COLS], BF16)
                wuq = sb(st, "wuq", [128, 2, 1024], BF16)
                wukv = sb(st, "wukv", [128, 1024], BF16)
                qg_t = sb(st, "qg_t", [128, 2], F32)
                kvg_t = sb(st, "kvg_t", [128, 1], F32)
                r_w = Res()
                d_w = SC.dsem()
                SC.dma(qg_t[:], qg, d_w, writes=[r_w])
                SC.dma(kvg_t[:], kvg, d_w, writes=[r_w], nodeps=True)
                with ExitStack() as st2:
                    winf = win[:].rearrange("p c n -> p (c n)")
                    stager2 = make_stager(st2)

                    def f_win(c0, c1, stg, rstg, k):
                        op("act" if k % 2 == 0 else "dve",
                           (lambda e: e.activation(out=winf[:, c0:c1], in_=stg, func=AF.Copy)) if k % 2 == 0 else
                           (lambda e: e.tensor_copy(out=winf[:, c0:c1], in_=stg)),
                           reads=[rstg], writes=[r_w])
                    cast_stream(stager2, w_in, 8 * WIN_COLS, f_win)

                    def f_wuq(c0, c1, stg, rstg, k):
                        kc = c0 // 1024
                        op("dve", lambda e: e.tensor_scalar(out=wuq[:, kc, :], in0=stg, scalar1=qg_t[:, kc:kc + 1],
                                                            scalar2=None, op0=ALU.mult),
                           reads=[rstg, r_w], writes=[r_w])
                    cast_stream(stager2, w_uq, 2048, f_wuq, piece=1024)

                    def f_wukv(c0, c1, stg, rstg, k):
                        op("dve", lambda e: e.tensor_scalar(out=wukv[:, :], in0=stg, scalar1=kvg_t[:, 0:1],
                                                            scalar2=None, op0=ALU.mult),
                           reads=[rstg, r_w], writes=[r_w])
                    cast_stream(stager2, w_ukv, 1024, f_wukv, piece=1024)
                    SC.flush()

                xt = [sb(st, f"xt{i}", [128, 4, D], F32) for i in range(2)]
                r_xt = [Res(), Res()]
                d_xt = [SC.dsem(), SC.dsem()]
                rope_t = [sb(st, f"rope{i}", [128, 2, 512], F32) for i in range(2)]
                r_rope = [Res(), Res()]
                d_rope = [SC.dsem(), SC.dsem()]
                xT = sb(st, "xT", [128, 8, 512], BF16)
                r_xT = [Res() for _ in range(8)]
                cqf = sb(st, "cqf", [128, 3, 512], F32)
                r_cqf = [Res() for _ in range(3)]
                sq = sb(st, "sq", [128, 3, 512], BF16)
                r_sq = [Res() for _ in range(3)]
                rstd = sb(st, "rstd", [128, 2, 512], F32)
                r_rstd = [Res(), Res()]
                cqn = sb(st, "cqn", [128, 3, 512], BF16)
                r_cqn = [Res() for _ in range(3)]
                tmp1 = sb(st, "tmp1", [128, 512], F32)
                r_tmp1 = Res()
                tmp2 = sb(st, "tmp2", [128, 512], F32)
                r_tmp2 = Res()
                rtmp = [(sb(st, f"rtA{i}", [128, 512], F32), Res(), sb(st, f"rtB{i}", [128, 512], F32), Res())
                        for i in range(2)]
                rtk = {"k": 0}
                pastm = sb(st, "pastm", [128, 16, 16], F32)
                kmsum = sb(st, "kmsum", [128, 4, 16], F32)
                kmT = sb(st, "kmT", [128, 4, 32], BF16)
                r_km = Res()
                gm = sb(st, "gm", [128, 8, 16], F32)
                r_gm = Res()
                m8 = sb(st, "m8", [128, 8, 8], F32)
                r_m8 = Res()
                seln = sb(st, "seln", [128, 128], F32)
                r_seln = Res()
                tmpg = sb(st, "tmpg", [128, 8, 16], F32)
                r_tmpg = Res()
                d_pm = SC.dsem()
                r_pm = Res()
                SC.dma(pastm[:].rearrange("p a b -> p (a b)"), c_pastm, d_pm, writes=[r_pm])
                op("pool", lambda e: e.memset(kmT[:], 0.0), writes=[r_km])
                op("pool", lambda e: e.memset(kmsum[:], 0.0), writes=[r_km])

                def obuf(name, shape, n=1):
                    return sb(st, name, shape, BF16), ([Res() for _ in range(n)] if n > 1 else Res())
                o_qn, r_qn = obuf("o_qn", [128, 4, 512], 4)
                o_qp, r_qp = obuf("o_qp", [128, 2, 512], 2)
                o_kn, r_kn = obuf("o_kn", [128, 4, 512], 4)
                o_kp, r_kp = obuf("o_kp", [32, 512])
                o_va, r_va = obuf("o_va", [128, 4, 512], 4)
                o_mq, r_mq = obuf("o_mq", [128, 4, 512], 4)
                o_mk, r_mk = obuf("o_mk", [128, 4, 512], 4)
                o_mv, r_mv = obuf("o_mv", [128, 4, 512], 4)
                o_ms, r_ms = obuf("o_ms", [128, 512])

                evk = {"k": 0}

                def evac(outap, inap, reads, writes):
                    evk["k"] += 1
                    if evk["k"] % 2 == 0:
                        op("act", lambda e: e.activation(out=outap, in_=inap, func=AF.Copy), reads, writes)
                    else:
                        op("dve", lambda e: e.tensor_copy(out=outap, in_=inap), reads, writes)

                def load_tile(t):
                    b = t % 2
                    SC.dma(xt[b][:], x[s, t * 512:(t + 1) * 512, :].rearrange("(a p) d -> p a d", p=128),
                           d_xt[b], writes=[r_xt[b]])
                    SC.dma(rope_t[b][:].rearrange("p a n -> p (a n)"), c_rope[t], d_rope[b], writes=[r_rope[b]])

                load_tile(0)
                for t in range(NT):
                    b = t % 2
                    tsl = slice(t * 512, (t + 1) * 512)
                    if t + 1 < NT:
                        load_tile(t + 1)
                    for c in range(8):
                        bk, rb = bank()
                        for a in range(4):
                            op("pe", lambda e, a=a, c=c, bk=bk: e.transpose(
                                out=bk[:, a * 128:(a + 1) * 128], in_=xt[b][:, a, c * 128:(c + 1) * 128],
                                identity=ident[:]), reads=[r_xt[b], r_const], writes=[rb])
                        evac(xT[:, c, :], bk[:, :], [rb], [r_xT[c]])

                    chk("A1")

                    def proj(col0, m, rhs_t=None):
                        bk, rb = bank()
                        for c in range(8):
                            op("pe", lambda e, c=c, bk=bk: e.matmul(bk[0:m, :], lhsT=win[:, c, col0:col0 + m],
                                                                     rhs=xT[:, c, :], start=(c == 0), stop=(c == 7)),
                               reads=[r_w, r_xT[c]], writes=[rb])
                        return bk, rb

                    for m in range(3):
                        bk, rb = proj(m * 128, 128)
                        op("dve", lambda e, m=m, bk=bk: e.tensor_copy(out=cqf[:, m, :], in_=bk[:, :]),
                           reads=[rb], writes=[r_cqf[m]])
                        op("act", lambda e, m=m: e.activation(out=sq[:, m, :], in_=cqf[:, m, :], func=AF.Square),
                           reads=[r_cqf[m]], writes=[r_sq[m]])
                    chk("A1b")
                    for g, (chs, n) in enumerate((((0, 1), 256.0), ((2,), 128.0))):
                        bk, rb = bank()
                        for i, m in enumerate(chs):
                            op("pe", lambda e, m=m, bk=bk, i=i, chs=chs: e.matmul(
                                bk[:, :], lhsT=ones[:], rhs=sq[:, m, :], start=(i == 0), stop=(i == len(chs) - 1)),
                               reads=[r_sq[m], r_const], writes=[rb])
                        op("dve", lambda e, bk=bk, n=n: e.tensor_scalar(out=tmp1[:], in0=bk[:, :], scalar1=1.0 / n,
                                                                        scalar2=EPS, op0=ALU.mult, op1=ALU.add),
                           reads=[rb], writes=[r_tmp1])
                        op("act", lambda e: e.activation(out=tmp2[:], in_=tmp1[:], func=AF.Sqrt),
                           reads=[r_tmp1], writes=[r_tmp2])
                        op("dve", lambda e, g=g: e.reciprocal(out=rstd[:, g, :], in_=tmp2[:]),
                           reads=[r_tmp2], writes=[r_rstd[g]])
                        chk("A1c")
                        for m in chs:
                            op("pool", lambda e, m=m, g=g: e.tensor_tensor(out=cqn[:, m, :], in0=cqf[:, m, :],
                                                                           in1=rstd[:, g, :], op=ALU.mult),
                               reads=[r_cqf[m], r_rstd[g]], writes=[r_cqn[m]])

                    chk("A2")

                    def rope_comb(bkP, rbP, bkR, rbR, nrow, outap, rout):
                        tA, rA, tB, rB = rtmp[rtk["k"] % 2]
                        rtk["k"] += 1
                        op("dve", lambda e: e.tensor_tensor(out=tA[0:nrow, :], in0=bkP[0:nrow, :],
                                                            in1=rope_t[b][0:nrow, 0, :], op=ALU.mult),
                           reads=[rbP, r_rope[b]], writes=[rA])
                        op("dve", lambda e: e.tensor_tensor(out=tB[0:nrow, :], in0=bkR[0:nrow, :],
                                                            in1=rope_t[b][0:nrow, 1, :], op=ALU.mult),
                           reads=[rbR, r_rope[b]], writes=[rB])
                        op("pool", lambda e: e.tensor_tensor(out=outap, in0=tA[0:nrow, :], in1=tB[0:nrow, :],
                                                             op=ALU.add),
                           reads=[rA, rB], writes=[rout])

                    bkP, rbP = proj(384, 32)
                    bkR, rbR = proj(416, 32)
                    rope_comb(bkP, rbP, bkR, rbR, 32, o_kp[:, :], r_kp)

                    chk("A3")
                    for m in range(4):
                        bk, rb = proj(448 + m * 128, 128)
                        evac(o_mq[:, m, :], bk[:, :], [rb], [r_mq[m]])
                    for m in range(4):
                        bk, rb = proj(960 + m * 128, 128)
                        op("dve", lambda e, m=m, bk=bk: e.tensor_copy(out=o_mk[:, m, :], in_=bk[:, :]),
                           reads=[rb], writes=[r_mk[m]])
                        op("dve", lambda e, m=m, bk=bk: e.tensor_reduce(
                            out=kmsum[:, m, 2 * t:2 * t + 2], in_=bk[:, :].rearrange("p (b l) -> p b l", b=2),
                            op=ALU.add, axis=AX.X), reads=[rb], writes=[r_km])
                    for hp in range(2):
                        op("dve", lambda e, hp=hp: e.tensor_scalar(
                            out=kmT[64 * hp:64 * hp + 64, :, 16 * hp + 2 * t:16 * hp + 2 * t + 2],
                            in0=kmsum[64 * hp:64 * hp + 64, :, 2 * t:2 * t + 2],
                            scalar1=1.0 / 256.0, scalar2=None, op0=ALU.mult), reads=[r_km], writes=[r_km])
                    for a in range(4):
                        bk, rb = bank()
                        for c in range(8):
                            op("pe", lambda e, c=c, a=a, bk=bk: e.matmul(
                                bk[:, :], lhsT=xT[:, c, a * 128:(a + 1) * 128], rhs=win[:, c, 1472:1984],
                                start=(c == 0), stop=(c == 7)), reads=[r_w, r_xT[c]], writes=[rb])
                        evac(o_mv[:, a, :], bk[:, :], [rb], [r_mv[a]])

                    chk("A4")
                    for m in range(4):
                        bk, rb = bank()
                        for kc in range(2):
                            op("pe", lambda e, kc=kc, m=m, bk=bk: e.matmul(
                                bk[:, :], lhsT=wuq[:, kc, m * 128:(m + 1) * 128], rhs=cqn[:, kc, :],
                                start=(kc == 0), stop=(kc == 1)), reads=[r_w, r_cqn[kc]], writes=[rb])
                        evac(o_qn[:, m, :], bk[:, :], [rb], [r_qn[m]])
                    for m in range(2):
                        pr = []
                        for off in (512, 768):
                            bk, rb = bank()
                            for kc in range(2):
                                op("pe", lambda e, kc=kc, m=m, bk=bk, off=off: e.matmul(
                                    bk[:, :], lhsT=wuq[:, kc, off + m * 128:off + (m + 1) * 128], rhs=cqn[:, kc, :],
                                    start=(kc == 0), stop=(kc == 1)), reads=[r_w, r_cqn[kc]], writes=[rb])
                            pr.append((bk, rb))
                        rope_comb(pr[0][0], pr[0][1], pr[1][0], pr[1][1], 128, o_qp[:, m, :], r_qp[m])
                    for m in range(4):
                        bk, rb = bank()
                        op("pe", lambda e, m=m, bk=bk: e.matmul(bk[:, :], lhsT=wukv[:, m * 128:(m + 1) * 128],
                                                                rhs=cqn[:, 2, :], start=True, stop=True),
                           reads=[r_w, r_cqn[2]], writes=[rb])
                        evac(o_kn[:, m, :], bk[:, :], [rb], [r_kn[m]])
                    for a in range(4):
                        bk, rb = bank()
                        op("pe", lambda e, a=a, bk=bk: e.matmul(bk[:, :], lhsT=cqn[:, 2, a * 128:(a + 1) * 128],
                                                                rhs=wukv[:, 512:1024], start=True, stop=True),
                           reads=[r_w, r_cqn[2]], writes=[rb])
                        evac(o_va[:, a, :], bk[:, :], [rb], [r_va[a]])

                    chk("A5")
                    bkT, rbT = bank()
                    for a in range(4):
                        own = 2 * t + a // 2
                        bk, rb = bank()
                        for m in range(4):
                            op("pe", lambda e, m=m, a=a, bk=bk: e.matmul(
                                bk[:, 32 * m:32 * m + 32], lhsT=o_mq[:, m, a * 128:(a + 1) * 128],
                                rhs=kmT[:, m, :], start=True, stop=True),
                               reads=[r_mq[m], r_km], writes=[rb])
                        op("dve", lambda e, bk=bk, own=own: e.tensor_tensor(
                            out=gm[:], in0=bk[:, 0:128].rearrange("p (h n) -> p h n", h=8),
                            in1=pastm[:, own:own + 1, :].broadcast_to([128, 8, 16]), op=ALU.add),
                           reads=[rb, r_pm], writes=[r_gm])
                        gm2 = seln[:].rearrange("p (h n) -> p h n", h=8)
                        op("dve", lambda e: e.tensor_copy(out=gm2, in_=gm[:]), reads=[r_gm], writes=[r_seln])
                        for rnd in range(3):
                            op("dve", lambda e: e.tensor_reduce(out=m8[:, :, 0:1], in_=gm2, op=ALU.max, axis=AX.X),
                               reads=[r_seln], writes=[r_m8])
                            op("dve", lambda e: e.tensor_tensor(out=m8[:, :, 1:2].broadcast_to([128, 8, 16]) if False else tmpg[:],
                                                                in0=gm2, in1=m8[:, :, 0:1].broadcast_to([128, 8, 16]),
                                                                op=ALU.is_ge), reads=[r_seln, r_m8], writes=[r_tmpg])
                            op("dve", lambda e: e.scalar_tensor_tensor(out=gm2, in0=tmpg[:], scalar=-2e30, in1=gm2,
                                                                       op0=ALU.mult, op1=ALU.add),
                               reads=[r_tmpg, r_seln], writes=[r_seln])
                        op("dve", lambda e: e.tensor_reduce(out=m8[:, :, 3:4], in_=gm2, op=ALU.max, axis=AX.X),
                           reads=[r_seln], writes=[r_m8])
                        op("dve", lambda e: e.tensor_tensor(
                            out=seln[:].rearrange("p (h n) -> p h n", h=8), in0=gm[:],
                            in1=m8[:, :, 3:4].broadcast_to([128, 8, 16]), op=ALU.is_lt),
                           reads=[r_gm, r_m8], writes=[r_seln])
                        op("pe", lambda e, a=a: e.transpose(out=bkT[:, a * 128:(a + 1) * 128], in_=seln[:],
                                                            identity=ident[:]),
                           reads=[r_seln, r_const], writes=[rbT])
                    evac(o_ms[:, :], bkT[:, :], [rbT], [r_ms])

                    chk("A6")
                    def store(dst, src, rsrc):
                        SC.dma(dst, src, d_stA, reads=(rsrc if isinstance(rsrc, list) else [rsrc]),
                               writes=[r_scrA], live=True)
                    store(sc["qn"][:, :, tsl].rearrange("m p t -> p m t"), o_qn[:], r_qn)
                    store(sc["qp"][:, :, tsl].rearrange("m p t -> p m t"), o_qp[:], r_qp)
                    store(sc["kn"][:, :, tsl].rearrange("m p t -> p m t"), o_kn[:], r_kn)
                    store(sc["kp"][:, tsl], o_kp[:], r_kp)
                    store(sc["va"][tsl, :].rearrange("(a p) c -> p a c", p=128), o_va[:], r_va)
                    store(sc["mq"][:, :, tsl].rearrange("m p t -> p m t"), o_mq[:], r_mq)
                    store(sc["mk"][:, :, tsl].rearrange("m p t -> p m t"), o_mk[:], r_mk)
                    store(sc["mv"][tsl, :].rearrange("(a p) c -> p a c", p=128), o_mv[:], r_mv)
                    store(sc["ms"][:, tsl], o_ms[:], r_ms)
                SC.flush()

            if stop == "A":
                return nc
            with ExitStack() as st:
                QT = [sb(st, f"QT{i}", [128, S], BF16) for i in range(2)]
                KT = [sb(st, f"KT{i}", [128, S], BF16) for i in range(2)]
                VA = [sb(st, f"VA{i}", [128, NKT, 128], BF16) for i in range(2)]
                r_QT = [Res(), Res()]
                r_KT = [Res(), Res()]
                r_VA = [Res(), Res()]
                d_QT = [SC.dsem(), SC.dsem()]
                d_KT = [SC.dsem(), SC.dsem()]
                d_VA = [SC.dsem(), SC.dsem()]
                PT = [sb(st, f"PT{i}", [128, 512], BF16) for i in range(4)]
                r_PT = [Res() for _ in range(4)]
                aT = [sb(st, f"aT{i}", [128, S], BF16) for i in range(2)]
                r_aT = [Res(), Res()]
                rd = sb(st, "rd", [128, 512], F32)
                r_rd = Res()
                cmask = sb(st, "cmask", [128, 4, 512], BF16)
                abias = sb(st, "abias", [128, 8, 35], F32)
                r_cB = Res()
                d_cB = SC.dsem()
                SC.dma(cmask[:].rearrange("p a n -> p (a n)"), c_cmask, d_cB, writes=[r_cB])
                SC.dma(abias[:].rearrange("p a n -> p (a n)"), c_abias, d_cB, writes=[r_cB], nodeps=True)
                op("pool", lambda e: e.memset(VA[0][:, :, 64:128], 1.0), writes=[r_VA[0]])
                op("pool", lambda e: e.memset(VA[1][:, :, 0:64], 1.0), writes=[r_VA[1]])

                sbank = [(banks[i], bres[i]) for i in range(4)]
                obank = [(banks[4 + i], bres[4 + i]) for i in range(2)]

                def load_head(hh):
                    b = hh % 2
                    h = hh % 8
                    if hh < 8:
                        R = 96
                        SC.dma(QT[b][0:64, :], sc["qn"][h // 2, 64 * (h % 2):64 * (h % 2) + 64, :], d_QT[b],
                               reads=[r_scrA], writes=[r_QT[b]])
                        SC.dma(QT[b][64:96, :], sc["qp"][h // 4, 32 * (h % 4):32 * (h % 4) + 32, :], d_QT[b],
                               reads=[r_scrA], writes=[r_QT[b]], nodeps=True)
                        SC.dma(KT[b][0:64, :], sc["kn"][h // 2, 64 * (h % 2):64 * (h % 2) + 64, :], d_KT[b],
                               reads=[r_scrA], writes=[r_KT[b]])
                        SC.dma(KT[b][64:96, :], sc["kp"][:, :], d_KT[b], reads=[r_scrA], writes=[r_KT[b]], nodeps=True)
                        vsrc = sc["va"]
                    else:
                        SC.dma(QT[b][0:64, :], sc["mq"][h // 2, 64 * (h % 2):64 * (h % 2) + 64, :], d_QT[b],
                               reads=[r_scrA], writes=[r_QT[b]])
                        SC.dma(QT[b][64:66, :], c_qalibi[h], d_QT[b], writes=[r_QT[b]], nodeps=True)
                        SC.dma(QT[b][66:82, :], sc["ms"][16 * h:16 * h + 16, :], d_QT[b], writes=[r_QT[b]], nodeps=True)
                        SC.dma(KT[b][0:64, :], sc["mk"][h // 2, 64 * (h % 2):64 * (h % 2) + 64, :], d_KT[b],
                               reads=[r_scrA], writes=[r_KT[b]])
                        SC.dma(KT[b][64:82, :], c_kconst, d_KT[b], writes=[r_KT[b]], nodeps=True)
                        vsrc = sc["mv"]
                    c0 = 0 if b == 0 else 64
                    for k0 in range(0, NKT, 8):
                        SC.dma(VA[b][:, k0:k0 + 8, c0:c0 + 64],
                               vsrc[k0 * 128:(k0 + 8) * 128, 64 * h:64 * h + 64].rearrange("(k p) d -> p k d", p=128),
                               d_VA[b], reads=[r_scrA], writes=[r_VA[b]], nodeps=(k0 > 0))

                steps = []
                for hh in range(16):
                    for j in range(NT):
                        nk = 4 * j + 4
                        for kt in range(nk):
                            steps.append((hh, j, kt, kt == 0, kt == nk - 1))

                def emit_score(i):
                    hh, j, kt, first, last = steps[i]
                    b = hh % 2
                    R = 96 if hh < 8 else 82
                    r = kt - 4 * j
                    q0 = 128 * r if r > 0 else 0
                    bk, rb = sbank[i % 4]
                    diag = r >= 0
                    op("pe", lambda e: e.matmul(bk[:, q0:512], lhsT=KT[b][0:R, kt * 128:(kt + 1) * 128],
                                                rhs=QT[b][0:R, j * 512 + q0:(j + 1) * 512], start=True, stop=not diag),
                       reads=[r_KT[b], r_QT[b]], writes=[rb])
                    if diag:
                        op("pe", lambda e: e.matmul(bk[:, q0:512], lhsT=identb[:], rhs=cmask[:, r, q0:512],
                                                    start=False, stop=True), reads=[r_cB, r_const], writes=[rb])

                def emit_rest(i):
                    hh, j, kt, first, last = steps[i]
                    b = hh % 2
                    h = hh % 8
                    r = kt - 4 * j
                    q0 = 128 * r if r > 0 else 0
                    bk, rb = sbank[i % 4]
                    pt, rpt = PT[i % 4], r_PT[i % 4]
                    ob, rob = obank[(hh * NT + j) % 2]
                    if hh < 8:
                        op("act", lambda e: e.activation(out=pt[:, q0:512], in_=bk[:, q0:512], func=AF.Exp,
                                                         scale=SC_MLA), reads=[rb], writes=[rpt])
                    else:
                        di = (512 * j - 128 * kt + 384) // 128
                        op("act", lambda e: e.activation(out=pt[:, q0:512], in_=bk[:, q0:512], func=AF.Exp,
                                                         scale=SC_MOBA, bias=abias[:, h, di:di + 1]),
                           reads=[rb, r_cB], writes=[rpt])
                    op("pe", lambda e: e.matmul(ob[:, q0:512], lhsT=VA[b][:, kt, :], rhs=pt[:, q0:512],
                                                start=first, stop=last), reads=[r_VA[b], rpt], writes=[rob])
                    if last:
                        u0, d0 = (0, 64) if b == 0 else (64, 0)
                        pair = hh // 2
                        ab = pair % 2
                        op("dve", lambda e: e.reciprocal(out=rd[u0:u0 + 64, :], in_=ob[d0:d0 + 64, :]),
                           reads=[rob], writes=[r_rd])
                        op("dve", lambda e: e.tensor_tensor(out=aT[ab][u0:u0 + 64, j * 512:(j + 1) * 512],
                                                            in0=ob[u0:u0 + 64, :], in1=rd[u0:u0 + 64, :], op=ALU.mult),
                           reads=[rob, r_rd], writes=[r_aT[ab]])
                        if j == NT - 1 and b == 1:
                            SC.dma(sc["at"][pair], aT[ab][:], d_stB, reads=[r_aT[ab]], writes=[r_scrB], live=True)

                load_head(0)
                LOOK = 3
                nst = len(steps)
                for i in range(min(LOOK, nst)):
                    emit_score(i)
                for i in range(nst):
                    hh, j, kt, first, last = steps[i]
                    if first and j == 0 and hh + 1 < 16:
                        load_head(hh + 1)
                    if i + LOOK < nst:
                        emit_score(i + LOOK)
                    emit_rest(i)
                SC.flush()

            if stop == "B":
                return nc
            with ExitStack() as st:
                wo = sb(st, "wo", [128, 8, 1024], BF16)
                wdn = sb(st, "wdn", [128, NPAIR, 1024], BF16)
                lnt = sb(st, "lnt", [128, 4, 1024], F32)
                cvp = sb(st, "cvp", [128, 44, 4], F32)
                r_cw = Res()
                d_cw = SC.dsem()
                SC.dma(wo[:].rearrange("p a n -> p (a n)"), wo_s, d_cw, reads=[r_wscr], writes=[r_cw])
                SC.dma(wdn[:].rearrange("p a n -> p (a n)"), wdn_s, d_cw, writes=[r_cw], nodeps=True)
                SC.dma(lnt[:].rearrange("p a n -> p (a n)"), lnp, d_cw, writes=[r_cw], nodeps=True)
                SC.dma(cvp[:].rearrange("p a n -> p (a n)"), convp, d_cw, writes=[r_cw], nodeps=True)
                aTt = sb(st, "aTt", [128, 8, 512], BF16)
                r_aTt = Res()
                d_aTt = SC.dsem()
                xr = [sb(st, f"xr{i}", [128, D], F32) for i in range(2)]
                r_xr = [Res(), Res()]
                d_xr = [SC.dsem(), SC.dsem()]
                ybs = [sb(st, f"yb{i}", [128, D], F32) for i in range(2)]
                r_ybs = [Res(), Res()]
                lnk = {"k": 0}
                x1f = sb(st, "x1f", [128, 4, D], F32)
                r_x1f = [Res() for _ in range(4)]
                x1T = sb(st, "x1T", [128, 8, 512], BF16)
                r_x1T = [Res() for _ in range(8)]
                statss = [sb(st, f"stats{i}", [128, 2, 6], F32) for i in range(2)]
                mvs = [sb(st, f"mv_{i}", [128, 2], F32) for i in range(2)]
                lnss = [sb(st, f"lns{i}", [128, 4], F32) for i in range(2)]
                r_lns = [Res(), Res()]
                wupt = [sb(st, f"wupt{i}", [128, 8, 256], BF16) for i in range(3)]
                r_wupt = [Res() for _ in range(3)]
                d_wupt = [SC.dsem() for _ in range(3)]
                hraw = [sb(st, f"hraw{i}", [128, 514], F32) for i in range(4)]
                r_hraw = [Res() for _ in range(4)]
                cacc = [sb(st, f"cacc{i}", [128, 512], F32) for i in range(4)]
                r_cacc = [Res() for _ in range(4)]
                sgs = [sb(st, f"sg{i}", [128, 512], F32) for i in range(2)]
                r_sgs = [Res(), Res()]
                carry = sb(st, "carry", [128, 44, 2], F32)
                r_carry = Res()
                actT = sb(st, "actT", [128, NPAIR, 512], BF16)
                r_actT = [Res() for _ in range(NPAIR)]
                ot = [sb(st, f"ot{i}", [128, D], F32) for i in range(2)]
                r_ot = [Res(), Res()]
                op("pool", lambda e: e.memset(carry[:], 0.0), writes=[r_carry])

                def layer_norm(k, gi, dst, rdst):
                    src, rsrc = ybs[k], r_ybs[k]
                    stats, mv_, lns, r_ln = statss[k], mvs[k], lnss[k], r_lns[k]
                    for hf in range(2):
                        op("dve", lambda e, hf=hf: e.bn_stats(out=stats[:, hf, :], in_=src[:, hf * 512:(hf + 1) * 512]),
                           reads=[rsrc], writes=[r_ln])
                    op("dve", lambda e: e.bn_aggr(out=mv_[:], in_=stats[:].rearrange("p a n -> p (a n)")),
                       reads=[r_ln], writes=[r_ln])
                    op("dve", lambda e: e.tensor_scalar(out=lns[:, 0:1], in0=mv_[:, 1:2], scalar1=EPS, scalar2=None,
                                                        op0=ALU.add), reads=[r_ln], writes=[r_ln])
                    op("act", lambda e: e.activation(out=lns[:, 1:2], in_=lns[:, 0:1], func=AF.Sqrt),
                       reads=[r_ln], writes=[r_ln])
                    op("dve", lambda e: e.reciprocal(out=lns[:, 2:3], in_=lns[:, 1:2]), reads=[r_ln], writes=[r_ln])
                    op("dve", lambda e: e.tensor_scalar(out=lns[:, 3:4], in0=mv_[:, 0:1], scalar1=-1.0,
                                                        scalar2=lns[:, 2:3], op0=ALU.mult, op1=ALU.mult),
                       reads=[r_ln], writes=[r_ln])
                    op("act", lambda e: e.activation(out=src[:], in_=src[:], func=AF.Identity, scale=lns[:, 2:3],
                                                     bias=lns[:, 3:4]), reads=[rsrc, r_ln], writes=[rsrc])
                    op("pool", lambda e: e.tensor_tensor(out=src[:], in0=src[:], in1=lnt[:, gi, :], op=ALU.mult),
                       reads=[rsrc, r_cw], writes=[rsrc])
                    op("pool", lambda e: e.tensor_tensor(out=dst, in0=src[:], in1=lnt[:, gi + 1, :], op=ALU.add),
                       reads=[rsrc, r_cw], writes=[rdst])

                def load_x(t, a):
                    k = (t * 4 + a) % 2
                    SC.dma(xr[k][:], x[s, t * 512 + a * 128:t * 512 + (a + 1) * 128, :], d_xr[k], writes=[r_xr[k]])

                wk = {"k": 0}

                def load_wup(p):
                    k = wk["k"] % 3
                    wk["k"] += 1
                    SC.dma(wupt[k][:].rearrange("p a n -> p (a n)"), wup_s[p], d_wupt[k], reads=[r_wscr],
                           writes=[r_wupt[k]])
                    return k

                for t in range(NT):
                    tsl = slice(t * 512, (t + 1) * 512)
                    SC.dma(aTt[:], sc["at"][:, :, tsl].rearrange("c p t -> p c t"), d_aTt, reads=[r_scrB],
                           writes=[r_aTt])
                    load_x(t, 0)
                    wq = [load_wup(0), load_wup(1)]
                    for a in range(4):
                        k = (t * 4 + a) % 2
                        if a + 1 < 4:
                            load_x(t, a + 1)
                        bks = []
                        for n in range(2):
                            bk, rb = bank()
                            for c in range(8):
                                op("pe", lambda e, c=c, n=n, bk=bk: e.matmul(
                                    bk[:, :], lhsT=aTt[:, c, a * 128:(a + 1) * 128], rhs=wo[:, c, n * 512:(n + 1) * 512],
                                    start=(c == 0), stop=(c == 7)), reads=[r_aTt, r_cw], writes=[rb])
                            bks.append((bk, rb))
                        ky = lnk["k"] % 2
                        lnk["k"] += 1
                        for n in range(2):
                            bk, rb = bks[n]
                            op("dve", lambda e, n=n, bk=bk: e.scalar_tensor_tensor(
                                out=ybs[ky][:, n * 512:(n + 1) * 512], in0=xr[k][:, n * 512:(n + 1) * 512], scalar=ALPHA,
                                in1=bk[:, :], op0=ALU.mult, op1=ALU.add), reads=[r_xr[k], rb], writes=[r_ybs[ky]])
                        layer_norm(ky, 0, x1f[:, a, :], r_x1f[a])
                    for c in range(8):
                        bk, rb = bank()
                        for a in range(4):
                            op("pe", lambda e, a=a, c=c, bk=bk: e.transpose(
                                out=bk[:, a * 128:(a + 1) * 128], in_=x1f[:, a, c * 128:(c + 1) * 128],
                                identity=ident[:]), reads=[r_x1f[a], r_const], writes=[rb])
                        if c % 2 == 0:
                            op("act", lambda e, c=c, bk=bk: e.activation(out=x1T[:, c, :], in_=bk[:, :], func=AF.Copy),
                               reads=[rb], writes=[r_x1T[c]])
                        else:
                            op("dve", lambda e, c=c, bk=bk: e.tensor_copy(out=x1T[:, c, :], in_=bk[:, :]),
                               reads=[rb], writes=[r_x1T[c]])
                    for p in range(NPAIR):
                        kw = wq.pop(0)
                        if p + 2 < NPAIR:
                            wq.append(load_wup(p + 2))
                        for gu in range(2):
                            ch = p + 22 * gu
                            bk, rb = bank()
                            for c in range(8):
                                op("pe", lambda e, c=c, bk=bk, gu=gu: e.matmul(
                                    bk[:, :], lhsT=wupt[kw][:, c, gu * 128:(gu + 1) * 128], rhs=x1T[:, c, :],
                                    start=(c == 0), stop=(c == 7)), reads=[r_wupt[kw], r_x1T[c]], writes=[rb])
                            bi = 2 * (p % 2) + gu
                            hr, rhr = hraw[bi], r_hraw[bi]
                            ca, rca = cacc[bi], r_cacc[bi]
                            op("pool", lambda e, hr=hr, ch=ch: e.tensor_copy(out=hr[:, 0:2], in_=carry[:, ch, :]),
                               reads=[r_carry], writes=[rhr])
                            op("act", lambda e, hr=hr, bk=bk: e.activation(out=hr[:, 2:514], in_=bk[:, :], func=AF.Copy),
                               reads=[rb], writes=[rhr])
                            op("act", lambda e, ca=ca, bk=bk, ch=ch: e.activation(
                                out=ca[:], in_=bk[:, :], func=AF.Identity, scale=cvp[:, ch, 2:3], bias=cvp[:, ch, 3:4]),
                               reads=[rb, r_cw], writes=[rca])
                            op("pool", lambda e, hr=hr, ch=ch: e.tensor_copy(out=carry[:, ch, :], in_=hr[:, 512:514]),
                               reads=[rhr], writes=[r_carry])
                            op("dve", lambda e, ca=ca, hr=hr, ch=ch: e.scalar_tensor_tensor(
                                out=ca[:], in0=hr[:, 1:513], scalar=cvp[:, ch, 1:2], in1=ca[:],
                                op0=ALU.mult, op1=ALU.add), reads=[rhr, rca, r_cw], writes=[rca])
                            op("dve", lambda e, ca=ca, hr=hr, ch=ch: e.scalar_tensor_tensor(
                                out=ca[:], in0=hr[:, 0:512], scalar=cvp[:, ch, 0:1], in1=ca[:],
                                op0=ALU.mult, op1=ALU.add), reads=[rhr, rca, r_cw], writes=[rca])
                        bg, bu = 2 * (p % 2), 2 * (p % 2) + 1
                        sg, r_sg = sgs[p % 2], r_sgs[p % 2]
                        op("act", lambda e, sg=sg, bg=bg: e.activation(out=sg[:], in_=cacc[bg][:], func=AF.Silu),
                           reads=[r_cacc[bg]], writes=[r_sg])
                        op("pool", lambda e, p=p, sg=sg, bu=bu: e.tensor_tensor(out=actT[:, p, :], in0=sg[:],
                                                                                 in1=cacc[bu][:], op=ALU.mult),
                           reads=[r_sg, r_cacc[bu]], writes=[r_actT[p]])
                    for a in range(4):
                        ko = (t * 4 + a) % 2
                        bks = []
                        for n in range(2):
                            bk, rb = bank()
                            for p in range(NPAIR):
                                op("pe", lambda e, p=p, n=n, bk=bk: e.matmul(
                                    bk[:, :], lhsT=actT[:, p, a * 128:(a + 1) * 128], rhs=wdn[:, p, n * 512:(n + 1) * 512],
                                    start=(p == 0), stop=(p == NPAIR - 1)), reads=[r_actT[p], r_cw], writes=[rb])
                            bks.append((bk, rb))
                        ky = lnk["k"] % 2
                        lnk["k"] += 1
                        for n in range(2):
                            bk, rb = bks[n]
                            op("dve", lambda e, n=n, bk=bk: e.scalar_tensor_tensor(
                                out=ybs[ky][:, n * 512:(n + 1) * 512], in0=x1f[:, a, n * 512:(n + 1) * 512], scalar=ALPHA,
                                in1=bk[:, :], op0=ALU.mult, op1=ALU.add), reads=[r_x1f[a], rb], writes=[r_ybs[ky]])
                        layer_norm(ky, 2, ot[ko][:], r_ot[ko])
                        SC.dma(out[s, t * 512 + a * 128:t * 512 + (a + 1) * 128, :], ot[ko][:], d_out,
                               reads=[r_ot[ko]], writes=[r_out], live=True)
                SC.flush()

        SC._wait("sp", ("d", d_out, d_out.cnt))
        SC.flush()
    return nc


def _bf(a):
    return np.ascontiguousarray(a.astype(ml_dtypes.bfloat16))


def make_consts(S):
    NT = S // 512
    c = {}
    c["c_ident"] = np.eye(128, dtype=np.float32)
    c["c_identb"] = _bf(np.eye(128, dtype=np.float32))
    k = np.arange(128)[:, None, None]
    r = np.arange(4)[None, :, None]
    q = np.arange(512)[None, None, :]
    c["c_cmask"] = _bf(np.where(128 * r + k > q, NEGBIG, 0.0).astype(np.float32).reshape(128, 2048))
    half = 16
    inv = (np.float32(10000.0) ** (-np.arange(half, dtype=np.float32) / np.float32(half))).astype(np.float32)
    pos = np.arange(S, dtype=np.float32)
    ang = (pos[:, None] * inv[None, :]).astype(np.float32)
    cos = np.cos(ang).astype(np.float32)
    sin = np.sin(ang).astype(np.float32)
    d = np.arange(128) % 32
    cosT = cos[:, d % 16].T
    sinT = sin[:, d % 16].T * np.where(d < 16, -1.0, 1.0)[:, None]
    rope = np.stack([cosT.reshape(128, NT, 512), sinT.reshape(128, NT, 512)], axis=2)
    c["c_rope"] = np.ascontiguousarray(rope.transpose(1, 0, 2, 3).reshape(NT, 128, 1024).astype(np.float32))
    kc = np.zeros((18, S), np.float32)
    kc[0:2] = 1.0
    blk = np.arange(S) // 256
    for n in range(16):
        kc[2 + n] = np.where(blk == n, NEGBIG, 0.0)
    c["c_kconst"] = _bf(kc)
    slopes = (2.0 ** (-8.0 * np.arange(1, 9, dtype=np.float32) / 8.0)).astype(np.float32)
    dq = (np.arange(S) % 512).astype(np.float32)
    v = (-slopes[:, None] * dq[None, :] / np.float32(SC_MOBA)).astype(np.float32)
    hi = v.astype(ml_dtypes.bfloat16)
    lo = (v - hi.astype(np.float32)).astype(ml_dtypes.bfloat16)
    c["c_qalibi"] = np.ascontiguousarray(np.stack([hi, lo], axis=1))
    i = np.arange(35)
    Dd = (128 * i - 384).astype(np.float32)
    kk = np.arange(128, dtype=np.float32)
    ab = -slopes[None, :, None] * (Dd[None, None, :] - kk[:, None, None])
    c["c_abias"] = np.ascontiguousarray(ab.astype(np.float32).reshape(128, 8 * 35))
    own = np.arange(16)[:, None]
    n = np.arange(16)[None, :]
    pm = np.where(n < own, 0.0, np.where(n == own, 1e30, -1e30)).astype(np.float32)
    c["c_pastm"] = np.ascontiguousarray(np.broadcast_to(pm.reshape(1, 256), (128, 256)))
    return c


def prep_weights(w_in, q_norm_g, w_uq, kv_norm_g, w_ukv, w_o, ln1_g, ln1_b, w_up, conv_w, conv_b, w_down,
                 ln2_g, ln2_b):
    f = lambda a: np.ascontiguousarray(np.asarray(a, dtype=np.float32))
    w_in, w_uq, w_ukv, w_o, w_up, w_down = (f(a[0]) for a in (w_in, w_uq, w_ukv, w_o, w_up, w_down))
    r = np.arange(32)
    kr_sw = w_in[:, 384 + ((r + 16) % 32)]
    win = np.concatenate([w_in[:, 0:416], kr_sw, w_in[:, 416:1952]], axis=1)
    win = win.reshape(8, 128, WIN_COLS).transpose(1, 0, 2).reshape(128, 8 * WIN_COLS)
    h = np.arange(8)
    nope = (h[:, None] * 96 + np.arange(64)[None, :]).reshape(-1)
    pe = (h[:, None] * 96 + 64 + r[None, :]).reshape(-1)
    pesw = (h[:, None] * 96 + 64 + ((r + 16) % 32)[None, :]).reshape(-1)
    wuq = w_uq[:, np.concatenate([nope, pe, pesw])]
    wuq = wuq.reshape(2, 128, 1024).transpose(1, 0, 2).reshape(128, 2048)
    kcols = (h[:, None] * 128 + np.arange(64)[None, :]).reshape(-1)
    vcols = kcols + 64
    wukv = w_ukv[:, np.concatenate([kcols, vcols])]
    wo = w_o.reshape(8, 128, 1024).transpose(1, 0, 2).reshape(128, 8192)
    wdn = w_down.reshape(NPAIR, 128, 1024).transpose(1, 0, 2).reshape(128, NPAIR * 1024)
    wu = w_up.reshape(8, 128, 2, NPAIR, 128)
    wu = wu.transpose(3, 1, 0, 2, 4).reshape(NPAIR, 128, 8 * 256)
    lnp = np.stack([f(ln1_g[0]), f(ln1_b[0]), f(ln2_g[0]), f(ln2_b[0])], axis=0).reshape(1, 4096)
    lnp = np.broadcast_to(lnp, (128, 4096))
    cw = f(conv_w[0])
    cb = f(conv_b[0])
    cv = np.concatenate([cw, cb[None, :]], axis=0)
    cv = cv.reshape(4, 44, 128).transpose(2, 1, 0).reshape(128, 176)
    return {
        "w_in": f(win), "w_uq": f(wuq), "w_ukv": f(wukv), "w_o": f(wo), "w_dn": f(wdn), "w_up": f(wu),
        "qg": f(f(q_norm_g[0]).reshape(2, 128).T), "kvg": f(f(kv_norm_g[0]).reshape(128, 1)),
        "lnp": f(lnp), "convp": f(cv),
    }


_NC_CACHE = {}


def run(x, params, ncores, debug=False, stop=None):
    B, S, _ = x.shape
    nseq = B // ncores
    key = (nseq, S, debug)
    if key not in _NC_CACHE:
        _NC_CACHE[key] = build(nseq, S, debug, stop)
    nc = _NC_CACHE[key]
    shared = dict(prep_weights(**params))
    shared.update(make_consts(S))
    xs = np.ascontiguousarray(np.asarray(x, dtype=np.float32)).reshape(ncores, nseq, S, D)
    in_maps = [dict(shared, x=xs[i]) for i in range(ncores)]
    res = run_bass_kernel_spmd(nc, in_maps, core_ids=list(range(ncores)))
    return res


def kernel(x, w_in, q_norm_g, w_uq, kv_norm_g, w_ukv, w_o, ln1_g, ln1_b, w_up, conv_w, conv_b, w_down,
           ln2_g, ln2_b):
    params = dict(w_in=w_in, q_norm_g=q_norm_g, w_uq=w_uq, kv_norm_g=kv_norm_g, w_ukv=w_ukv, w_o=w_o,
                  ln1_g=ln1_g, ln1_b=ln1_b, w_up=w_up, conv_w=conv_w, conv_b=conv_b, w_down=w_down,
                  ln2_g=ln2_g, ln2_b=ln2_b)
    x = np.asarray(x)
    res = run(x, params, NCORES)
    outs = [np.asarray(r["out"]) for r in res.results]
    return np.concatenate(outs, axis=0).astype(np.float32)
```

```python
import math
from contextlib import ExitStack

import numpy as np
import ml_dtypes
import concourse.bass as bass
import concourse.mybir as mybir
from concourse.bass_utils import run_bass_kernel_spmd

F32 = mybir.dt.float32
BF16 = mybir.dt.bfloat16
AF = mybir.ActivationFunctionType
ALU = mybir.AluOpType
AX = mybir.AxisListType

D = 1024
NCORES = 8
EPS = 1e-5
NEGBIG = -30000.0
ALPHA = 2.0 ** 0.25
SC_MLA = 96.0 ** -0.5
SC_MOBA = 0.125
DFF = 2816
NPAIR = 22
WIN_COLS = 1984
import os as _os
ALL_INC = _os.environ.get("ALL_INC", "0") == "1"


class Res:
    __slots__ = ("w", "r", "scratch")

    def __init__(self, scratch=False):
        self.w = None
        self.r = []
        self.scratch = scratch


class DSem:
    def __init__(self, sem):
        self.sem = sem
        self.cnt = 0


class _Rec:
    def __getattr__(self, name):
        def f(*a, **k):
            self.call = (name, a, k)
        return f


class Sched:
    ENGS = ("pe", "act", "dve", "pool", "sp")
    CENGS = ("pe", "act", "dve", "pool")

    def __init__(self, nc, stack):
        self.nc = nc
        self.stack = stack
        self.prog = {e: [] for e in self.ENGS}
        self.sem = {}
        self.cnt = {}
        self.targets = {e: set() for e in self.CENGS}
        for e in self.CENGS:
            self.sem[e] = stack.enter_context(nc.semaphore("s_" + e))
            self.cnt[e] = 0
        self.seen = {e: {} for e in self.ENGS}
        self.dsems = []
        self.rank = {e: {} for e in self.CENGS}
        self.flushed = {e: 0 for e in self.CENGS}

    def dsem(self):
        s = self.stack.enter_context(self.nc.semaphore(f"d{len(self.dsems)}"))
        d = DSem(s)
        self.dsems.append(d)
        return d

    def _wait(self, eng, ev):
        if ev is None:
            return
        kind, obj, val = ev
        if kind == "e":
            if obj == eng and eng == "pe":
                return
            key = obj
        else:
            key = id(obj)
            if val is None:
                val = obj.cnt
        if val <= 0 or self.seen[eng].get(key, 0) >= val:
            return
        self.seen[eng][key] = val
        if kind == "e":
            assert val > self.flushed[obj] or val in self.rank[obj], (eng, obj, val)
            self.targets[obj].add(val)
        self.prog[eng].append(("w", kind, obj, val))

    def _deps(self, eng, reads, writes):
        for r in reads:
            self._wait(eng, r.w)
        for w in writes:
            if w.scratch:
                continue
            self._wait(eng, w.w)
            for ev in w.r:
                self._wait(eng, ev)

    def _commit(self, ev, reads, writes):
        for r in reads:
            if not r.scratch:
                r.r.append(ev)
        for w in writes:
            w.w = ev
            w.r = []

    def op(self, eng, fn, reads=(), writes=()):
        self._deps(eng, reads, writes)
        self.cnt[eng] += 1
        ev = ("e", eng, self.cnt[eng])
        rec = _Rec()
        fn(rec)
        if ALL_INC:
            self.targets[eng].add(self.cnt[eng])
        self.prog[eng].append(("i", rec.call, self.cnt[eng]))
        self._commit(ev, reads, writes)
        return ev

    def dma(self, out, in_, ds, reads=(), writes=(), live=False, nodeps=False, q="sp"):
        if not nodeps:
            self._deps(q, reads, writes)
        ds.cnt += 16
        assert ds.cnt < 65000
        ev = ("d", ds, None if live else ds.cnt)
        self.prog[q].append(("d", out, in_, ds.sem))
        self._commit(ev, reads, writes)
        return ev

    def barrier(self):
        for e in self.ENGS:
            for o in self.CENGS:
                self._wait(e, ("e", o, self.cnt[o]))
            for d in self.dsems:
                self._wait(e, ("d", d, d.cnt))

    def flush(self):
        self.barrier()
        rank = self.rank
        for e in self.CENGS:
            new = sorted(v for v in self.targets[e] if v not in rank[e])
            base = len(rank[e])
            for i, v in enumerate(new):
                assert v > self.flushed[e]
                rank[e][v] = base + i + 1
            assert len(rank[e]) < 65000, (e, len(rank[e]))

        def replay(name):
            def body(eng):
                for it in self.prog[name]:
                    if it[0] == "w":
                        _, kind, obj, val = it
                        if kind == "e":
                            eng.wait_ge(self.sem[obj], rank[obj][val])
                        else:
                            eng.wait_ge(obj.sem, val)
                    elif it[0] == "i":
                        nm, a, k = it[1]
                        ins = getattr(eng, nm)(*a, **k)
                        if it[2] in rank[name]:
                            ins.then_inc(self.sem[name], 1)
                    else:
                        eng.dma_start(out=it[1], in_=it[2]).then_inc(it[3], 16)
            return body

        with self.nc.Block() as block:
            block.tensor(replay("pe"))
            block.scalar(replay("act"))
            block.vector(replay("dve"))
            block.gpsimd(replay("pool"))
            block.sync(replay("sp"))
        for e in self.ENGS:
            self.prog[e] = []
        for e in self.CENGS:
            self.flushed[e] = self.cnt[e]


class _Stop(Exception):
    pass


def build(NSEQ, S, debug=False, stop=None):
    st_ = {}
    try:
        return _build(NSEQ, S, debug, stop, st_)
    except _Stop:
        return st_["nc"]


def _build(NSEQ, S, debug, stop, st_):
    assert S % 512 == 0
    NT = S // 512
    NKT = S // 128
    nc = bass.Bass("TRN2", target_bir_lowering=False)
    st_["nc"] = nc

    def din(name, shape, dt=F32):
        return nc.dram_tensor(name, list(shape), dt, kind="ExternalInput").ap()

    def dscr(name, shape, dt=BF16):
        kind = "ExternalOutput" if debug else "Internal"
        return nc.dram_tensor(name, list(shape), dt, kind=kind).ap()

    x = din("x", [NSEQ, S, D])
    out = nc.dram_tensor("out", [NSEQ, S, D], F32, kind="ExternalOutput").ap()
    w_in = din("w_in", [128, 8 * WIN_COLS])
    w_uq = din("w_uq", [128, 2 * 1024])
    w_ukv = din("w_ukv", [128, 1024])
    qg = din("qg", [128, 2])
    kvg = din("kvg", [128, 1])
    w_o = din("w_o", [128, 8 * 1024])
    w_dn = din("w_dn", [128, NPAIR * 1024])
    w_up = din("w_up", [NPAIR, 128, 2048])
    lnp = din("lnp", [128, 4 * 1024])
    convp = din("convp", [128, 44 * 4])
    c_ident = din("c_ident", [128, 128])
    c_identb = din("c_identb", [128, 128], BF16)
    c_cmask = din("c_cmask", [128, 4 * 512], BF16)
    c_rope = din("c_rope", [NT, 128, 2 * 512])
    c_kconst = din("c_kconst", [18, S], BF16)
    c_qalibi = din("c_qalibi", [8, 2, S], BF16)
    c_abias = din("c_abias", [128, 8 * 35])
    c_pastm = din("c_pastm", [128, 16 * 16])

    wo_s = dscr("wo_s", [128, 8 * 1024])
    wdn_s = dscr("wdn_s", [128, NPAIR * 1024])
    wup_s = dscr("wup_s", [NPAIR, 128, 2048])
    scr = []
    for s in range(NSEQ):
        scr.append(dict(
            qn=dscr(f"qn{s}", [4, 128, S]), qp=dscr(f"qp{s}", [2, 128, S]),
            kn=dscr(f"kn{s}", [4, 128, S]), kp=dscr(f"kp{s}", [32, S]),
            va=dscr(f"va{s}", [S, 512]),
            mq=dscr(f"mq{s}", [4, 128, S]), mk=dscr(f"mk{s}", [4, 128, S]),
            mv=dscr(f"mv{s}", [S, 512]), ms=dscr(f"ms{s}", [128, S]),
            at=dscr(f"at{s}", [8, 128, S]),
        ))

    with ExitStack() as top:
        SC = Sched(nc, top)
        op = SC.op
        uniq = {"n": 0}

        def sb(st, name, shape, dt):
            uniq["n"] += 1
            return st.enter_context(nc.sbuf_tensor(f"{name}_{uniq['n']}", list(shape), dt))

        banks = [top.enter_context(nc.psum_tensor(f"bank{i}", [128, 512], F32)) for i in range(8)]
        bres = [Res() for _ in range(8)]
        bstate = {"i": 0}

        def bank():
            i = bstate["i"]
            bstate["i"] = (i + 1) % 8
            return banks[i], bres[i]

        ident = sb(top, "ident", [128, 128], F32)
        identb = sb(top, "identb", [128, 128], BF16)
        ones = sb(top, "ones", [128, 128], BF16)
        r_const = Res()
        d_const = SC.dsem()
        SC.dma(ident[:], c_ident, d_const, writes=[r_const])
        SC.dma(identb[:], c_identb, d_const, writes=[r_const], nodeps=True)
        op("pool", lambda e: e.memset(ones[:], 1.0), writes=[r_const])

        d_wscr = SC.dsem()
        r_wscr = Res(scratch=True)

        def chk(tag):
            if stop == tag:
                SC.flush()
                raise _Stop()

        def make_stager(st, piece=2048):
            return dict(stgs=[sb(st, f"stg{i}", [128, piece], F32) for i in range(2)], rs=[Res(), Res()],
                        ds=[SC.dsem(), SC.dsem()], k=0, piece=piece)

        def cast_stream(sg_, src, ncols, dst_fn, piece=2048):
            for c0 in range(0, ncols, piece):
                c1 = min(ncols, c0 + piece)
                k = sg_["k"]
                sg_["k"] += 1
                b = k % 2
                SC.dma(sg_["stgs"][b][:, 0:c1 - c0], src[:, c0:c1], sg_["ds"][b], writes=[sg_["rs"][b]])
                dst_fn(c0, c1, sg_["stgs"][b][:, 0:c1 - c0], sg_["rs"][b], k)

        pro_jobs = []
        for c0 in range(0, 8 * 1024, 2048):
            pro_jobs.append((w_o[:, c0:c0 + 2048], wo_s[:, c0:c0 + 2048]))
        for c0 in range(0, NPAIR * 1024, 2048):
            pro_jobs.append((w_dn[:, c0:c0 + 2048], wdn_s[:, c0:c0 + 2048]))
        for p in range(NPAIR):
            pro_jobs.append((w_up[p], wup_s[p]))

        d_out = SC.dsem()
        r_out = Res(scratch=True)

        for s in range(NSEQ):
            sc = scr[s]
            d_stA = SC.dsem()
            r_scrA = Res(scratch=True)
            d_stB = SC.dsem()
            r_scrB = Res(scratch=True)

            with ExitStack() as st:
                win = sb(st, "win", [128, 8, WIN_COLS], BF16)
                wuq = sb(st, "wuq", [128, 2, 1024], BF16)
                wukv = sb(st, "wukv", [128, 1024], BF16)
                qg_t = sb(st, "qg_t", [128, 2], F32)
                kvg_t = sb(st, "kvg_t", [128, 1], F32)
                r_w = Res()
                d_w = SC.dsem()
                SC.dma(qg_t[:], qg, d_w, writes=[r_w])
                SC.dma(kvg_t[:], kvg, d_w, writes=[r_w], nodeps=True)
                with ExitStack() as st2:
                    winf = win[:].rearrange("p c n -> p (c n)")
                    stager2 = make_stager(st2)

                    def f_win(c0, c1, stg, rstg, k):
                        op("act" if k % 2 == 0 else "dve",
                           (lambda e: e.activation(out=winf[:, c0:c1], in_=stg, func=AF.Copy)) if k % 2 == 0 else
                           (lambda e: e.tensor_copy(out=winf[:, c0:c1], in_=stg)),
                           reads=[rstg], writes=[r_w])
                    cast_stream(stager2, w_in, 8 * WIN_COLS, f_win)

                    def f_wuq(c0, c1, stg, rstg, k):
                        kc = c0 // 1024
                        op("dve", lambda e: e.tensor_scalar(out=wuq[:, kc, :], in0=stg, scalar1=qg_t[:, kc:kc + 1],
                                                            scalar2=None, op0=ALU.mult),
                           reads=[rstg, r_w], writes=[r_w])
                    cast_stream(stager2, w_uq, 2048, f_wuq, piece=1024)

                    def f_wukv(c0, c1, stg, rstg, k):
                        op("dve", lambda e: e.tensor_scalar(out=wukv[:, :], in0=stg, scalar1=kvg_t[:, 0:1],
                                                            scalar2=None, op0=ALU.mult),
                           reads=[rstg, r_w], writes=[r_w])
                    cast_stream(stager2, w_ukv, 1024, f_wukv, piece=1024)
                    SC.flush()

                xt = [sb(st, f"xt{i}", [128, 4, D], F32) for i in range(2)]
                r_xt = [Res(), Res()]
                d_xt = [SC.dsem(), SC.dsem()]
                rope_t = [sb(st, f"rope{i}", [128, 2, 512], F32) for i in range(2)]
                r_rope = [Res(), Res()]
                d_rope = [SC.dsem(), SC.dsem()]
                xT = sb(st, "xT", [128, 8, 512], BF16)
                r_xT = [Res() for _ in range(8)]
                cqf = sb(st, "cqf", [128, 3, 512], F32)
                r_cqf = [Res() for _ in range(3)]
                sq = sb(st, "sq", [128, 3, 512], BF16)
                r_sq = [Res() for _ in range(3)]
                rstd = sb(st, "rstd", [128, 2, 512], F32)
                r_rstd = [Res(), Res()]
                cqn = sb(st, "cqn", [128, 3, 512], BF16)
                r_cqn = [Res() for _ in range(3)]
                tmp1 = sb(st, "tmp1", [128, 512], F32)
                r_tmp1 = Res()
                tmp2 = sb(st, "tmp2", [128, 512], F32)
                r_tmp2 = Res()
                rtmp = [(sb(st, f"rtA{i}", [128, 512], F32), Res(), sb(st, f"rtB{i}", [128, 512], F32), Res())
                        for i in range(2)]
                rtk = {"k": 0}
                pastm = sb(st, "pastm", [128, 16, 16], F32)
                kmsum = sb(st, "kmsum", [128, 4, 16], F32)
                kmT = sb(st, "kmT", [128, 4, 32], BF16)
                r_km = Res()
                gm = sb(st, "gm", [128, 8, 16], F32)
                r_gm = Res()
                m8 = sb(st, "m8", [128, 8, 8], F32)
                r_m8 = Res()
                seln = sb(st, "seln", [128, 128], F32)
                r_seln = Res()
                tmpg = sb(st, "tmpg", [128, 8, 16], F32)
                r_tmpg = Res()
                d_pm = SC.dsem()
                r_pm = Res()
                SC.dma(pastm[:].rearrange("p a b -> p (a b)"), c_pastm, d_pm, writes=[r_pm])
                op("pool", lambda e: e.memset(kmT[:], 0.0), writes=[r_km])
                op("pool", lambda e: e.memset(kmsum[:], 0.0), writes=[r_km])

                def obuf(name, shape, n=1):
                    return sb(st, name, shape, BF16), ([Res() for _ in range(n)] if n > 1 else Res())
                o_qn, r_qn = obuf("o_qn", [128, 4, 512], 4)
                o_qp, r_qp = obuf("o_qp", [128, 2, 512], 2)
                o_kn, r_kn = obuf("o_kn", [128, 4, 512], 4)
                o_kp, r_kp = obuf("o_kp", [32, 512])
                o_va, r_va = obuf("o_va", [128, 4, 512], 4)
                o_mq, r_mq = obuf("o_mq", [128, 4, 512], 4)
                o_mk, r_mk = obuf("o_mk", [128, 4, 512], 4)
                o_mv, r_mv = obuf("o_mv", [128, 4, 512], 4)
                o_ms, r_ms = obuf("o_ms", [128, 512])

                if s == 0:
                    pstg = [sb(st, f"pstg{i}", [128, 2048], F32) for i in range(2)]
                    pstb = [sb(st, f"pstb{i}", [128, 2048], BF16) for i in range(2)]
                    r_pstg = [Res(), Res()]
                    r_pstb = [Res(), Res()]
                    d_pstg = [SC.dsem(), SC.dsem()]
                    pjk = {"k": 0}
                    per_tile = -(-len(pro_jobs) // NT)

                    def pro_job():
                        k = pjk["k"]
                        if k >= len(pro_jobs):
                            return
                        pjk["k"] += 1
                        src, dst = pro_jobs[k]
                        b = k % 2
                        SC.dma(pstg[b][:], src, d_pstg[b], writes=[r_pstg[b]])
                        op("pool", lambda e: e.tensor_copy(out=pstb[b][:], in_=pstg[b][:]),
                           reads=[r_pstg[b]], writes=[r_pstb[b]])
                        SC.dma(dst, pstb[b][:], d_wscr, reads=[r_pstb[b]], writes=[r_wscr], live=True)

                evk = {"k": 0}

                def evac(outap, inap, reads, writes):
                    evk["k"] += 1
                    if evk["k"] % 2 == 0:
                        op("act", lambda e: e.activation(out=outap, in_=inap, func=AF.Copy), reads, writes)
                    else:
                        op("dve", lambda e: e.tensor_copy(out=outap, in_=inap), reads, writes)

                def load_tile(t):
                    b = t % 2
                    SC.dma(xt[b][:], x[s, t * 512:(t + 1) * 512, :].rearrange("(a p) d -> p a d", p=128),
                           d_xt[b], writes=[r_xt[b]])
                    SC.dma(rope_t[b][:].rearrange("p a n -> p (a n)"), c_rope[t], d_rope[b], writes=[r_rope[b]])

                load_tile(0)
                for t in range(NT):
                    b = t % 2
                    tsl = slice(t * 512, (t + 1) * 512)
                    if t + 1 < NT:
                        load_tile(t + 1)
                    for c in range(8):
                        bk, rb = bank()
                        for a in range(4):
                            op("pe", lambda e, a=a, c=c, bk=bk: e.transpose(
                                out=bk[:, a * 128:(a + 1) * 128], in_=xt[b][:, a, c * 128:(c + 1) * 128],
                                identity=ident[:]), reads=[r_xt[b], r_const], writes=[rb])
                        evac(xT[:, c, :], bk[:, :], [rb], [r_xT[c]])

                    chk("A1")

                    def proj(col0, m, rhs_t=None):
                        bk, rb = bank()
                        for c in range(8):
                            op("pe", lambda e, c=c, bk=bk: e.matmul(bk[0:m, :], lhsT=win[:, c, col0:col0 + m],
                                                                     rhs=xT[:, c, :], start=(c == 0), stop=(c == 7)),
                               reads=[r_w, r_xT[c]], writes=[rb])
                        return bk, rb

                    for m in range(3):
                        bk, rb = proj(m * 128, 128)
                        op("dve", lambda e, m=m, bk=bk: e.tensor_copy(out=cqf[:, m, :], in_=bk[:, :]),
                           reads=[rb], writes=[r_cqf[m]])
                        op("act", lambda e, m=m: e.activation(out=sq[:, m, :], in_=cqf[:, m, :], func=AF.Square),
                           reads=[r_cqf[m]], writes=[r_sq[m]])
                    chk("A1b")
                    for g, (chs, n) in enumerate((((0, 1), 256.0), ((2,), 128.0))):
                        bk, rb = bank()
                        for i, m in enumerate(chs):
                            op("pe", lambda e, m=m, bk=bk, i=i, chs=chs: e.matmul(
                                bk[:, :], lhsT=ones[:], rhs=sq[:, m, :], start=(i == 0), stop=(i == len(chs) - 1)),
                               reads=[r_sq[m], r_const], writes=[rb])
                        op("dve", lambda e, bk=bk, n=n: e.tensor_scalar(out=tmp1[:], in0=bk[:, :], scalar1=1.0 / n,
                                                                        scalar2=EPS, op0=ALU.mult, op1=ALU.add),
                           reads=[rb], writes=[r_tmp1])
                        op("act", lambda e: e.activation(out=tmp2[:], in_=tmp1[:], func=AF.Sqrt),
                           reads=[r_tmp1], writes=[r_tmp2])
                        op("dve", lambda e, g=g: e.reciprocal(out=rstd[:, g, :], in_=tmp2[:]),
                           reads=[r_tmp2], writes=[r_rstd[g]])
                        chk("A1c")
                        for m in chs:
                            op("pool", lambda e, m=m, g=g: e.tensor_tensor(out=cqn[:, m, :], in0=cqf[:, m, :],
                                                                           in1=rstd[:, g, :], op=ALU.mult),
                               reads=[r_cqf[m], r_rstd[g]], writes=[r_cqn[m]])

                    chk("A2")

                    def rope_comb(bkP, rbP, bkR, rbR, nrow, outap, rout):
                        tA, rA, tB, rB = rtmp[rtk["k"] % 2]
                        rtk["k"] += 1
                        op("dve", lambda e: e.tensor_tensor(out=tA[0:nrow, :], in0=bkP[0:nrow, :],
                                                            in1=rope_t[b][0:nrow, 0, :], op=ALU.mult),
                           reads=[rbP, r_rope[b]], writes=[rA])
                        op("dve", lambda e: e.tensor_tensor(out=tB[0:nrow, :], in0=bkR[0:nrow, :],
                                                            in1=rope_t[b][0:nrow, 1, :], op=ALU.mult),
                           reads=[rbR, r_rope[b]], writes=[rB])
                        op("pool", lambda e: e.tensor_tensor(out=outap, in0=tA[0:nrow, :], in1=tB[0:nrow, :],
                                                             op=ALU.add),
                           reads=[rA, rB], writes=[rout])

                    bkP, rbP = proj(384, 32)
                    bkR, rbR = proj(416, 32)
                    rope_comb(bkP, rbP, bkR, rbR, 32, o_kp[:, :], r_kp)

                    chk("A3")
                    for m in range(4):
                        bk, rb = proj(448 + m * 128, 128)
                        evac(o_mq[:, m, :], bk[:, :], [rb], [r_mq[m]])
                    for m in range(4):
                        bk, rb = proj(960 + m * 128, 128)
                        op("dve", lambda e, m=m, bk=bk: e.tensor_copy(out=o_mk[:, m, :], in_=bk[:, :]),
                           reads=[rb], writes=[r_mk[m]])
                        op("dve", lambda e, m=m, bk=bk: e.tensor_reduce(
                            out=kmsum[:, m, 2 * t:2 * t + 2], in_=bk[:, :].rearrange("p (b l) -> p b l", b=2),
                            op=ALU.add, axis=AX.X), reads=[rb], writes=[r_km])
                    for hp in range(2):
                        op("dve", lambda e, hp=hp: e.tensor_scalar(
                            out=kmT[64 * hp:64 * hp + 64, :, 16 * hp + 2 * t:16 * hp + 2 * t + 2],
                            in0=kmsum[64 * hp:64 * hp + 64, :, 2 * t:2 * t + 2],
                            scalar1=1.0 / 256.0, scalar2=None, op0=ALU.mult), reads=[r_km], writes=[r_km])
                    for a in range(4):
                        bk, rb = bank()
                        for c in range(8):
                            op("pe", lambda e, c=c, a=a, bk=bk: e.matmul(
                                bk[:, :], lhsT=xT[:, c, a * 128:(a + 1) * 128], rhs=win[:, c, 1472:1984],
                                start=(c == 0), stop=(c == 7)), reads=[r_w, r_xT[c]], writes=[rb])
                        evac(o_mv[:, a, :], bk[:, :], [rb], [r_mv[a]])

                    chk("A4")
                    for m in range(4):
                        bk, rb = bank()
                        for kc in range(2):
                            op("pe", lambda e, kc=kc, m=m, bk=bk: e.matmul(
                                bk[:, :], lhsT=wuq[:, kc, m * 128:(m + 1) * 128], rhs=cqn[:, kc, :],
                                start=(kc == 0), stop=(kc == 1)), reads=[r_w, r_cqn[kc]], writes=[rb])
                        evac(o_qn[:, m, :], bk[:, :], [rb], [r_qn[m]])
                    for m in range(2):
                        pr = []
                        for off in (512, 768):
                            bk, rb = bank()
                            for kc in range(2):
                                op("pe", lambda e, kc=kc, m=m, bk=bk, off=off: e.matmul(
                                    bk[:, :], lhsT=wuq[:, kc, off + m * 128:off + (m + 1) * 128], rhs=cqn[:, kc, :],
                                    start=(kc == 0), stop=(kc == 1)), reads=[r_w, r_cqn[kc]], writes=[rb])
                            pr.append((bk, rb))
                        rope_comb(pr[0][0], pr[0][1], pr[1][0], pr[1][1], 128, o_qp[:, m, :], r_qp[m])
                    for m in range(4):
                        bk, rb = bank()
                        op("pe", lambda e, m=m, bk=bk: e.matmul(bk[:, :], lhsT=wukv[:, m * 128:(m + 1) * 128],
                                                                rhs=cqn[:, 2, :], start=True, stop=True),
                           reads=[r_w, r_cqn[2]], writes=[rb])
                        evac(o_kn[:, m, :], bk[:, :], [rb], [r_kn[m]])
                    for a in range(4):
                        bk, rb = bank()
                        op("pe", lambda e, a=a, bk=bk: e.matmul(bk[:, :], lhsT=cqn[:, 2, a * 128:(a + 1) * 128],
                                                                rhs=wukv[:, 512:1024], start=True, stop=True),
                           reads=[r_w, r_cqn[2]], writes=[rb])
                        evac(o_va[:, a, :], bk[:, :], [rb], [r_va[a]])

                    chk("A5")
                    bkT, rbT = bank()
                    for a in range(4):
                        own = 2 * t + a // 2
                        bk, rb = bank()
                        for m in range(4):
                            op("pe", lambda e, m=m, a=a, bk=bk: e.matmul(
                                bk[:, 32 * m:32 * m + 32], lhsT=o_mq[:, m, a * 128:(a + 1) * 128],
                                rhs=kmT[:, m, :], start=True, stop=True),
                               reads=[r_mq[m], r_km], writes=[rb])
                        op("dve", lambda e, bk=bk, own=own: e.tensor_tensor(
                            out=gm[:], in0=bk[:, 0:128].rearrange("p (h n) -> p h n", h=8),
                            in1=pastm[:, own:own + 1, :].broadcast_to([128, 8, 16]), op=ALU.add),
                           reads=[rb, r_pm], writes=[r_gm])
                        gm2 = seln[:].rearrange("p (h n) -> p h n", h=8)
                        op("dve", lambda e: e.tensor_copy(out=gm2, in_=gm[:]), reads=[r_gm], writes=[r_seln])
                        for rnd in range(3):
                            op("dve", lambda e: e.tensor_reduce(out=m8[:, :, 0:1], in_=gm2, op=ALU.max, axis=AX.X),
                               reads=[r_seln], writes=[r_m8])
                            op("dve", lambda e: e.tensor_tensor(out=m8[:, :, 1:2].broadcast_to([128, 8, 16]) if False else tmpg[:],
                                                                in0=gm2, in1=m8[:, :, 0:1].broadcast_to([128, 8, 16]),
                                                                op=ALU.is_ge), reads=[r_seln, r_m8], writes=[r_tmpg])
                            op("dve", lambda e: e.scalar_tensor_tensor(out=gm2, in0=tmpg[:], scalar=-2e30, in1=gm2,
                                                                       op0=ALU.mult, op1=ALU.add),
                               reads=[r_tmpg, r_seln], writes=[r_seln])
                        op("dve", lambda e: e.tensor_reduce(out=m8[:, :, 3:4], in_=gm2, op=ALU.max, axis=AX.X),
                           reads=[r_seln], writes=[r_m8])
                        op("dve", lambda e: e.tensor_tensor(
                            out=seln[:].rearrange("p (h n) -> p h n", h=8), in0=gm[:],
                            in1=m8[:, :, 3:4].broadcast_to([128, 8, 16]), op=ALU.is_lt),
                           reads=[r_gm, r_m8], writes=[r_seln])
                        op("pe", lambda e, a=a: e.transpose(out=bkT[:, a * 128:(a + 1) * 128], in_=seln[:],
                                                            identity=ident[:]),
                           reads=[r_seln, r_const], writes=[rbT])
                    evac(o_ms[:, :], bkT[:, :], [rbT], [r_ms])

                    chk("A6")
                    def store(dst, src, rsrc):
                        SC.dma(dst, src, d_stA, reads=(rsrc if isinstance(rsrc, list) else [rsrc]),
                               writes=[r_scrA], live=True)
                    store(sc["qn"][:, :, tsl].rearrange("m p t -> p m t"), o_qn[:], r_qn)
                    store(sc["qp"][:, :, tsl].rearrange("m p t -> p m t"), o_qp[:], r_qp)
                    store(sc["kn"][:, :, tsl].rearrange("m p t -> p m t"), o_kn[:], r_kn)
                    store(sc["kp"][:, tsl], o_kp[:], r_kp)
                    store(sc["va"][tsl, :].rearrange("(a p) c -> p a c", p=128), o_va[:], r_va)
                    store(sc["mq"][:, :, tsl].rearrange("m p t -> p m t"), o_mq[:], r_mq)
                    store(sc["mk"][:, :, tsl].rearrange("m p t -> p m t"), o_mk[:], r_mk)
                    store(sc["mv"][tsl, :].rearrange("(a p) c -> p a c", p=128), o_mv[:], r_mv)
                    store(sc["ms"][:, tsl], o_ms[:], r_ms)
                    if s == 0:
                        for _ in range(per_tile):
                            pro_job()
                SC.flush()

            if stop == "A":
                return nc
            with ExitStack() as st:
                QT = [sb(st, f"QT{i}", [128, S], BF16) for i in range(2)]
                KT = [sb(st, f"KT{i}", [128, S], BF16) for i in range(2)]
                VA = [sb(st, f"VA{i}", [128, NKT, 128], BF16) for i in range(2)]
                r_QT = [Res(), Res()]
                r_KT = [Res(), Res()]
                r_VA = [Res(), Res()]
                d_QT = [SC.dsem(), SC.dsem()]
                d_KT = [SC.dsem(), SC.dsem()]
                d_VA = [SC.dsem(), SC.dsem()]
                PT = [sb(st, f"PT{i}", [128, 512], BF16) for i in range(4)]
                r_PT = [Res() for _ in range(4)]
                aT = [sb(st, f"aT{i}", [128, S], BF16) for i in range(2)]
                r_aT = [Res(), Res()]
                rd = sb(st, "rd", [128, 512], F32)
                r_rd = Res()
                cmask = sb(st, "cmask", [128, 4, 512], BF16)
                abias = sb(st, "abias", [128, 8, 35], F32)
                r_cB = Res()
                d_cB = SC.dsem()
                SC.dma(cmask[:].rearrange("p a n -> p (a n)"), c_cmask, d_cB, writes=[r_cB])
                SC.dma(abias[:].rearrange("p a n -> p (a n)"), c_abias, d_cB, writes=[r_cB], nodeps=True)
                op("pool", lambda e: e.memset(VA[0][:, :, 64:128], 1.0), writes=[r_VA[0]])
                op("pool", lambda e: e.memset(VA[1][:, :, 0:64], 1.0), writes=[r_VA[1]])

                sbank = [(banks[i], bres[i]) for i in range(4)]
                obank = [(banks[4 + i], bres[4 + i]) for i in range(2)]

                def load_head(hh):
                    b = hh % 2
                    h = hh % 8
                    if hh < 8:
                        R = 96
                        SC.dma(QT[b][0:64, :], sc["qn"][h // 2, 64 * (h % 2):64 * (h % 2) + 64, :], d_QT[b],
                               reads=[r_scrA], writes=[r_QT[b]])
                        SC.dma(QT[b][64:96, :], sc["qp"][h // 4, 32 * (h % 4):32 * (h % 4) + 32, :], d_QT[b],
                               reads=[r_scrA], writes=[r_QT[b]], nodeps=True)
                        SC.dma(KT[b][0:64, :], sc["kn"][h // 2, 64 * (h % 2):64 * (h % 2) + 64, :], d_KT[b],
                               reads=[r_scrA], writes=[r_KT[b]])
                        SC.dma(KT[b][64:96, :], sc["kp"][:, :], d_KT[b], reads=[r_scrA], writes=[r_KT[b]], nodeps=True)
                        vsrc = sc["va"]
                    else:
                        SC.dma(QT[b][0:64, :], sc["mq"][h // 2, 64 * (h % 2):64 * (h % 2) + 64, :], d_QT[b],
                               reads=[r_scrA], writes=[r_QT[b]])
                        SC.dma(QT[b][64:66, :], c_qalibi[h], d_QT[b], writes=[r_QT[b]], nodeps=True)
                        SC.dma(QT[b][66:82, :], sc["ms"][16 * h:16 * h + 16, :], d_QT[b], writes=[r_QT[b]], nodeps=True)
                        SC.dma(KT[b][0:64, :], sc["mk"][h // 2, 64 * (h % 2):64 * (h % 2) + 64, :], d_KT[b],
                               reads=[r_scrA], writes=[r_KT[b]])
                        SC.dma(KT[b][64:82, :], c_kconst, d_KT[b], writes=[r_KT[b]], nodeps=True)
                        vsrc = sc["mv"]
                    c0 = 0 if b == 0 else 64
                    for k0 in range(0, NKT, 8):
                        SC.dma(VA[b][:, k0:k0 + 8, c0:c0 + 64],
                               vsrc[k0 * 128:(k0 + 8) * 128, 64 * h:64 * h + 64].rearrange("(k p) d -> p k d", p=128),
                               d_VA[b], reads=[r_scrA], writes=[r_VA[b]], nodeps=(k0 > 0))

                steps = []
                for hh in range(16):
                    for j in range(NT):
                        nk = 4 * j + 4
                        for kt in range(nk):
                            steps.append((hh, j, kt, kt == 0, kt == nk - 1))

                def emit_score(i):
                    hh, j, kt, first, last = steps[i]
                    b = hh % 2
                    R = 96 if hh < 8 else 82
                    r = kt - 4 * j
                    q0 = 128 * r if r > 0 else 0
                    bk, rb = sbank[i % 4]
                    diag = r >= 0
                    op("pe", lambda e: e.matmul(bk[:, q0:512], lhsT=KT[b][0:R, kt * 128:(kt + 1) * 128],
                                                rhs=QT[b][0:R, j * 512 + q0:(j + 1) * 512], start=True, stop=not diag),
                       reads=[r_KT[b], r_QT[b]], writes=[rb])
                    if diag:
                        op("pe", lambda e: e.matmul(bk[:, q0:512], lhsT=identb[:], rhs=cmask[:, r, q0:512],
                                                    start=False, stop=True), reads=[r_cB, r_const], writes=[rb])

                def emit_rest(i):
                    hh, j, kt, first, last = steps[i]
                    b = hh % 2
                    h = hh % 8
                    r = kt - 4 * j
                    q0 = 128 * r if r > 0 else 0
                    bk, rb = sbank[i % 4]
                    pt, rpt = PT[i % 4], r_PT[i % 4]
                    ob, rob = obank[(hh * NT + j) % 2]
                    if hh < 8:
                        op("act", lambda e: e.activation(out=pt[:, q0:512], in_=bk[:, q0:512], func=AF.Exp,
                                                         scale=SC_MLA), reads=[rb], writes=[rpt])
                    else:
                        di = (512 * j - 128 * kt + 384) // 128
                        op("act", lambda e: e.activation(out=pt[:, q0:512], in_=bk[:, q0:512], func=AF.Exp,
                                                         scale=SC_MOBA, bias=abias[:, h, di:di + 1]),
                           reads=[rb, r_cB], writes=[rpt])
                    op("pe", lambda e: e.matmul(ob[:, q0:512], lhsT=VA[b][:, kt, :], rhs=pt[:, q0:512],
                                                start=first, stop=last), reads=[r_VA[b], rpt], writes=[rob])
                    if last:
                        u0, d0 = (0, 64) if b == 0 else (64, 0)
                        pair = hh // 2
                        ab = pair % 2
                        op("dve", lambda e: e.reciprocal(out=rd[u0:u0 + 64, :], in_=ob[d0:d0 + 64, :]),
                           reads=[rob], writes=[r_rd])
                        op("dve", lambda e: e.tensor_tensor(out=aT[ab][u0:u0 + 64, j * 512:(j + 1) * 512],
                                                            in0=ob[u0:u0 + 64, :], in1=rd[u0:u0 + 64, :], op=ALU.mult),
                           reads=[rob, r_rd], writes=[r_aT[ab]])
                        if j == NT - 1 and b == 1:
                            SC.dma(sc["at"][pair], aT[ab][:], d_stB, reads=[r_aT[ab]], writes=[r_scrB], live=True)

                load_head(0)
                LOOK = 3
                nst = len(steps)
                for i in range(min(LOOK, nst)):
                    emit_score(i)
                for i in range(nst):
                    hh, j, kt, first, last = steps[i]
                    if first and j == 0 and hh + 1 < 16:
                        load_head(hh + 1)
                    if i + LOOK < nst:
                        emit_score(i + LOOK)
                    emit_rest(i)
                SC.flush()

            if stop == "B":
                return nc
            with ExitStack() as st:
                wo = sb(st, "wo", [128, 8, 1024], BF16)
                wdn = sb(st, "wdn", [128, NPAIR, 1024], BF16)
                lnt = sb(st, "lnt", [128, 4, 1024], F32)
                cvp = sb(st, "cvp", [128, 44, 4], F32)
                r_cw = Res()
                d_cw = SC.dsem()
                SC.dma(wo[:].rearrange("p a n -> p (a n)"), wo_s, d_cw, reads=[r_wscr], writes=[r_cw])
                SC.dma(wdn[:].rearrange("p a n -> p (a n)"), wdn_s, d_cw, writes=[r_cw], nodeps=True)
                SC.dma(lnt[:].rearrange("p a n -> p (a n)"), lnp, d_cw, writes=[r_cw], nodeps=True)
                SC.dma(cvp[:].rearrange("p a n -> p (a n)"), convp, d_cw, writes=[r_cw], nodeps=True)
                aTt = sb(st, "aTt", [128, 8, 512], BF16)
                r_aTt = Res()
                d_aTt = SC.dsem()
                xr = [sb(st, f"xr{i}", [128, D], F32) for i in range(2)]
                r_xr = [Res(), Res()]
                d_xr = [SC.dsem(), SC.dsem()]
                ybs = [sb(st, f"yb{i}", [128, D], F32) for i in range(2)]
                r_ybs = [Res(), Res()]
                lnk = {"k": 0}
                x1f = sb(st, "x1f", [128, 4, D], F32)
                r_x1f = [Res() for _ in range(4)]
                x1T = sb(st, "x1T", [128, 8, 512], BF16)
                r_x1T = [Res() for _ in range(8)]
                statss = [sb(st, f"stats{i}", [128, 2, 6], F32) for i in range(2)]
                mvs = [sb(st, f"mv_{i}", [128, 2], F32) for i in range(2)]
                lnss = [sb(st, f"lns{i}", [128, 4], F32) for i in range(2)]
                r_lns = [Res(), Res()]
                wupt = [sb(st, f"wupt{i}", [128, 8, 256], BF16) for i in range(4)]
                r_wupt = [Res() for _ in range(4)]
                d_wupt = [SC.dsem() for _ in range(4)]
                hraw = [sb(st, f"hraw{i}", [128, 514], F32) for i in range(4)]
                r_hraw = [Res() for _ in range(4)]
                cacc = [sb(st, f"cacc{i}", [128, 512], F32) for i in range(4)]
                r_cacc = [Res() for _ in range(4)]
                sgs = [sb(st, f"sg{i}", [128, 512], F32) for i in range(2)]
                r_sgs = [Res(), Res()]
                carry = sb(st, "carry", [128, 44, 2], F32)
                r_carry = Res()
                actT = sb(st, "actT", [128, NPAIR, 512], BF16)
                r_actT = [Res() for _ in range(NPAIR)]
                ot = [sb(st, f"ot{i}", [128, D], F32) for i in range(2)]
                r_ot = [Res(), Res()]
                op("pool", lambda e: e.memset(carry[:], 0.0), writes=[r_carry])

                def layer_norm(k, gi, dst, rdst):
                    src, rsrc = ybs[k], r_ybs[k]
                    stats, mv_, lns, r_ln = statss[k], mvs[k], lnss[k], r_lns[k]
                    for hf in range(2):
                        op("dve", lambda e, hf=hf: e.bn_stats(out=stats[:, hf, :], in_=src[:, hf * 512:(hf + 1) * 512]),
                           reads=[rsrc], writes=[r_ln])
                    op("dve", lambda e: e.bn_aggr(out=mv_[:], in_=stats[:].rearrange("p a n -> p (a n)")),
                       reads=[r_ln], writes=[r_ln])
                    op("dve", lambda e: e.tensor_scalar(out=lns[:, 0:1], in0=mv_[:, 1:2], scalar1=EPS, scalar2=None,
                                                        op0=ALU.add), reads=[r_ln], writes=[r_ln])
                    op("act", lambda e: e.activation(out=lns[:, 1:2], in_=lns[:, 0:1], func=AF.Sqrt),
                       reads=[r_ln], writes=[r_ln])
                    op("dve", lambda e: e.reciprocal(out=lns[:, 2:3], in_=lns[:, 1:2]), reads=[r_ln], writes=[r_ln])
                    op("dve", lambda e: e.tensor_scalar(out=lns[:, 3:4], in0=mv_[:, 0:1], scalar1=-1.0,
                                                        scalar2=lns[:, 2:3], op0=ALU.mult, op1=ALU.mult),
                       reads=[r_ln], writes=[r_ln])
                    op("act", lambda e: e.activation(out=src[:], in_=src[:], func=AF.Identity, scale=lns[:, 2:3],
                                                     bias=lns[:, 3:4]), reads=[rsrc, r_ln], writes=[rsrc])
                    op("pool", lambda e: e.tensor_tensor(out=src[:], in0=src[:], in1=lnt[:, gi, :], op=ALU.mult),
                       reads=[rsrc, r_cw], writes=[rsrc])
                    op("pool", lambda e: e.tensor_tensor(out=dst, in0=src[:], in1=lnt[:, gi + 1, :], op=ALU.add),
                       reads=[rsrc, r_cw], writes=[rdst])

                def load_x(t, a):
                    k = (t * 4 + a) % 2
                    SC.dma(xr[k][:], x[s, t * 512 + a * 128:t * 512 + (a + 1) * 128, :], d_xr[k], writes=[r_xr[k]])

                wk = {"k": 0}

                def load_wup(p):
                    k = wk["k"] % 4
                    wk["k"] += 1
                    SC.dma(wupt[k][:].rearrange("p a n -> p (a n)"), wup_s[p], d_wupt[k], reads=[r_wscr],
                           writes=[r_wupt[k]])
                    return k

                for t in range(NT):
                    tsl = slice(t * 512, (t + 1) * 512)
                    SC.dma(aTt[:], sc["at"][:, :, tsl].rearrange("c p t -> p c t"), d_aTt, reads=[r_scrB],
                           writes=[r_aTt])
                    load_x(t, 0)
                    wq = [load_wup(0), load_wup(1), load_wup(2)]
                    for a in range(4):
                        k = (t * 4 + a) % 2
                        if a + 1 < 4:
                            load_x(t, a + 1)
                        bks = []
                        for n in range(2):
                            bk, rb = bank()
                            for c in range(8):
                                op("pe", lambda e, c=c, n=n, bk=bk: e.matmul(
                                    bk[:, :], lhsT=aTt[:, c, a * 128:(a + 1) * 128], rhs=wo[:, c, n * 512:(n + 1) * 512],
                                    start=(c == 0), stop=(c == 7)), reads=[r_aTt, r_cw], writes=[rb])
                            bks.append((bk, rb))
                        ky = lnk["k"] % 2
                        lnk["k"] += 1
                        for n in range(2):
                            bk, rb = bks[n]
                            op("dve", lambda e, n=n, bk=bk: e.scalar_tensor_tensor(
                                out=ybs[ky][:, n * 512:(n + 1) * 512], in0=xr[k][:, n * 512:(n + 1) * 512], scalar=ALPHA,
                                in1=bk[:, :], op0=ALU.mult, op1=ALU.add), reads=[r_xr[k], rb], writes=[r_ybs[ky]])
                        layer_norm(ky, 0, x1f[:, a, :], r_x1f[a])
                    for c in range(8):
                        bk, rb = bank()
                        for a in range(4):
                            op("pe", lambda e, a=a, c=c, bk=bk: e.transpose(
                                out=bk[:, a * 128:(a + 1) * 128], in_=x1f[:, a, c * 128:(c + 1) * 128],
                                identity=ident[:]), reads=[r_x1f[a], r_const], writes=[rb])
                        if c % 2 == 0:
                            op("act", lambda e, c=c, bk=bk: e.activation(out=x1T[:, c, :], in_=bk[:, :], func=AF.Copy),
                               reads=[rb], writes=[r_x1T[c]])
                        else:
                            op("dve", lambda e, c=c, bk=bk: e.tensor_copy(out=x1T[:, c, :], in_=bk[:, :]),
                               reads=[rb], writes=[r_x1T[c]])
                    for p in range(NPAIR):
                        kw = wq.pop(0)
                        if p + 3 < NPAIR:
                            wq.append(load_wup(p + 3))
                        for gu in range(2):
                            ch = p + 22 * gu
                            bk, rb = bank()
                            for c in range(8):
                                op("pe", lambda e, c=c, bk=bk, gu=gu: e.matmul(
                                    bk[:, :], lhsT=wupt[kw][:, c, gu * 128:(gu + 1) * 128], rhs=x1T[:, c, :],
                                    start=(c == 0), stop=(c == 7)), reads=[r_wupt[kw], r_x1T[c]], writes=[rb])
                            bi = 2 * (p % 2) + gu
                            hr, rhr = hraw[bi], r_hraw[bi]
                            ca, rca = cacc[bi], r_cacc[bi]
                            op("pool", lambda e, hr=hr, ch=ch: e.tensor_copy(out=hr[:, 0:2], in_=carry[:, ch, :]),
                               reads=[r_carry], writes=[rhr])
                            op("act", lambda e, hr=hr, bk=bk: e.activation(out=hr[:, 2:514], in_=bk[:, :], func=AF.Copy),
                               reads=[rb], writes=[rhr])
                            op("act", lambda e, ca=ca, bk=bk, ch=ch: e.activation(
                                out=ca[:], in_=bk[:, :], func=AF.Identity, scale=cvp[:, ch, 2:3], bias=cvp[:, ch, 3:4]),
                               reads=[rb, r_cw], writes=[rca])
                            op("pool", lambda e, hr=hr, ch=ch: e.tensor_copy(out=carry[:, ch, :], in_=hr[:, 512:514]),
                               reads=[rhr], writes=[r_carry])
                            op("dve", lambda e, ca=ca, hr=hr, ch=ch: e.scalar_tensor_tensor(
                                out=ca[:], in0=hr[:, 1:513], scalar=cvp[:, ch, 1:2], in1=ca[:],
                                op0=ALU.mult, op1=ALU.add), reads=[rhr, rca, r_cw], writes=[rca])
                            op("dve", lambda e, ca=ca, hr=hr, ch=ch: e.scalar_tensor_tensor(
                                out=ca[:], in0=hr[:, 0:512], scalar=cvp[:, ch, 0:1], in1=ca[:],
                                op0=ALU.mult, op1=ALU.add), reads=[rhr, rca, r_cw], writes=[rca])
                        bg, bu = 2 * (p % 2), 2 * (p % 2) + 1
                        sg, r_sg = sgs[p % 2], r_sgs[p % 2]
                        op("act", lambda e, sg=sg, bg=bg: e.activation(out=sg[:], in_=cacc[bg][:], func=AF.Silu),
                           reads=[r_cacc[bg]], writes=[r_sg])
                        op("pool", lambda e, p=p, sg=sg, bu=bu: e.tensor_tensor(out=actT[:, p, :], in0=sg[:],
                                                                                 in1=cacc[bu][:], op=ALU.mult),
                           reads=[r_sg, r_cacc[bu]], writes=[r_actT[p]])
                    for a in range(4):
                        ko = (t * 4 + a) % 2
                        bks = []
                        for n in range(2):
                            bk, rb = bank()
                            for p in range(NPAIR):
                                op("pe", lambda e, p=p, n=n, bk=bk: e.matmul(
                                    bk[:, :], lhsT=actT[:, p, a * 128:(a + 1) * 128], rhs=wdn[:, p, n * 512:(n + 1) * 512],
                                    start=(p == 0), stop=(p == NPAIR - 1)), reads=[r_actT[p], r_cw], writes=[rb])
                            bks.append((bk, rb))
                        ky = lnk["k"] % 2
                        lnk["k"] += 1
                        for n in range(2):
                            bk, rb = bks[n]
                            op("dve", lambda e, n=n, bk=bk: e.scalar_tensor_tensor(
                                out=ybs[ky][:, n * 512:(n + 1) * 512], in0=x1f[:, a, n * 512:(n + 1) * 512], scalar=ALPHA,
                                in1=bk[:, :], op0=ALU.mult, op1=ALU.add), reads=[r_x1f[a], rb], writes=[r_ybs[ky]])
                        layer_norm(ky, 2, ot[ko][:], r_ot[ko])
                        SC.dma(out[s, t * 512 + a * 128:t * 512 + (a + 1) * 128, :], ot[ko][:], d_out,
                               reads=[r_ot[ko]], writes=[r_out], live=True, q="pool")
                SC.flush()

        SC._wait("sp", ("d", d_out, d_out.cnt))
        SC.flush()
    return nc


def _bf(a):
    return np.ascontiguousarray(a.astype(ml_dtypes.bfloat16))


def make_consts(S):
    NT = S // 512
    c = {}
    c["c_ident"] = np.eye(128, dtype=np.float32)
    c["c_identb"] = _bf(np.eye(128, dtype=np.float32))
    k = np.arange(128)[:, None, None]
    r = np.arange(4)[None, :, None]
    q = np.arange(512)[None, None, :]
    c["c_cmask"] = _bf(np.where(128 * r + k > q, NEGBIG, 0.0).astype(np.float32).reshape(128, 2048))
    half = 16
    inv = (np.float32(10000.0) ** (-np.arange(half, dtype=np.float32) / np.float32(half))).astype(np.float32)
    pos = np.arange(S, dtype=np.float32)
    ang = (pos[:, None] * inv[None, :]).astype(np.float32)
    cos = np.cos(ang).astype(np.float32)
    sin = np.sin(ang).astype(np.float32)
    d = np.arange(128) % 32
    cosT = cos[:, d % 16].T
    sinT = sin[:, d % 16].T * np.where(d < 16, -1.0, 1.0)[:, None]
    rope = np.stack([cosT.reshape(128, NT, 512), sinT.reshape(128, NT, 512)], axis=2)
    c["c_rope"] = np.ascontiguousarray(rope.transpose(1, 0, 2, 3).reshape(NT, 128, 1024).astype(np.float32))
    kc = np.zeros((18, S), np.float32)
    kc[0:2] = 1.0
    blk = np.arange(S) // 256
    for n in range(16):
        kc[2 + n] = np.where(blk == n, NEGBIG, 0.0)
    c["c_kconst"] = _bf(kc)
    slopes = (2.0 ** (-8.0 * np.arange(1, 9, dtype=np.float32) / 8.0)).astype(np.float32)
    dq = (np.arange(S) % 512).astype(np.float32)
    v = (-slopes[:, None] * dq[None, :] / np.float32(SC_MOBA)).astype(np.float32)
    hi = v.astype(ml_dtypes.bfloat16)
    lo = (v - hi.astype(np.float32)).astype(ml_dtypes.bfloat16)
    c["c_qalibi"] = np.ascontiguousarray(np.stack([hi, lo], axis=1))
    i = np.arange(35)
    Dd = (128 * i - 384).astype(np.float32)
    kk = np.arange(128, dtype=np.float32)
    ab = -slopes[None, :, None] * (Dd[None, None, :] - kk[:, None, None])
    c["c_abias"] = np.ascontiguousarray(ab.astype(np.float32).reshape(128, 8 * 35))
    own = np.arange(16)[:, None]
    n = np.arange(16)[None, :]
    pm = np.where(n < own, 0.0, np.where(n == own, 1e30, -1e30)).astype(np.float32)
    c["c_pastm"] = np.ascontiguousarray(np.broadcast_to(pm.reshape(1, 256), (128, 256)))
    return c


def prep_weights(w_in, q_norm_g, w_uq, kv_norm_g, w_ukv, w_o, ln1_g, ln1_b, w_up, conv_w, conv_b, w_down,
                 ln2_g, ln2_b):
    f = lambda a: np.ascontiguousarray(np.asarray(a, dtype=np.float32))
    w_in, w_uq, w_ukv, w_o, w_up, w_down = (f(a[0]) for a in (w_in, w_uq, w_ukv, w_o, w_up, w_down))
    r = np.arange(32)
    kr_sw = w_in[:, 384 + ((r + 16) % 32)]
    win = np.concatenate([w_in[:, 0:416], kr_sw, w_in[:, 416:1952]], axis=1)
    win = win.reshape(8, 128, WIN_COLS).transpose(1, 0, 2).reshape(128, 8 * WIN_COLS)
    h = np.arange(8)
    nope = (h[:, None] * 96 + np.arange(64)[None, :]).reshape(-1)
    pe = (h[:, None] * 96 + 64 + r[None, :]).reshape(-1)
    pesw = (h[:, None] * 96 + 64 + ((r + 16) % 32)[None, :]).reshape(-1)
    wuq = w_uq[:, np.concatenate([nope, pe, pesw])]
    wuq = wuq.reshape(2, 128, 1024).transpose(1, 0, 2).reshape(128, 2048)
    kcols = (h[:, None] * 128 + np.arange(64)[None, :]).reshape(-1)
    vcols = kcols + 64
    wukv = w_ukv[:, np.concatenate([kcols, vcols])]
    wo = w_o.reshape(8, 128, 1024).transpose(1, 0, 2).reshape(128, 8192)
    wdn = w_down.reshape(NPAIR, 128, 1024).transpose(1, 0, 2).reshape(128, NPAIR * 1024)
    wu = w_up.reshape(8, 128, 2, NPAIR, 128)
    wu = wu.transpose(3, 1, 0, 2, 4).reshape(NPAIR, 128, 8 * 256)
    lnp = np.stack([f(ln1_g[0]), f(ln1_b[0]), f(ln2_g[0]), f(ln2_b[0])], axis=0).reshape(1, 4096)
    lnp = np.broadcast_to(lnp, (128, 4096))
    cw = f(conv_w[0])
    cb = f(conv_b[0])
    cv = np.concatenate([cw, cb[None, :]], axis=0)
    cv = cv.reshape(4, 44, 128).transpose(2, 1, 0).reshape(128, 176)
    return {
        "w_in": f(win), "w_uq": f(wuq), "w_ukv": f(wukv), "w_o": f(wo), "w_dn": f(wdn), "w_up": f(wu),
        "qg": f(f(q_norm_g[0]).reshape(2, 128).T), "kvg": f(f(kv_norm_g[0]).reshape(128, 1)),
        "lnp": f(lnp), "convp": f(cv),
    }


_NC_CACHE = {}


def run(x, params, ncores, debug=False, stop=None):
    B, S, _ = x.shape
    nseq = B // ncores
    key = (nseq, S, debug)
    if key not in _NC_CACHE:
        _NC_CACHE[key] = build(nseq, S, debug, stop)
    nc = _NC_CACHE[key]
    shared = dict(prep_weights(**params))
    shared.update(make_consts(S))
    xs = np.ascontiguousarray(np.asarray(x, dtype=np.float32)).reshape(ncores, nseq, S, D)
    in_maps = [dict(shared, x=xs[i]) for i in range(ncores)]
    res = run_bass_kernel_spmd(nc, in_maps, core_ids=list(range(ncores)))
    return res


def kernel(x, w_in, q_norm_g, w_uq, kv_norm_g, w_ukv, w_o, ln1_g, ln1_b, w_up, conv_w, conv_b, w_down,
           ln2_g, ln2_b):
    params = dict(w_in=w_in, q_norm_g=q_norm_g, w_uq=w_uq, kv_norm_g=kv_norm_g, w_ukv=w_ukv, w_o=w_o,
                  ln1_g=ln1_g, ln1_b=ln1_b, w_up=w_up, conv_w=conv_w, conv_b=conv_b, w_down=w_down,
                  ln2_g=ln2_g, ln2_b=ln2_b)
    x = np.asarray(x)
    res = run(x, params, NCORES)
    outs = [np.asarray(r["out"]) for r in res.results]
    return np.concatenate(outs, axis=0).astype(np.float32)
```

```python
import math
from contextlib import ExitStack

import numpy as np
import ml_dtypes
import concourse.bass as bass
import concourse.mybir as mybir
from concourse.bass_utils import run_bass_kernel_spmd

F32 = mybir.dt.float32
BF16 = mybir.dt.bfloat16
AF = mybir.ActivationFunctionType
ALU = mybir.AluOpType
AX = mybir.AxisListType

D = 1024
NCORES = 8
EPS = 1e-5
NEGBIG = -30000.0
ALPHA = 2.0 ** 0.25
SC_MLA = 96.0 ** -0.5
SC_MOBA = 0.125
DFF = 2816
NPAIR = 22
WIN_COLS = 1984
import os as _os
ALL_INC = _os.environ.get("ALL_INC", "0") == "1"


class Res:
    __slots__ = ("w", "r", "scratch")

    def __init__(self, scratch=False):
        self.w = None
        self.r = []
        self.scratch = scratch


class DSem:
    def __init__(self, sem):
        self.sem = sem
        self.cnt = 0


class _Rec:
    def __getattr__(self, name):
        def f(*a, **k):
            self.call = (name, a, k)
        return f


class Sched:
    ENGS = ("pe", "act", "dve", "pool", "sp")
    CENGS = ("pe", "act", "dve", "pool")

    def __init__(self, nc, stack):
        self.nc = nc
        self.stack = stack
        self.prog = {e: [] for e in self.ENGS}
        self.sem = {}
        self.cnt = {}
        self.targets = {e: set() for e in self.CENGS}
        for e in self.CENGS:
            self.sem[e] = stack.enter_context(nc.semaphore("s_" + e))
            self.cnt[e] = 0
        self.seen = {e: {} for e in self.ENGS}
        self.dsems = []
        self.rank = {e: {} for e in self.CENGS}
        self.flushed = {e: 0 for e in self.CENGS}

    def dsem(self):
        s = self.stack.enter_context(self.nc.semaphore(f"d{len(self.dsems)}"))
        d = DSem(s)
        self.dsems.append(d)
        return d

    def _wait(self, eng, ev):
        if ev is None:
            return
        kind, obj, val = ev
        if kind == "e":
            if obj == eng and eng == "pe":
                return
            key = obj
        else:
            key = id(obj)
            if val is None:
                val = obj.cnt
        if val <= 0 or self.seen[eng].get(key, 0) >= val:
            return
        self.seen[eng][key] = val
        if kind == "e":
            assert val > self.flushed[obj] or val in self.rank[obj], (eng, obj, val)
            self.targets[obj].add(val)
        self.prog[eng].append(("w", kind, obj, val))

    def _deps(self, eng, reads, writes):
        for r in reads:
            self._wait(eng, r.w)
        for w in writes:
            if w.scratch:
                continue
            self._wait(eng, w.w)
            for ev in w.r:
                self._wait(eng, ev)

    def _commit(self, ev, reads, writes):
        for r in reads:
            if not r.scratch:
                r.r.append(ev)
        for w in writes:
            w.w = ev
            w.r = []

    def op(self, eng, fn, reads=(), writes=()):
        self._deps(eng, reads, writes)
        self.cnt[eng] += 1
        ev = ("e", eng, self.cnt[eng])
        rec = _Rec()
        fn(rec)
        if ALL_INC:
            self.targets[eng].add(self.cnt[eng])
        self.prog[eng].append(("i", rec.call, self.cnt[eng]))
        self._commit(ev, reads, writes)
        return ev

    def dma(self, out, in_, ds, reads=(), writes=(), live=False, nodeps=False, q="sp"):
        if not nodeps:
            self._deps(q, reads, writes)
        ds.cnt += 16
        assert ds.cnt < 65000
        ev = ("d", ds, None if live else ds.cnt)
        self.prog[q].append(("d", out, in_, ds.sem))
        self._commit(ev, reads, writes)
        return ev

    def barrier(self):
        for e in self.ENGS:
            for o in self.CENGS:
                self._wait(e, ("e", o, self.cnt[o]))
            for d in self.dsems:
                self._wait(e, ("d", d, d.cnt))

    def flush(self):
        self.barrier()
        rank = self.rank
        for e in self.CENGS:
            new = sorted(v for v in self.targets[e] if v not in rank[e])
            base = len(rank[e])
            for i, v in enumerate(new):
                assert v > self.flushed[e]
                rank[e][v] = base + i + 1
            assert len(rank[e]) < 65000, (e, len(rank[e]))

        def replay(name):
            def body(eng):
                for it in self.prog[name]:
                    if it[0] == "w":
                        _, kind, obj, val = it
                        if kind == "e":
                            eng.wait_ge(self.sem[obj], rank[obj][val])
                        else:
                            eng.wait_ge(obj.sem, val)
                    elif it[0] == "i":
                        nm, a, k = it[1]
                        ins = getattr(eng, nm)(*a, **k)
                        if it[2] in rank[name]:
                            ins.then_inc(self.sem[name], 1)
                    else:
                        eng.dma_start(out=it[1], in_=it[2]).then_inc(it[3], 16)
            return body

        with self.nc.Block() as block:
            block.tensor(replay("pe"))
            block.scalar(replay("act"))
            block.vector(replay("dve"))
            block.gpsimd(replay("pool"))
            block.sync(replay("sp"))
        for e in self.ENGS:
            self.prog[e] = []
        for e in self.CENGS:
            self.flushed[e] = self.cnt[e]


class _Stop(Exception):
    pass


def build(NSEQ, S, debug=False, stop=None):
    st_ = {}
    try:
        return _build(NSEQ, S, debug, stop, st_)
    except _Stop:
        return st_["nc"]


def _build(NSEQ, S, debug, stop, st_):
    assert S % 512 == 0
    NT = S // 512
    NKT = S // 128
    nc = bass.Bass("TRN2", target_bir_lowering=False)
    st_["nc"] = nc

    def din(name, shape, dt=F32):
        return nc.dram_tensor(name, list(shape), dt, kind="ExternalInput").ap()

    def dscr(name, shape, dt=BF16):
        kind = "ExternalOutput" if debug else "Internal"
        return nc.dram_tensor(name, list(shape), dt, kind=kind).ap()

    x = din("x", [NSEQ, S, D])
    out = nc.dram_tensor("out", [NSEQ, S, D], F32, kind="ExternalOutput").ap()
    w_in = din("w_in", [128, 8 * WIN_COLS])
    w_uq = din("w_uq", [128, 2 * 1024])
    w_ukv = din("w_ukv", [128, 1024])
    qg = din("qg", [128, 2])
    kvg = din("kvg", [128, 1])
    w_o = din("w_o", [128, 8 * 1024])
    w_dn = din("w_dn", [128, NPAIR * 1024])
    w_up = din("w_up", [NPAIR, 128, 2048])
    lnp = din("lnp", [128, 4 * 1024])
    convp = din("convp", [128, 44 * 4])
    c_ident = din("c_ident", [128, 128])
    c_identb = din("c_identb", [128, 128], BF16)
    c_cmask = din("c_cmask", [128, 4 * 512], BF16)
    c_rope = din("c_rope", [NT, 128, 2 * 512])
    c_kconst = din("c_kconst", [18, S], BF16)
    c_qalibi = din("c_qalibi", [8, 2, S], BF16)
    c_abias = din("c_abias", [128, 8 * 35])
    c_pastm = din("c_pastm", [128, 16 * 16])

    wo_s = dscr("wo_s", [128, 8 * 1024])
    wdn_s = dscr("wdn_s", [128, NPAIR * 1024])
    wup_s = dscr("wup_s", [NPAIR, 128, 2048])
    scr = []
    for s in range(NSEQ):
        scr.append(dict(
            qn=dscr(f"qn{s}", [4, 128, S]), qp=dscr(f"qp{s}", [2, 128, S]),
            kn=dscr(f"kn{s}", [4, 128, S]), kp=dscr(f"kp{s}", [32, S]),
            va=dscr(f"va{s}", [S, 512]),
            mq=dscr(f"mq{s}", [4, 128, S]), mk=dscr(f"mk{s}", [4, 128, S]),
            mv=dscr(f"mv{s}", [S, 512]), ms=dscr(f"ms{s}", [128, S]),
            at=dscr(f"at{s}", [8, 128, S]),
        ))

    with ExitStack() as top:
        SC = Sched(nc, top)
        op = SC.op
        uniq = {"n": 0}

        def sb(st, name, shape, dt):
            uniq["n"] += 1
            return st.enter_context(nc.sbuf_tensor(f"{name}_{uniq['n']}", list(shape), dt))

        banks = [top.enter_context(nc.psum_tensor(f"bank{i}", [128, 512], F32)) for i in range(8)]
        bres = [Res() for _ in range(8)]
        bstate = {"i": 0}

        def bank():
            i = bstate["i"]
            bstate["i"] = (i + 1) % 8
            return banks[i], bres[i]

        ident = sb(top, "ident", [128, 128], F32)
        identb = sb(top, "identb", [128, 128], BF16)
        ones = sb(top, "ones", [128, 128], BF16)
        r_const = Res()
        d_const = SC.dsem()
        SC.dma(ident[:], c_ident, d_const, writes=[r_const])
        SC.dma(identb[:], c_identb, d_const, writes=[r_const], nodeps=True)
        op("pool", lambda e: e.memset(ones[:], 1.0), writes=[r_const])

        d_wscr = SC.dsem()
        r_wscr = Res(scratch=True)

        def chk(tag):
            if stop == tag:
                SC.flush()
                raise _Stop()

        def make_stager(st, piece=2048):
            return dict(stgs=[sb(st, f"stg{i}", [128, piece], F32) for i in range(2)], rs=[Res(), Res()],
                        ds=[SC.dsem(), SC.dsem()], k=0, piece=piece)

        def cast_stream(sg_, src, ncols, dst_fn, piece=2048):
            for c0 in range(0, ncols, piece):
                c1 = min(ncols, c0 + piece)
                k = sg_["k"]
                sg_["k"] += 1
                b = k % 2
                SC.dma(sg_["stgs"][b][:, 0:c1 - c0], src[:, c0:c1], sg_["ds"][b], writes=[sg_["rs"][b]])
                dst_fn(c0, c1, sg_["stgs"][b][:, 0:c1 - c0], sg_["rs"][b], k)

        pro_jobs = []
        for c0 in range(0, 8 * 1024, 2048):
            pro_jobs.append((w_o[:, c0:c0 + 2048], wo_s[:, c0:c0 + 2048]))
        for c0 in range(0, NPAIR * 1024, 2048):
            pro_jobs.append((w_dn[:, c0:c0 + 2048], wdn_s[:, c0:c0 + 2048]))
        for p in range(NPAIR):
            pro_jobs.append((w_up[p], wup_s[p]))

        d_out = SC.dsem()
        r_out = Res(scratch=True)

        for s in range(NSEQ):
            sc = scr[s]
            d_stA = SC.dsem()
            r_scrA = Res(scratch=True)
            d_stB = SC.dsem()
            r_scrB = Res(scratch=True)

            with ExitStack() as st:
                win = sb(st, "win", [128, 8, WIN_COLS], BF16)
                wuq = sb(st, "wuq", [128, 2, 1024], BF16)
                wukv = sb(st, "wukv", [128, 1024], BF16)
                qg_t = sb(st, "qg_t", [128, 2], F32)
                kvg_t = sb(st, "kvg_t", [128, 1], F32)
                r_w = Res()
                d_w = SC.dsem()
                SC.dma(qg_t[:], qg, d_w, writes=[r_w])
                SC.dma(kvg_t[:], kvg, d_w, writes=[r_w], nodeps=True)
                with ExitStack() as st2:
                    winf = win[:].rearrange("p c n -> p (c n)")
                    stager2 = make_stager(st2)

                    def f_win(c0, c1, stg, rstg, k):
                        op("act" if k % 2 == 0 else "dve",
                           (lambda e: e.activation(out=winf[:, c0:c1], in_=stg, func=AF.Copy)) if k % 2 == 0 else
                           (lambda e: e.tensor_copy(out=winf[:, c0:c1], in_=stg)),
                           reads=[rstg], writes=[r_w])
                    cast_stream(stager2, w_in, 8 * WIN_COLS, f_win)

                    def f_wuq(c0, c1, stg, rstg, k):
                        kc = c0 // 1024
                        op("dve", lambda e: e.tensor_scalar(out=wuq[:, kc, :], in0=stg, scalar1=qg_t[:, kc:kc + 1],
                                                            scalar2=None, op0=ALU.mult),
                           reads=[rstg, r_w], writes=[r_w])
                    cast_stream(stager2, w_uq, 2048, f_wuq, piece=1024)

                    def f_wukv(c0, c1, stg, rstg, k):
                        op("dve", lambda e: e.tensor_scalar(out=wukv[:, :], in0=stg, scalar1=kvg_t[:, 0:1],
                                                            scalar2=None, op0=ALU.mult),
                           reads=[rstg, r_w], writes=[r_w])
                    cast_stream(stager2, w_ukv, 1024, f_wukv, piece=1024)
                    SC.flush()

                xt = [sb(st, f"xt{i}", [128, 4, D], F32) for i in range(2)]
                r_xt = [Res(), Res()]
                d_xt = [SC.dsem(), SC.dsem()]
                rope_t = [sb(st, f"rope{i}", [128, 2, 512], F32) for i in range(2)]
                r_rope = [Res(), Res()]
                d_rope = [SC.dsem(), SC.dsem()]
                xT = sb(st, "xT", [128, 8, 512], BF16)
                r_xT = [Res() for _ in range(8)]
                cqf = sb(st, "cqf", [128, 3, 512], F32)
                r_cqf = [Res() for _ in range(3)]
                sq = sb(st, "sq", [128, 3, 512], BF16)
                r_sq = [Res() for _ in range(3)]
                rstd = sb(st, "rstd", [128, 2, 512], F32)
                r_rstd = [Res(), Res()]
                cqn = sb(st, "cqn", [128, 3, 512], BF16)
                r_cqn = [Res() for _ in range(3)]
                tmp1 = sb(st, "tmp1", [128, 512], F32)
                r_tmp1 = Res()
                tmp2 = sb(st, "tmp2", [128, 512], F32)
                r_tmp2 = Res()
                rtmp = [(sb(st, f"rtA{i}", [128, 512], F32), Res(), sb(st, f"rtB{i}", [128, 512], F32), Res())
                        for i in range(2)]
                rtk = {"k": 0}
                pastm = sb(st, "pastm", [128, 16, 16], F32)
                kmsum = sb(st, "kmsum", [128, 4, 16], F32)
                kmT = sb(st, "kmT", [128, 4, 32], BF16)
                r_km = Res()
                gm = sb(st, "gm", [128, 8, 16], F32)
                r_gm = Res()
                m8 = sb(st, "m8", [128, 8, 8], F32)
                r_m8 = Res()
                seln = sb(st, "seln", [128, 128], F32)
                r_seln = Res()
                tmpg = sb(st, "tmpg", [128, 8, 16], F32)
                r_tmpg = Res()
                d_pm = SC.dsem()
                r_pm = Res()
                SC.dma(pastm[:].rearrange("p a b -> p (a b)"), c_pastm, d_pm, writes=[r_pm])
                op("pool", lambda e: e.memset(kmT[:], 0.0), writes=[r_km])
                op("pool", lambda e: e.memset(kmsum[:], 0.0), writes=[r_km])

                def obuf(name, shape, n=1):
                    return sb(st, name, shape, BF16), ([Res() for _ in range(n)] if n > 1 else Res())
                o_qn, r_qn = obuf("o_qn", [128, 4, 512], 4)
                o_qp, r_qp = obuf("o_qp", [128, 2, 512], 2)
                o_kn, r_kn = obuf("o_kn", [128, 4, 512], 4)
                o_kp, r_kp = obuf("o_kp", [32, 512])
                o_va, r_va = obuf("o_va", [128, 4, 512], 4)
                o_mq, r_mq = obuf("o_mq", [128, 4, 512], 4)
                o_mk, r_mk = obuf("o_mk", [128, 4, 512], 4)
                o_mv, r_mv = obuf("o_mv", [128, 4, 512], 4)
                o_ms, r_ms = obuf("o_ms", [128, 512])

                if s == 0:
                    pstg = [sb(st, f"pstg{i}", [128, 2048], F32) for i in range(2)]
                    pstb = [sb(st, f"pstb{i}", [128, 2048], BF16) for i in range(2)]
                    r_pstg = [Res(), Res()]
                    r_pstb = [Res(), Res()]
                    d_pstg = [SC.dsem(), SC.dsem()]
                    pjk = {"k": 0}
                    per_tile = -(-len(pro_jobs) // NT)

                    def pro_job():
                        k = pjk["k"]
                        if k >= len(pro_jobs):
                            return
                        pjk["k"] += 1
                        src, dst = pro_jobs[k]
                        b = k % 2
                        SC.dma(pstg[b][:], src, d_pstg[b], writes=[r_pstg[b]])
                        op("pool", lambda e: e.tensor_copy(out=pstb[b][:], in_=pstg[b][:]),
                           reads=[r_pstg[b]], writes=[r_pstb[b]])
                        SC.dma(dst, pstb[b][:], d_wscr, reads=[r_pstb[b]], writes=[r_wscr], live=True)

                evk = {"k": 0}

                def evac(outap, inap, reads, writes):
                    evk["k"] += 1
                    if evk["k"] % 4 != 0:
                        op("act", lambda e: e.activation(out=outap, in_=inap, func=AF.Copy), reads, writes)
                    else:
                        op("dve", lambda e: e.tensor_copy(out=outap, in_=inap), reads, writes)

                def load_tile(t):
                    b = t % 2
                    SC.dma(xt[b][:], x[s, t * 512:(t + 1) * 512, :].rearrange("(a p) d -> p a d", p=128),
                           d_xt[b], writes=[r_xt[b]])
                    SC.dma(rope_t[b][:].rearrange("p a n -> p (a n)"), c_rope[t], d_rope[b], writes=[r_rope[b]])

                load_tile(0)
                for t in range(NT):
                    b = t % 2
                    tsl = slice(t * 512, (t + 1) * 512)
                    if t + 1 < NT:
                        load_tile(t + 1)
                    for c in range(8):
                        bk, rb = bank()
                        for a in range(4):
                            op("pe", lambda e, a=a, c=c, bk=bk: e.transpose(
                                out=bk[:, a * 128:(a + 1) * 128], in_=xt[b][:, a, c * 128:(c + 1) * 128],
                                identity=ident[:]), reads=[r_xt[b], r_const], writes=[rb])
                        evac(xT[:, c, :], bk[:, :], [rb], [r_xT[c]])

                    chk("A1")

                    def proj(col0, m, rhs_t=None):
                        bk, rb = bank()
                        for c in range(8):
                            op("pe", lambda e, c=c, bk=bk: e.matmul(bk[0:m, :], lhsT=win[:, c, col0:col0 + m],
                                                                     rhs=xT[:, c, :], start=(c == 0), stop=(c == 7)),
                               reads=[r_w, r_xT[c]], writes=[rb])
                        return bk, rb

                    for m in range(3):
                        bk, rb = proj(m * 128, 128)
                        op("dve", lambda e, m=m, bk=bk: e.tensor_copy(out=cqf[:, m, :], in_=bk[:, :]),
                           reads=[rb], writes=[r_cqf[m]])
                        op("act", lambda e, m=m: e.activation(out=sq[:, m, :], in_=cqf[:, m, :], func=AF.Square),
                           reads=[r_cqf[m]], writes=[r_sq[m]])
                    chk("A1b")
                    for g, (chs, n) in enumerate((((0, 1), 256.0), ((2,), 128.0))):
                        bk, rb = bank()
                        for i, m in enumerate(chs):
                            op("pe", lambda e, m=m, bk=bk, i=i, chs=chs: e.matmul(
                                bk[:, :], lhsT=ones[:], rhs=sq[:, m, :], start=(i == 0), stop=(i == len(chs) - 1)),
                               reads=[r_sq[m], r_const], writes=[rb])
                        op("dve", lambda e, bk=bk, n=n: e.tensor_scalar(out=tmp1[:], in0=bk[:, :], scalar1=1.0 / n,
                                                                        scalar2=EPS, op0=ALU.mult, op1=ALU.add),
                           reads=[rb], writes=[r_tmp1])
                        op("act", lambda e: e.activation(out=tmp2[:], in_=tmp1[:], func=AF.Sqrt),
                           reads=[r_tmp1], writes=[r_tmp2])
                        op("dve", lambda e, g=g: e.reciprocal(out=rstd[:, g, :], in_=tmp2[:]),
                           reads=[r_tmp2], writes=[r_rstd[g]])
                        chk("A1c")
                        for m in chs:
                            op("pool", lambda e, m=m, g=g: e.tensor_tensor(out=cqn[:, m, :], in0=cqf[:, m, :],
                                                                           in1=rstd[:, g, :], op=ALU.mult),
                               reads=[r_cqf[m], r_rstd[g]], writes=[r_cqn[m]])

                    chk("A2")

                    def rope_comb(bkP, rbP, bkR, rbR, nrow, outap, rout):
                        tA, rA, tB, rB = rtmp[rtk["k"] % 2]
                        rtk["k"] += 1
                        op("dve", lambda e: e.tensor_tensor(out=tA[0:nrow, :], in0=bkP[0:nrow, :],
                                                            in1=rope_t[b][0:nrow, 0, :], op=ALU.mult),
                           reads=[rbP, r_rope[b]], writes=[rA])
                        op("dve", lambda e: e.tensor_tensor(out=tB[0:nrow, :], in0=bkR[0:nrow, :],
                                                            in1=rope_t[b][0:nrow, 1, :], op=ALU.mult),
                           reads=[rbR, r_rope[b]], writes=[rB])
                        op("pool", lambda e: e.tensor_tensor(out=outap, in0=tA[0:nrow, :], in1=tB[0:nrow, :],
                                                             op=ALU.add),
                           reads=[rA, rB], writes=[rout])

                    bkP, rbP = proj(384, 32)
                    bkR, rbR = proj(416, 32)
                    rope_comb(bkP, rbP, bkR, rbR, 32, o_kp[:, :], r_kp)

                    chk("A3")
                    for m in range(4):
                        bk, rb = proj(448 + m * 128, 128)
                        evac(o_mq[:, m, :], bk[:, :], [rb], [r_mq[m]])
                    for m in range(4):
                        bk, rb = proj(960 + m * 128, 128)
                        op("dve", lambda e, m=m, bk=bk: e.tensor_copy(out=o_mk[:, m, :], in_=bk[:, :]),
                           reads=[rb], writes=[r_mk[m]])
                        op("dve", lambda e, m=m, bk=bk: e.tensor_reduce(
                            out=kmsum[:, m, 2 * t:2 * t + 2], in_=bk[:, :].rearrange("p (b l) -> p b l", b=2),
                            op=ALU.add, axis=AX.X), reads=[rb], writes=[r_km])
                    for hp in range(2):
                        op("dve", lambda e, hp=hp: e.tensor_scalar(
                            out=kmT[64 * hp:64 * hp + 64, :, 16 * hp + 2 * t:16 * hp + 2 * t + 2],
                            in0=kmsum[64 * hp:64 * hp + 64, :, 2 * t:2 * t + 2],
                            scalar1=1.0 / 256.0, scalar2=None, op0=ALU.mult), reads=[r_km], writes=[r_km])
                    for a in range(4):
                        bk, rb = bank()
                        for c in range(8):
                            op("pe", lambda e, c=c, a=a, bk=bk: e.matmul(
                                bk[:, :], lhsT=xT[:, c, a * 128:(a + 1) * 128], rhs=win[:, c, 1472:1984],
                                start=(c == 0), stop=(c == 7)), reads=[r_w, r_xT[c]], writes=[rb])
                        evac(o_mv[:, a, :], bk[:, :], [rb], [r_mv[a]])

                    chk("A4")
                    for m in range(4):
                        bk, rb = bank()
                        for kc in range(2):
                            op("pe", lambda e, kc=kc, m=m, bk=bk: e.matmul(
                                bk[:, :], lhsT=wuq[:, kc, m * 128:(m + 1) * 128], rhs=cqn[:, kc, :],
                                start=(kc == 0), stop=(kc == 1)), reads=[r_w, r_cqn[kc]], writes=[rb])
                        evac(o_qn[:, m, :], bk[:, :], [rb], [r_qn[m]])
                    for m in range(2):
                        pr = []
                        for off in (512, 768):
                            bk, rb = bank()
                            for kc in range(2):
                                op("pe", lambda e, kc=kc, m=m, bk=bk, off=off: e.matmul(
                                    bk[:, :], lhsT=wuq[:, kc, off + m * 128:off + (m + 1) * 128], rhs=cqn[:, kc, :],
                                    start=(kc == 0), stop=(kc == 1)), reads=[r_w, r_cqn[kc]], writes=[rb])
                            pr.append((bk, rb))
                        rope_comb(pr[0][0], pr[0][1], pr[1][0], pr[1][1], 128, o_qp[:, m, :], r_qp[m])
                    for m in range(4):
                        bk, rb = bank()
                        op("pe", lambda e, m=m, bk=bk: e.matmul(bk[:, :], lhsT=wukv[:, m * 128:(m + 1) * 128],
                                                                rhs=cqn[:, 2, :], start=True, stop=True),
                           reads=[r_w, r_cqn[2]], writes=[rb])
                        evac(o_kn[:, m, :], bk[:, :], [rb], [r_kn[m]])
                    for a in range(4):
                        bk, rb = bank()
                        op("pe", lambda e, a=a, bk=bk: e.matmul(bk[:, :], lhsT=cqn[:, 2, a * 128:(a + 1) * 128],
                                                                rhs=wukv[:, 512:1024], start=True, stop=True),
                           reads=[r_w, r_cqn[2]], writes=[rb])
                        evac(o_va[:, a, :], bk[:, :], [rb], [r_va[a]])

                    chk("A5")
                    bkT, rbT = bank()
                    for a in range(4):
                        own = 2 * t + a // 2
                        bk, rb = bank()
                        for m in range(4):
                            op("pe", lambda e, m=m, a=a, bk=bk: e.matmul(
                                bk[:, 32 * m:32 * m + 32], lhsT=o_mq[:, m, a * 128:(a + 1) * 128],
                                rhs=kmT[:, m, :], start=True, stop=True),
                               reads=[r_mq[m], r_km], writes=[rb])
                        op("dve", lambda e, bk=bk, own=own: e.tensor_tensor(
                            out=gm[:], in0=bk[:, 0:128].rearrange("p (h n) -> p h n", h=8),
                            in1=pastm[:, own:own + 1, :].broadcast_to([128, 8, 16]), op=ALU.add),
                           reads=[rb, r_pm], writes=[r_gm])
                        gm2 = seln[:].rearrange("p (h n) -> p h n", h=8)
                        op("dve", lambda e: e.tensor_copy(out=gm2, in_=gm[:]), reads=[r_gm], writes=[r_seln])
                        for rnd in range(3):
                            op("dve", lambda e: e.tensor_reduce(out=m8[:, :, 0:1], in_=gm2, op=ALU.max, axis=AX.X),
                               reads=[r_seln], writes=[r_m8])
                            op("dve", lambda e: e.tensor_tensor(out=m8[:, :, 1:2].broadcast_to([128, 8, 16]) if False else tmpg[:],
                                                                in0=gm2, in1=m8[:, :, 0:1].broadcast_to([128, 8, 16]),
                                                                op=ALU.is_ge), reads=[r_seln, r_m8], writes=[r_tmpg])
                            op("dve", lambda e: e.scalar_tensor_tensor(out=gm2, in0=tmpg[:], scalar=-2e30, in1=gm2,
                                                                       op0=ALU.mult, op1=ALU.add),
                               reads=[r_tmpg, r_seln], writes=[r_seln])
                        op("dve", lambda e: e.tensor_reduce(out=m8[:, :, 3:4], in_=gm2, op=ALU.max, axis=AX.X),
                           reads=[r_seln], writes=[r_m8])
                        op("dve", lambda e: e.tensor_tensor(
                            out=seln[:].rearrange("p (h n) -> p h n", h=8), in0=gm[:],
                            in1=m8[:, :, 3:4].broadcast_to([128, 8, 16]), op=ALU.is_lt),
                           reads=[r_gm, r_m8], writes=[r_seln])
                        op("pe", lambda e, a=a: e.transpose(out=bkT[:, a * 128:(a + 1) * 128], in_=seln[:],
                                                            identity=ident[:]),
                           reads=[r_seln, r_const], writes=[rbT])
                    evac(o_ms[:, :], bkT[:, :], [rbT], [r_ms])

                    chk("A6")
                    def store(dst, src, rsrc):
                        SC.dma(dst, src, d_stA, reads=(rsrc if isinstance(rsrc, list) else [rsrc]),
                               writes=[r_scrA], live=True)
                    store(sc["qn"][:, :, tsl].rearrange("m p t -> p m t"), o_qn[:], r_qn)
                    store(sc["qp"][:, :, tsl].rearrange("m p t -> p m t"), o_qp[:], r_qp)
                    store(sc["kn"][:, :, tsl].rearrange("m p t -> p m t"), o_kn[:], r_kn)
                    store(sc["kp"][:, tsl], o_kp[:], r_kp)
                    store(sc["va"][tsl, :].rearrange("(a p) c -> p a c", p=128), o_va[:], r_va)
                    store(sc["mq"][:, :, tsl].rearrange("m p t -> p m t"), o_mq[:], r_mq)
                    store(sc["mk"][:, :, tsl].rearrange("m p t -> p m t"), o_mk[:], r_mk)
                    store(sc["mv"][tsl, :].rearrange("(a p) c -> p a c", p=128), o_mv[:], r_mv)
                    store(sc["ms"][:, tsl], o_ms[:], r_ms)
                    if s == 0:
                        for _ in range(per_tile):
                            pro_job()
                SC.flush()

            if stop == "A":
                return nc
            with ExitStack() as st:
                QT = [sb(st, f"QT{i}", [128, S], BF16) for i in range(2)]
                KT = [sb(st, f"KT{i}", [128, S], BF16) for i in range(2)]
                VA = [sb(st, f"VA{i}", [128, NKT, 128], BF16) for i in range(2)]
                r_QT = [Res(), Res()]
                r_KT = [Res(), Res()]
                r_VA = [Res(), Res()]
                d_QT = [SC.dsem(), SC.dsem()]
                d_KT = [SC.dsem(), SC.dsem()]
                d_VA = [SC.dsem(), SC.dsem()]
                PT = [sb(st, f"PT{i}", [128, 512], BF16) for i in range(4)]
                r_PT = [Res() for _ in range(4)]
                aT = [sb(st, f"aT{i}", [128, S], BF16) for i in range(2)]
                r_aT = [Res(), Res()]
                rd = sb(st, "rd", [128, 512], F32)
                r_rd = Res()
                cmask = sb(st, "cmask", [128, 4, 512], BF16)
                abias = sb(st, "abias", [128, 8, 35], F32)
                r_cB = Res()
                d_cB = SC.dsem()
                SC.dma(cmask[:].rearrange("p a n -> p (a n)"), c_cmask, d_cB, writes=[r_cB])
                SC.dma(abias[:].rearrange("p a n -> p (a n)"), c_abias, d_cB, writes=[r_cB], nodeps=True)
                op("pool", lambda e: e.memset(VA[0][:, :, 64:128], 1.0), writes=[r_VA[0]])
                op("pool", lambda e: e.memset(VA[1][:, :, 0:64], 1.0), writes=[r_VA[1]])

                sbank = [(banks[i], bres[i]) for i in range(4)]
                obank = [(banks[4 + i], bres[4 + i]) for i in range(2)]

                def load_head(hh):
                    b = hh % 2
                    h = hh % 8
                    if hh < 8:
                        R = 96
                        SC.dma(QT[b][0:64, :], sc["qn"][h // 2, 64 * (h % 2):64 * (h % 2) + 64, :], d_QT[b],
                               reads=[r_scrA], writes=[r_QT[b]])
                        SC.dma(QT[b][64:96, :], sc["qp"][h // 4, 32 * (h % 4):32 * (h % 4) + 32, :], d_QT[b],
                               reads=[r_scrA], writes=[r_QT[b]], nodeps=True)
                        SC.dma(KT[b][0:64, :], sc["kn"][h // 2, 64 * (h % 2):64 * (h % 2) + 64, :], d_KT[b],
                               reads=[r_scrA], writes=[r_KT[b]])
                        SC.dma(KT[b][64:96, :], sc["kp"][:, :], d_KT[b], reads=[r_scrA], writes=[r_KT[b]], nodeps=True)
                        vsrc = sc["va"]
                    else:
                        SC.dma(QT[b][0:64, :], sc["mq"][h // 2, 64 * (h % 2):64 * (h % 2) + 64, :], d_QT[b],
                               reads=[r_scrA], writes=[r_QT[b]])
                        SC.dma(QT[b][64:66, :], c_qalibi[h], d_QT[b], writes=[r_QT[b]], nodeps=True)
                        SC.dma(QT[b][66:82, :], sc["ms"][16 * h:16 * h + 16, :], d_QT[b], writes=[r_QT[b]], nodeps=True)
                        SC.dma(KT[b][0:64, :], sc["mk"][h // 2, 64 * (h % 2):64 * (h % 2) + 64, :], d_KT[b],
                               reads=[r_scrA], writes=[r_KT[b]])
                        SC.dma(KT[b][64:82, :], c_kconst, d_KT[b], writes=[r_KT[b]], nodeps=True)
                        vsrc = sc["mv"]
                    c0 = 0 if b == 0 else 64
                    for k0 in range(0, NKT, 8):
                        SC.dma(VA[b][:, k0:k0 + 8, c0:c0 + 64],
                               vsrc[k0 * 128:(k0 + 8) * 128, 64 * h:64 * h + 64].rearrange("(k p) d -> p k d", p=128),
                               d_VA[b], reads=[r_scrA], writes=[r_VA[b]], nodeps=(k0 > 0))

                steps = []
                for hh in range(16):
                    for j in range(NT):
                        nk = 4 * j + 4
                        for kt in range(nk):
                            steps.append((hh, j, kt, kt == 0, kt == nk - 1))

                def emit_score(i):
                    hh, j, kt, first, last = steps[i]
                    b = hh % 2
                    R = 96 if hh < 8 else 82
                    r = kt - 4 * j
                    q0 = 128 * r if r > 0 else 0
                    bk, rb = sbank[i % 4]
                    diag = r >= 0
                    op("pe", lambda e: e.matmul(bk[:, q0:512], lhsT=KT[b][0:R, kt * 128:(kt + 1) * 128],
                                                rhs=QT[b][0:R, j * 512 + q0:(j + 1) * 512], start=True, stop=not diag),
                       reads=[r_KT[b], r_QT[b]], writes=[rb])
                    if diag:
                        op("pe", lambda e: e.matmul(bk[:, q0:512], lhsT=identb[:], rhs=cmask[:, r, q0:512],
                                                    start=False, stop=True), reads=[r_cB, r_const], writes=[rb])

                def emit_rest(i):
                    hh, j, kt, first, last = steps[i]
                    b = hh % 2
                    h = hh % 8
                    r = kt - 4 * j
                    q0 = 128 * r if r > 0 else 0
                    bk, rb = sbank[i % 4]
                    pt, rpt = PT[i % 4], r_PT[i % 4]
                    ob, rob = obank[(hh * NT + j) % 2]
                    if hh < 8:
                        op("act", lambda e: e.activation(out=pt[:, q0:512], in_=bk[:, q0:512], func=AF.Exp,
                                                         scale=SC_MLA), reads=[rb], writes=[rpt])
                    else:
                        di = (512 * j - 128 * kt + 384) // 128
                        op("act", lambda e: e.activation(out=pt[:, q0:512], in_=bk[:, q0:512], func=AF.Exp,
                                                         scale=SC_MOBA, bias=abias[:, h, di:di + 1]),
                           reads=[rb, r_cB], writes=[rpt])
                    op("pe", lambda e: e.matmul(ob[:, q0:512], lhsT=VA[b][:, kt, :], rhs=pt[:, q0:512],
                                                start=first, stop=last), reads=[r_VA[b], rpt], writes=[rob])
                    if last:
                        u0, d0 = (0, 64) if b == 0 else (64, 0)
                        pair = hh // 2
                        ab = pair % 2
                        op("dve", lambda e: e.reciprocal(out=rd[u0:u0 + 64, :], in_=ob[d0:d0 + 64, :]),
                           reads=[rob], writes=[r_rd])
                        op("dve", lambda e: e.tensor_tensor(out=aT[ab][u0:u0 + 64, j * 512:(j + 1) * 512],
                                                            in0=ob[u0:u0 + 64, :], in1=rd[u0:u0 + 64, :], op=ALU.mult),
                           reads=[rob, r_rd], writes=[r_aT[ab]])
                        if j == NT - 1 and b == 1:
                            SC.dma(sc["at"][pair], aT[ab][:], d_stB, reads=[r_aT[ab]], writes=[r_scrB], live=True)

                load_head(0)
                LOOK = 3
                nst = len(steps)
                for i in range(min(LOOK, nst)):
                    emit_score(i)
                for i in range(nst):
                    hh, j, kt, first, last = steps[i]
                    if first and j == 0 and hh + 1 < 16:
                        load_head(hh + 1)
                    if i + LOOK < nst:
                        emit_score(i + LOOK)
                    emit_rest(i)
                SC.flush()

            if stop == "B":
                return nc
            with ExitStack() as st:
                wo = sb(st, "wo", [128, 8, 1024], BF16)
                wdn = sb(st, "wdn", [128, NPAIR, 1024], BF16)
                lnt = sb(st, "lnt", [128, 4, 1024], F32)
                cvp = sb(st, "cvp", [128, 44, 4], F32)
                r_cw = Res()
                d_cw = SC.dsem()
                SC.dma(wo[:].rearrange("p a n -> p (a n)"), wo_s, d_cw, reads=[r_wscr], writes=[r_cw])
                SC.dma(wdn[:].rearrange("p a n -> p (a n)"), wdn_s, d_cw, writes=[r_cw], nodeps=True)
                SC.dma(lnt[:].rearrange("p a n -> p (a n)"), lnp, d_cw, writes=[r_cw], nodeps=True)
                SC.dma(cvp[:].rearrange("p a n -> p (a n)"), convp, d_cw, writes=[r_cw], nodeps=True)
                aTt = sb(st, "aTt", [128, 8, 512], BF16)
                r_aTt = Res()
                d_aTt = SC.dsem()
                xr = [sb(st, f"xr{i}", [128, D], F32) for i in range(2)]
                r_xr = [Res(), Res()]
                d_xr = [SC.dsem(), SC.dsem()]
                ybs = [sb(st, f"yb{i}", [128, D], F32) for i in range(2)]
                r_ybs = [Res(), Res()]
                lnk = {"k": 0}
                x1f2 = [sb(st, f"x1f{i}", [128, 4, D], F32) for i in range(2)]
                r_x1f2 = [[Res() for _ in range(4)] for _ in range(2)]
                x1T = sb(st, "x1T", [128, 8, 512], BF16)
                r_x1T = [Res() for _ in range(8)]
                statss = [sb(st, f"stats{i}", [128, 2, 6], F32) for i in range(2)]
                mvs = [sb(st, f"mv_{i}", [128, 2], F32) for i in range(2)]
                lnss = [sb(st, f"lns{i}", [128, 4], F32) for i in range(2)]
                r_lns = [Res(), Res()]
                wupt = [sb(st, f"wupt{i}", [128, 8, 256], BF16) for i in range(3)]
                r_wupt = [Res() for _ in range(3)]
                d_wupt = [SC.dsem() for _ in range(3)]
                hraw = [sb(st, f"hraw{i}", [128, 514], F32) for i in range(4)]
                r_hraw = [Res() for _ in range(4)]
                cacc = [sb(st, f"cacc{i}", [128, 512], F32) for i in range(4)]
                r_cacc = [Res() for _ in range(4)]
                sgs = [sb(st, f"sg{i}", [128, 512], F32) for i in range(2)]
                r_sgs = [Res(), Res()]
                carry = sb(st, "carry", [128, 44, 2], F32)
                r_carry = Res()
                actT = sb(st, "actT", [128, NPAIR, 512], BF16)
                r_actT = [Res() for _ in range(NPAIR)]
                ot = [sb(st, f"ot{i}", [128, D], F32) for i in range(2)]
                r_ot = [Res(), Res()]
                op("pool", lambda e: e.memset(carry[:], 0.0), writes=[r_carry])

                def layer_norm(k, gi, dst, rdst):
                    src, rsrc = ybs[k], r_ybs[k]
                    stats, mv_, lns, r_ln = statss[k], mvs[k], lnss[k], r_lns[k]
                    for hf in range(2):
                        op("dve", lambda e, hf=hf: e.bn_stats(out=stats[:, hf, :], in_=src[:, hf * 512:(hf + 1) * 512]),
                           reads=[rsrc], writes=[r_ln])
                    op("dve", lambda e: e.bn_aggr(out=mv_[:], in_=stats[:].rearrange("p a n -> p (a n)")),
                       reads=[r_ln], writes=[r_ln])
                    op("dve", lambda e: e.tensor_scalar(out=lns[:, 0:1], in0=mv_[:, 1:2], scalar1=EPS, scalar2=None,
                                                        op0=ALU.add), reads=[r_ln], writes=[r_ln])
                    op("act", lambda e: e.activation(out=lns[:, 1:2], in_=lns[:, 0:1], func=AF.Sqrt),
                       reads=[r_ln], writes=[r_ln])
                    op("dve", lambda e: e.reciprocal(out=lns[:, 2:3], in_=lns[:, 1:2]), reads=[r_ln], writes=[r_ln])
                    op("dve", lambda e: e.tensor_scalar(out=lns[:, 3:4], in0=mv_[:, 0:1], scalar1=-1.0,
                                                        scalar2=lns[:, 2:3], op0=ALU.mult, op1=ALU.mult),
                       reads=[r_ln], writes=[r_ln])
                    op("act", lambda e: e.activation(out=src[:], in_=src[:], func=AF.Identity, scale=lns[:, 2:3],
                                                     bias=lns[:, 3:4]), reads=[rsrc, r_ln], writes=[rsrc])
                    op("pool", lambda e: e.tensor_tensor(out=src[:], in0=src[:], in1=lnt[:, gi, :], op=ALU.mult),
                       reads=[rsrc, r_cw], writes=[rsrc])
                    op("pool", lambda e: e.tensor_tensor(out=dst, in0=src[:], in1=lnt[:, gi + 1, :], op=ALU.add),
                       reads=[rsrc, r_cw], writes=[rdst])

                def load_x(t, a):
                    k = (t * 4 + a) % 2
                    SC.dma(xr[k][:], x[s, t * 512 + a * 128:t * 512 + (a + 1) * 128, :], d_xr[k], writes=[r_xr[k]])

                wk = {"k": 0}

                def load_wup(p):
                    k = wk["k"] % 3
                    wk["k"] += 1
                    SC.dma(wupt[k][:].rearrange("p a n -> p (a n)"), wup_s[p], d_wupt[k], reads=[r_wscr],
                           writes=[r_wupt[k]])
                    return k

                wqd = {}

                def stage_wo(t):
                    tsl = slice(t * 512, (t + 1) * 512)
                    SC.dma(aTt[:], sc["at"][:, :, tsl].rearrange("c p t -> p c t"), d_aTt, reads=[r_scrB],
                           writes=[r_aTt])
                    load_x(t, 0)
                    wqd[t] = [load_wup(0), load_wup(1)]
                    for a in range(4):
                        k = (t * 4 + a) % 2
                        if a + 1 < 4:
                            load_x(t, a + 1)
                        bks = []
                        for n in range(2):
                            bk, rb = bank()
                            for c in range(8):
                                op("pe", lambda e, c=c, n=n, bk=bk: e.matmul(
                                    bk[:, :], lhsT=aTt[:, c, a * 128:(a + 1) * 128], rhs=wo[:, c, n * 512:(n + 1) * 512],
                                    start=(c == 0), stop=(c == 7)), reads=[r_aTt, r_cw], writes=[rb])
                            bks.append((bk, rb))
                        ky = lnk["k"] % 2
                        lnk["k"] += 1
                        for n in range(2):
                            bk, rb = bks[n]
                            op("dve", lambda e, n=n, bk=bk: e.scalar_tensor_tensor(
                                out=ybs[ky][:, n * 512:(n + 1) * 512], in0=xr[k][:, n * 512:(n + 1) * 512], scalar=ALPHA,
                                in1=bk[:, :], op0=ALU.mult, op1=ALU.add), reads=[r_xr[k], rb], writes=[r_ybs[ky]])
                        layer_norm(ky, 0, x1f2[t % 2][:, a, :], r_x1f2[t % 2][a])

                def stage_tr(t):
                    for c in range(8):
                        bk, rb = bank()
                        for a in range(4):
                            op("pe", lambda e, a=a, c=c, bk=bk: e.transpose(
                                out=bk[:, a * 128:(a + 1) * 128], in_=x1f2[t % 2][:, a, c * 128:(c + 1) * 128],
                                identity=ident[:]), reads=[r_x1f2[t % 2][a], r_const], writes=[rb])
                        if c % 2 == 0:
                            op("act", lambda e, c=c, bk=bk: e.activation(out=x1T[:, c, :], in_=bk[:, :], func=AF.Copy),
                               reads=[rb], writes=[r_x1T[c]])
                        else:
                            op("dve", lambda e, c=c, bk=bk: e.tensor_copy(out=x1T[:, c, :], in_=bk[:, :]),
                               reads=[rb], writes=[r_x1T[c]])

                def stage_p1(t):
                    wq = wqd.pop(t)
                    def gate_mul(p):
                        bg, bu = 2 * (p % 2), 2 * (p % 2) + 1
                        sg, r_sg = sgs[p % 2], r_sgs[p % 2]
                        op("act", lambda e: e.activation(out=sg[:], in_=cacc[bg][:], func=AF.Silu),
                           reads=[r_cacc[bg]], writes=[r_sg])
                        op("pool", lambda e: e.tensor_tensor(out=actT[:, p, :], in0=sg[:], in1=cacc[bu][:], op=ALU.mult),
                           reads=[r_sg, r_cacc[bu]], writes=[r_actT[p]])

                    for p in range(NPAIR):
                        kw = wq.pop(0)
                        if p + 2 < NPAIR:
                            wq.append(load_wup(p + 2))
                        for gu in range(2):
                            ch = p + 22 * gu
                            bk, rb = bank()
                            for c in range(8):
                                op("pe", lambda e, c=c, bk=bk, gu=gu: e.matmul(
                                    bk[:, :], lhsT=wupt[kw][:, c, gu * 128:(gu + 1) * 128], rhs=x1T[:, c, :],
                                    start=(c == 0), stop=(c == 7)), reads=[r_wupt[kw], r_x1T[c]], writes=[rb])
                            bi = 2 * (p % 2) + gu
                            hr, rhr = hraw[bi], r_hraw[bi]
                            ca, rca = cacc[bi], r_cacc[bi]
                            op("pool", lambda e, hr=hr, ch=ch: e.tensor_copy(out=hr[:, 0:2], in_=carry[:, ch, :]),
                               reads=[r_carry], writes=[rhr])
                            op("act", lambda e, hr=hr, bk=bk: e.activation(out=hr[:, 2:514], in_=bk[:, :], func=AF.Copy),
                               reads=[rb], writes=[rhr])
                            op("act", lambda e, ca=ca, bk=bk, ch=ch: e.activation(
                                out=ca[:], in_=bk[:, :], func=AF.Identity, scale=cvp[:, ch, 2:3], bias=cvp[:, ch, 3:4]),
                               reads=[rb, r_cw], writes=[rca])
                            op("pool", lambda e, hr=hr, ch=ch: e.tensor_copy(out=carry[:, ch, :], in_=hr[:, 512:514]),
                               reads=[rhr], writes=[r_carry])
                            op("dve", lambda e, ca=ca, hr=hr, ch=ch: e.scalar_tensor_tensor(
                                out=ca[:], in0=hr[:, 1:513], scalar=cvp[:, ch, 1:2], in1=ca[:],
                                op0=ALU.mult, op1=ALU.add), reads=[rhr, rca, r_cw], writes=[rca])
                            op("dve", lambda e, ca=ca, hr=hr, ch=ch: e.scalar_tensor_tensor(
                                out=ca[:], in0=hr[:, 0:512], scalar=cvp[:, ch, 0:1], in1=ca[:],
                                op0=ALU.mult, op1=ALU.add), reads=[rhr, rca, r_cw], writes=[rca])
                        if p >= 1:
                            gate_mul(p - 1)
                    gate_mul(NPAIR - 1)

                def stage_p2(t):
                    for a in range(4):
                        ko = (t * 4 + a) % 2
                        bks = []
                        for n in range(2):
                            bk, rb = bank()
                            for p in range(NPAIR):
                                op("pe", lambda e, p=p, n=n, bk=bk: e.matmul(
                                    bk[:, :], lhsT=actT[:, p, a * 128:(a + 1) * 128], rhs=wdn[:, p, n * 512:(n + 1) * 512],
                                    start=(p == 0), stop=(p == NPAIR - 1)), reads=[r_actT[p], r_cw], writes=[rb])
                            bks.append((bk, rb))
                        ky = lnk["k"] % 2
                        lnk["k"] += 1
                        for n in range(2):
                            bk, rb = bks[n]
                            op("dve", lambda e, n=n, bk=bk: e.scalar_tensor_tensor(
                                out=ybs[ky][:, n * 512:(n + 1) * 512], in0=x1f2[t % 2][:, a, n * 512:(n + 1) * 512], scalar=ALPHA,
                                in1=bk[:, :], op0=ALU.mult, op1=ALU.add), reads=[r_x1f2[t % 2][a], rb], writes=[r_ybs[ky]])
                        layer_norm(ky, 2, ot[ko][:], r_ot[ko])
                        SC.dma(out[s, t * 512 + a * 128:t * 512 + (a + 1) * 128, :], ot[ko][:], d_out,
                               reads=[r_ot[ko]], writes=[r_out], live=True, q="pool")

                stage_wo(0)
                stage_tr(0)
                for t in range(NT):
                    stage_p1(t)
                    if t + 1 < NT:
                        stage_wo(t + 1)
                    stage_p2(t)
                    if t + 1 < NT:
                        stage_tr(t + 1)
                SC.flush()

        SC._wait("sp", ("d", d_out, d_out.cnt))
        SC.flush()
    return nc


def _bf(a):
    return np.ascontiguousarray(a.astype(ml_dtypes.bfloat16))


def make_consts(S):
    NT = S // 512
    c = {}
    c["c_ident"] = np.eye(128, dtype=np.float32)
    c["c_identb"] = _bf(np.eye(128, dtype=np.float32))
    k = np.arange(128)[:, None, None]
    r = np.arange(4)[None, :, None]
    q = np.arange(512)[None, None, :]
    c["c_cmask"] = _bf(np.where(128 * r + k > q, NEGBIG, 0.0).astype(np.float32).reshape(128, 2048))
    half = 16
    inv = (np.float32(10000.0) ** (-np.arange(half, dtype=np.float32) / np.float32(half))).astype(np.float32)
    pos = np.arange(S, dtype=np.float32)
    ang = (pos[:, None] * inv[None, :]).astype(np.float32)
    cos = np.cos(ang).astype(np.float32)
    sin = np.sin(ang).astype(np.float32)
    d = np.arange(128) % 32
    cosT = cos[:, d % 16].T
    sinT = sin[:, d % 16].T * np.where(d < 16, -1.0, 1.0)[:, None]
    rope = np.stack([cosT.reshape(128, NT, 512), sinT.reshape(128, NT, 512)], axis=2)
    c["c_rope"] = np.ascontiguousarray(rope.transpose(1, 0, 2, 3).reshape(NT, 128, 1024).astype(np.float32))
    kc = np.zeros((18, S), np.float32)
    kc[0:2] = 1.0
    blk = np.arange(S) // 256
    for n in range(16):
        kc[2 + n] = np.where(blk == n, NEGBIG, 0.0)
    c["c_kconst"] = _bf(kc)
    slopes = (2.0 ** (-8.0 * np.arange(1, 9, dtype=np.float32) / 8.0)).astype(np.float32)
    dq = (np.arange(S) % 512).astype(np.float32)
    v = (-slopes[:, None] * dq[None, :] / np.float32(SC_MOBA)).astype(np.float32)
    hi = v.astype(ml_dtypes.bfloat16)
    lo = (v - hi.astype(np.float32)).astype(ml_dtypes.bfloat16)
    c["c_qalibi"] = np.ascontiguousarray(np.stack([hi, lo], axis=1))
    i = np.arange(35)
    Dd = (128 * i - 384).astype(np.float32)
    kk = np.arange(128, dtype=np.float32)
    ab = -slopes[None, :, None] * (Dd[None, None, :] - kk[:, None, None])
    c["c_abias"] = np.ascontiguousarray(ab.astype(np.float32).reshape(128, 8 * 35))
    own = np.arange(16)[:, None]
    n = np.arange(16)[None, :]
    pm = np.where(n < own, 0.0, np.where(n == own, 1e30, -1e30)).astype(np.float32)
    c["c_pastm"] = np.ascontiguousarray(np.broadcast_to(pm.reshape(1, 256), (128, 256)))
    return c


def prep_weights(w_in, q_norm_g, w_uq, kv_norm_g, w_ukv, w_o, ln1_g, ln1_b, w_up, conv_w, conv_b, w_down,
                 ln2_g, ln2_b):
    f = lambda a: np.ascontiguousarray(np.asarray(a, dtype=np.float32))
    w_in, w_uq, w_ukv, w_o, w_up, w_down = (f(a[0]) for a in (w_in, w_uq, w_ukv, w_o, w_up, w_down))
    r = np.arange(32)
    kr_sw = w_in[:, 384 + ((r + 16) % 32)]
    win = np.concatenate([w_in[:, 0:416], kr_sw, w_in[:, 416:1952]], axis=1)
    win = win.reshape(8, 128, WIN_COLS).transpose(1, 0, 2).reshape(128, 8 * WIN_COLS)
    h = np.arange(8)
    nope = (h[:, None] * 96 + np.arange(64)[None, :]).reshape(-1)
    pe = (h[:, None] * 96 + 64 + r[None, :]).reshape(-1)
    pesw = (h[:, None] * 96 + 64 + ((r + 16) % 32)[None, :]).reshape(-1)
    wuq = w_uq[:, np.concatenate([nope, pe, pesw])]
    wuq = wuq.reshape(2, 128, 1024).transpose(1, 0, 2).reshape(128, 2048)
    kcols = (h[:, None] * 128 + np.arange(64)[None, :]).reshape(-1)
    vcols = kcols + 64
    wukv = w_ukv[:, np.concatenate([kcols, vcols])]
    wo = w_o.reshape(8, 128, 1024).transpose(1, 0, 2).reshape(128, 8192)
    wdn = w_down.reshape(NPAIR, 128, 1024).transpose(1, 0, 2).reshape(128, NPAIR * 1024)
    wu = w_up.reshape(8, 128, 2, NPAIR, 128)
    wu = wu.transpose(3, 1, 0, 2, 4).reshape(NPAIR, 128, 8 * 256)
    lnp = np.stack([f(ln1_g[0]), f(ln1_b[0]), f(ln2_g[0]), f(ln2_b[0])], axis=0).reshape(1, 4096)
    lnp = np.broadcast_to(lnp, (128, 4096))
    cw = f(conv_w[0])
    cb = f(conv_b[0])
    cv = np.concatenate([cw, cb[None, :]], axis=0)
    cv = cv.reshape(4, 44, 128).transpose(2, 1, 0).reshape(128, 176)
    return {
        "w_in": f(win), "w_uq": f(wuq), "w_ukv": f(wukv), "w_o": f(wo), "w_dn": f(wdn), "w_up": f(wu),
        "qg": f(f(q_norm_g[0]).reshape(2, 128).T), "kvg": f(f(kv_norm_g[0]).reshape(128, 1)),
        "lnp": f(lnp), "convp": f(cv),
    }


_NC_CACHE = {}


def run(x, params, ncores, debug=False, stop=None):
    B, S, _ = x.shape
    nseq = B // ncores
    key = (nseq, S, debug)
    if key not in _NC_CACHE:
        _NC_CACHE[key] = build(nseq, S, debug, stop)
    nc = _NC_CACHE[key]
    shared = dict(prep_weights(**params))
    shared.update(make_consts(S))
    xs = np.ascontiguousarray(np.asarray(x, dtype=np.float32)).reshape(ncores, nseq, S, D)
    in_maps = [dict(shared, x=xs[i]) for i in range(ncores)]
    res = run_bass_kernel_spmd(nc, in_maps, core_ids=list(range(ncores)))
    return res


def kernel(x, w_in, q_norm_g, w_uq, kv_norm_g, w_ukv, w_o, ln1_g, ln1_b, w_up, conv_w, conv_b, w_down,
           ln2_g, ln2_b):
    params = dict(w_in=w_in, q_norm_g=q_norm_g, w_uq=w_uq, kv_norm_g=kv_norm_g, w_ukv=w_ukv, w_o=w_o,
                  ln1_g=ln1_g, ln1_b=ln1_b, w_up=w_up, conv_w=conv_w, conv_b=conv_b, w_down=w_down,
                  ln2_g=ln2_g, ln2_b=ln2_b)
    x = np.asarray(x)
    res = run(x, params, NCORES)
    outs = [np.asarray(r["out"]) for r in res.results]
    return np.concatenate(outs, axis=0).astype(np.float32)
```

```python
import math
from contextlib import ExitStack

import numpy as np
import ml_dtypes
import concourse.bass as bass
import concourse.mybir as mybir
from concourse.bass_utils import run_bass_kernel_spmd

F32 = mybir.dt.float32
BF16 = mybir.dt.bfloat16
AF = mybir.ActivationFunctionType
ALU = mybir.AluOpType
AX = mybir.AxisListType

D = 1024
NCORES = 8
EPS = 1e-5
NEGBIG = -30000.0
ALPHA = 2.0 ** 0.25
SC_MLA = 96.0 ** -0.5
SC_MOBA = 0.125
DFF = 2816
NPAIR = 22
WIN_COLS = 1984
import os as _os
ALL_INC = _os.environ.get("ALL_INC", "0") == "1"


class Res:
    __slots__ = ("w", "r", "scratch")

    def __init__(self, scratch=False):
        self.w = None
        self.r = []
        self.scratch = scratch


class DSem:
    def __init__(self, sem):
        self.sem = sem
        self.cnt = 0


class _Rec:
    def __getattr__(self, name):
        def f(*a, **k):
            self.call = (name, a, k)
        return f


class Sched:
    ENGS = ("pe", "act", "dve", "pool", "sp")
    CENGS = ("pe", "act", "dve", "pool")

    def __init__(self, nc, stack):
        self.nc = nc
        self.stack = stack
        self.prog = {e: [] for e in self.ENGS}
        self.sem = {}
        self.cnt = {}
        self.targets = {e: set() for e in self.CENGS}
        for e in self.CENGS:
            self.sem[e] = stack.enter_context(nc.semaphore("s_" + e))
            self.cnt[e] = 0
        self.seen = {e: {} for e in self.ENGS}
        self.dsems = []
        self.rank = {e: {} for e in self.CENGS}
        self.flushed = {e: 0 for e in self.CENGS}

    def dsem(self):
        s = self.stack.enter_context(self.nc.semaphore(f"d{len(self.dsems)}"))
        d = DSem(s)
        self.dsems.append(d)
        return d

    def _wait(self, eng, ev):
        if ev is None:
            return
        kind, obj, val = ev
        if kind == "e":
            if obj == eng and eng == "pe":
                return
            key = obj
        else:
            key = id(obj)
            if val is None:
                val = obj.cnt
        if val <= 0 or self.seen[eng].get(key, 0) >= val:
            return
        self.seen[eng][key] = val
        if kind == "e":
            assert val > self.flushed[obj] or val in self.rank[obj], (eng, obj, val)
            self.targets[obj].add(val)
        self.prog[eng].append(("w", kind, obj, val))

    def _deps(self, eng, reads, writes):
        for r in reads:
            self._wait(eng, r.w)
        for w in writes:
            if w.scratch:
                continue
            self._wait(eng, w.w)
            for ev in w.r:
                self._wait(eng, ev)

    def _commit(self, ev, reads, writes):
        for r in reads:
            if not r.scratch:
                r.r.append(ev)
        for w in writes:
            w.w = ev
            w.r = []

    def op(self, eng, fn, reads=(), writes=()):
        self._deps(eng, reads, writes)
        self.cnt[eng] += 1
        ev = ("e", eng, self.cnt[eng])
        rec = _Rec()
        fn(rec)
        if ALL_INC:
            self.targets[eng].add(self.cnt[eng])
        self.prog[eng].append(("i", rec.call, self.cnt[eng]))
        self._commit(ev, reads, writes)
        return ev

    def dma(self, out, in_, ds, reads=(), writes=(), live=False, nodeps=False, q="sp"):
        if not nodeps:
            self._deps(q, reads, writes)
        ds.cnt += 16
        assert ds.cnt < 65000
        ev = ("d", ds, None if live else ds.cnt)
        self.prog[q].append(("d", out, in_, ds.sem))
        self._commit(ev, reads, writes)
        return ev

    def barrier(self):
        for e in self.ENGS:
            for o in self.CENGS:
                self._wait(e, ("e", o, self.cnt[o]))
            for d in self.dsems:
                self._wait(e, ("d", d, d.cnt))

    def flush(self):
        self.barrier()
        rank = self.rank
        for e in self.CENGS:
            new = sorted(v for v in self.targets[e] if v not in rank[e])
            base = len(rank[e])
            for i, v in enumerate(new):
                assert v > self.flushed[e]
                rank[e][v] = base + i + 1
            assert len(rank[e]) < 65000, (e, len(rank[e]))

        def replay(name):
            def body(eng):
                for it in self.prog[name]:
                    if it[0] == "w":
                        _, kind, obj, val = it
                        if kind == "e":
                            eng.wait_ge(self.sem[obj], rank[obj][val])
                        else:
                            eng.wait_ge(obj.sem, val)
                    elif it[0] == "i":
                        nm, a, k = it[1]
                        ins = getattr(eng, nm)(*a, **k)
                        if it[2] in rank[name]:
                            ins.then_inc(self.sem[name], 1)
                    else:
                        eng.dma_start(out=it[1], in_=it[2]).then_inc(it[3], 16)
            return body

        with self.nc.Block() as block:
            block.tensor(replay("pe"))
            block.scalar(replay("act"))
            block.vector(replay("dve"))
            block.gpsimd(replay("pool"))
            block.sync(replay("sp"))
        for e in self.ENGS:
            self.prog[e] = []
        for e in self.CENGS:
            self.flushed[e] = self.cnt[e]


class _Stop(Exception):
    pass


def build(NSEQ, S, debug=False, stop=None):
    st_ = {}
    try:
        return _build(NSEQ, S, debug, stop, st_)
    except _Stop:
        return st_["nc"]


def _build(NSEQ, S, debug, stop, st_):
    assert S % 512 == 0
    NT = S // 512
    NKT = S // 128
    nc = bass.Bass("TRN2", target_bir_lowering=False)
    st_["nc"] = nc

    def din(name, shape, dt=F32):
        return nc.dram_tensor(name, list(shape), dt, kind="ExternalInput").ap()

    def dscr(name, shape, dt=BF16):
        kind = "ExternalOutput" if debug else "Internal"
        return nc.dram_tensor(name, list(shape), dt, kind=kind).ap()

    x = din("x", [NSEQ, S, D])
    out = nc.dram_tensor("out", [NSEQ, S, D], F32, kind="ExternalOutput").ap()
    w_in = din("w_in", [128, 8 * WIN_COLS])
    w_uq = din("w_uq", [128, 2 * 1024])
    w_ukv = din("w_ukv", [128, 1024])
    qg = din("qg", [128, 2])
    kvg = din("kvg", [128, 1])
    w_o = din("w_o", [128, 8 * 1024])
    w_dn = din("w_dn", [128, NPAIR * 1024])
    w_up = din("w_up", [NPAIR, 128, 2048])
    lnp = din("lnp", [128, 4 * 1024])
    convp = din("convp", [128, 44 * 4])
    c_ident = din("c_ident", [128, 128])
    c_identb = din("c_identb", [128, 128], BF16)
    c_cmask = din("c_cmask", [128, 4 * 512], BF16)
    c_rope = din("c_rope", [NT, 128, 2 * 512])
    c_kconst = din("c_kconst", [18, S], BF16)
    c_qalibi = din("c_qalibi", [8, 2, S], BF16)
    c_abias = din("c_abias", [128, 8 * 35])
    c_pastm = din("c_pastm", [128, 16 * 16])

    wo_s = dscr("wo_s", [128, 8 * 1024])
    wdn_s = dscr("wdn_s", [128, NPAIR * 1024])
    wup_s = dscr("wup_s", [NPAIR, 128, 2048])
    scr = []
    for s in range(NSEQ):
        scr.append(dict(
            qn=dscr(f"qn{s}", [4, 128, S]), qp=dscr(f"qp{s}", [2, 128, S]),
            kn=dscr(f"kn{s}", [4, 128, S]), kp=dscr(f"kp{s}", [32, S]),
            va=dscr(f"va{s}", [S, 512]),
            mq=dscr(f"mq{s}", [4, 128, S]), mk=dscr(f"mk{s}", [4, 128, S]),
            mv=dscr(f"mv{s}", [S, 512]), ms=dscr(f"ms{s}", [128, S]),
            at=dscr(f"at{s}", [8, 128, S]),
        ))

    with ExitStack() as top:
        SC = Sched(nc, top)
        op = SC.op
        uniq = {"n": 0}

        def sb(st, name, shape, dt):
            uniq["n"] += 1
            return st.enter_context(nc.sbuf_tensor(f"{name}_{uniq['n']}", list(shape), dt))

        banks = [top.enter_context(nc.psum_tensor(f"bank{i}", [128, 512], F32)) for i in range(8)]
        bres = [Res() for _ in range(8)]
        bstate = {"i": 0}

        def bank():
            i = bstate["i"]
            bstate["i"] = (i + 1) % 8
            return banks[i], bres[i]

        ident = sb(top, "ident", [128, 128], F32)
        identb = sb(top, "identb", [128, 128], BF16)
        ones = sb(top, "ones", [128, 128], BF16)
        r_const = Res()
        d_const = SC.dsem()
        SC.dma(ident[:], c_ident, d_const, writes=[r_const])
        SC.dma(identb[:], c_identb, d_const, writes=[r_const], nodeps=True)
        op("pool", lambda e: e.memset(ones[:], 1.0), writes=[r_const])

        d_wscr = SC.dsem()
        r_wscr = Res(scratch=True)

        def chk(tag):
            if stop == tag:
                SC.flush()
                raise _Stop()

        def make_stager(st, piece=2048):
            return dict(stgs=[sb(st, f"stg{i}", [128, piece], F32) for i in range(2)], rs=[Res(), Res()],
                        ds=[SC.dsem(), SC.dsem()], k=0, piece=piece)

        def cast_stream(sg_, src, ncols, dst_fn, piece=2048):
            for c0 in range(0, ncols, piece):
                c1 = min(ncols, c0 + piece)
                k = sg_["k"]
                sg_["k"] += 1
                b = k % 2
                SC.dma(sg_["stgs"][b][:, 0:c1 - c0], src[:, c0:c1], sg_["ds"][b], writes=[sg_["rs"][b]])
                dst_fn(c0, c1, sg_["stgs"][b][:, 0:c1 - c0], sg_["rs"][b], k)

        pro_jobs = []
        for c0 in range(0, 8 * 1024, 2048):
            pro_jobs.append((w_o[:, c0:c0 + 2048], wo_s[:, c0:c0 + 2048]))
        for c0 in range(0, NPAIR * 1024, 2048):
            pro_jobs.append((w_dn[:, c0:c0 + 2048], wdn_s[:, c0:c0 + 2048]))
        for p in range(NPAIR):
            pro_jobs.append((w_up[p], wup_s[p]))

        d_out = SC.dsem()
        r_out = Res(scratch=True)

        for s in range(NSEQ):
            sc = scr[s]
            d_stA = SC.dsem()
            r_scrA = Res(scratch=True)
            d_stB = SC.dsem()
            r_scrB = Res(scratch=True)

            with ExitStack() as st:
                win = sb(st, "win", [128, 8, WIN_COLS], BF16)
                wuq = sb(st, "wuq", [128, 2, 1024], BF16)
                wukv = sb(st, "wukv", [128, 1024], BF16)
                qg_t = sb(st, "qg_t", [128, 2], F32)
                kvg_t = sb(st, "kvg_t", [128, 1], F32)
                r_w = Res()
                d_w = SC.dsem()
                SC.dma(qg_t[:], qg, d_w, writes=[r_w])
                SC.dma(kvg_t[:], kvg, d_w, writes=[r_w], nodeps=True)
                with ExitStack() as st2:
                    winf = win[:].rearrange("p c n -> p (c n)")
                    stager2 = make_stager(st2)

                    def f_win(c0, c1, stg, rstg, k):
                        op("act" if k % 2 == 0 else "dve",
                           (lambda e: e.activation(out=winf[:, c0:c1], in_=stg, func=AF.Copy)) if k % 2 == 0 else
                           (lambda e: e.tensor_copy(out=winf[:, c0:c1], in_=stg)),
                           reads=[rstg], writes=[r_w])
                    cast_stream(stager2, w_in, 8 * WIN_COLS, f_win)

                    def f_wuq(c0, c1, stg, rstg, k):
                        kc = c0 // 1024
                        op("dve", lambda e: e.tensor_scalar(out=wuq[:, kc, :], in0=stg, scalar1=qg_t[:, kc:kc + 1],
                                                            scalar2=None, op0=ALU.mult),
                           reads=[rstg, r_w], writes=[r_w])
                    cast_stream(stager2, w_uq, 2048, f_wuq, piece=1024)

                    def f_wukv(c0, c1, stg, rstg, k):
                        op("dve", lambda e: e.tensor_scalar(out=wukv[:, :], in0=stg, scalar1=kvg_t[:, 0:1],
                                                            scalar2=None, op0=ALU.mult),
                           reads=[rstg, r_w], writes=[r_w])
                    cast_stream(stager2, w_ukv, 1024, f_wukv, piece=1024)
                    SC.flush()

                xt = [sb(st, f"xt{i}", [128, 4, D], F32) for i in range(2)]
                r_xt = [Res(), Res()]
                d_xt = [SC.dsem(), SC.dsem()]
                rope_t = [sb(st, f"rope{i}", [128, 2, 512], F32) for i in range(2)]
                r_rope = [Res(), Res()]
                d_rope = [SC.dsem(), SC.dsem()]
                xT = sb(st, "xT", [128, 8, 512], BF16)
                r_xT = [Res() for _ in range(8)]
                cqf = sb(st, "cqf", [128, 3, 512], F32)
                r_cqf = [Res() for _ in range(3)]
                sq = sb(st, "sq", [128, 3, 512], BF16)
                r_sq = [Res() for _ in range(3)]
                rstd = sb(st, "rstd", [128, 2, 512], F32)
                r_rstd = [Res(), Res()]
                cqn = sb(st, "cqn", [128, 3, 512], BF16)
                r_cqn = [Res() for _ in range(3)]
                tmp1 = sb(st, "tmp1", [128, 512], F32)
                r_tmp1 = Res()
                tmp2 = sb(st, "tmp2", [128, 512], F32)
                r_tmp2 = Res()
                rtmp = [(sb(st, f"rtA{i}", [128, 512], F32), Res(), sb(st, f"rtB{i}", [128, 512], F32), Res())
                        for i in range(2)]
                rtk = {"k": 0}
                pastm = sb(st, "pastm", [128, 16, 16], F32)
                kmsum = sb(st, "kmsum", [128, 4, 16], F32)
                kmT = sb(st, "kmT", [128, 4, 32], BF16)
                r_km = Res()
                gm = sb(st, "gm", [128, 8, 16], F32)
                r_gm = Res()
                m8 = sb(st, "m8", [128, 8, 8], F32)
                r_m8 = Res()
                selns = [sb(st, f"seln{i}", [128, 128], F32) for i in range(4)]
                r_selns = [Res() for _ in range(4)]
                tmpg = sb(st, "tmpg", [128, 8, 16], F32)
                r_tmpg = Res()
                d_pm = SC.dsem()
                r_pm = Res()
                SC.dma(pastm[:].rearrange("p a b -> p (a b)"), c_pastm, d_pm, writes=[r_pm])
                op("pool", lambda e: e.memset(kmT[:], 0.0), writes=[r_km])
                op("pool", lambda e: e.memset(kmsum[:], 0.0), writes=[r_km])

                def obuf(name, shape, n=1):
                    return sb(st, name, shape, BF16), ([Res() for _ in range(n)] if n > 1 else Res())
                o_qn, r_qn = obuf("o_qn", [128, 4, 512], 4)
                o_qp, r_qp = obuf("o_qp", [128, 2, 512], 2)
                o_kn, r_kn = obuf("o_kn", [128, 4, 512], 4)
                o_kp, r_kp = obuf("o_kp", [32, 512])
                o_va, r_va = obuf("o_va", [128, 4, 512], 4)
                o_mq, r_mq = obuf("o_mq", [128, 4, 512], 4)
                o_mk, r_mk = obuf("o_mk", [128, 4, 512], 4)
                o_mv, r_mv = obuf("o_mv", [128, 4, 512], 4)
                o_ms, r_ms = obuf("o_ms", [128, 512])

                if s == 0:
                    pstg = [sb(st, f"pstg{i}", [128, 2048], F32) for i in range(2)]
                    pstb = [sb(st, f"pstb{i}", [128, 2048], BF16) for i in range(2)]
                    r_pstg = [Res(), Res()]
                    r_pstb = [Res(), Res()]
                    d_pstg = [SC.dsem(), SC.dsem()]
                    pjk = {"k": 0}
                    per_tile = -(-len(pro_jobs) // NT)

                    def pro_job():
                        k = pjk["k"]
                        if k >= len(pro_jobs):
                            return
                        pjk["k"] += 1
                        src, dst = pro_jobs[k]
                        b = k % 2
                        SC.dma(pstg[b][:], src, d_pstg[b], writes=[r_pstg[b]])
                        op("pool", lambda e: e.tensor_copy(out=pstb[b][:], in_=pstg[b][:]),
                           reads=[r_pstg[b]], writes=[r_pstb[b]])
                        SC.dma(dst, pstb[b][:], d_wscr, reads=[r_pstb[b]], writes=[r_wscr], live=True)

                def gate_finish(tp):
                    bkT, rbT = bank()
                    for a in range(4):
                        op("pe", lambda e, a=a: e.transpose(out=bkT[:, a * 128:(a + 1) * 128], in_=selns[a][:],
                                                            identity=ident[:]),
                           reads=[r_selns[a], r_const], writes=[rbT])
                    op("act", lambda e: e.activation(out=o_ms[:, :], in_=bkT[:, :], func=AF.Copy),
                       reads=[rbT], writes=[r_ms])
                    SC.dma(sc["ms"][:, tp * 512:(tp + 1) * 512], o_ms[:], d_stA, reads=[r_ms], writes=[r_scrA],
                           live=True)

                evk = {"k": 0}

                def evac(outap, inap, reads, writes):
                    evk["k"] += 1
                    if evk["k"] % 4 != 0:
                        op("act", lambda e: e.activation(out=outap, in_=inap, func=AF.Copy), reads, writes)
                    else:
                        op("dve", lambda e: e.tensor_copy(out=outap, in_=inap), reads, writes)

                def load_tile(t):
                    b = t % 2
                    SC.dma(xt[b][:], x[s, t * 512:(t + 1) * 512, :].rearrange("(a p) d -> p a d", p=128),
                           d_xt[b], writes=[r_xt[b]])
                    SC.dma(rope_t[b][:].rearrange("p a n -> p (a n)"), c_rope[t], d_rope[b], writes=[r_rope[b]])

                load_tile(0)
                for t in range(NT):
                    b = t % 2
                    tsl = slice(t * 512, (t + 1) * 512)
                    if t + 1 < NT:
                        load_tile(t + 1)
                    for c in range(8):
                        bk, rb = bank()
                        for a in range(4):
                            op("pe", lambda e, a=a, c=c, bk=bk: e.transpose(
                                out=bk[:, a * 128:(a + 1) * 128], in_=xt[b][:, a, c * 128:(c + 1) * 128],
                                identity=ident[:]), reads=[r_xt[b], r_const], writes=[rb])
                        evac(xT[:, c, :], bk[:, :], [rb], [r_xT[c]])

                    chk("A1")

                    def proj(col0, m, rhs_t=None):
                        bk, rb = bank()
                        for c in range(8):
                            op("pe", lambda e, c=c, bk=bk: e.matmul(bk[0:m, :], lhsT=win[:, c, col0:col0 + m],
                                                                     rhs=xT[:, c, :], start=(c == 0), stop=(c == 7)),
                               reads=[r_w, r_xT[c]], writes=[rb])
                        return bk, rb

                    for m in range(3):
                        bk, rb = proj(m * 128, 128)
                        op("dve", lambda e, m=m, bk=bk: e.tensor_copy(out=cqf[:, m, :], in_=bk[:, :]),
                           reads=[rb], writes=[r_cqf[m]])
                        op("act", lambda e, m=m: e.activation(out=sq[:, m, :], in_=cqf[:, m, :], func=AF.Square),
                           reads=[r_cqf[m]], writes=[r_sq[m]])
                    chk("A1b")
                    for g, (chs, n) in enumerate((((0, 1), 256.0), ((2,), 128.0))):
                        bk, rb = bank()
                        for i, m in enumerate(chs):
                            op("pe", lambda e, m=m, bk=bk, i=i, chs=chs: e.matmul(
                                bk[:, :], lhsT=ones[:], rhs=sq[:, m, :], start=(i == 0), stop=(i == len(chs) - 1)),
                               reads=[r_sq[m], r_const], writes=[rb])
                        op("dve", lambda e, bk=bk, n=n: e.tensor_scalar(out=tmp1[:], in0=bk[:, :], scalar1=1.0 / n,
                                                                        scalar2=EPS, op0=ALU.mult, op1=ALU.add),
                           reads=[rb], writes=[r_tmp1])
                        op("act", lambda e: e.activation(out=tmp2[:], in_=tmp1[:], func=AF.Sqrt),
                           reads=[r_tmp1], writes=[r_tmp2])
                        op("dve", lambda e, g=g: e.reciprocal(out=rstd[:, g, :], in_=tmp2[:]),
                           reads=[r_tmp2], writes=[r_rstd[g]])
                        chk("A1c")
                        for m in chs:
                            op("pool", lambda e, m=m, g=g: e.tensor_tensor(out=cqn[:, m, :], in0=cqf[:, m, :],
                                                                           in1=rstd[:, g, :], op=ALU.mult),
                               reads=[r_cqf[m], r_rstd[g]], writes=[r_cqn[m]])

                    chk("A2")

                    def rope_comb(bkP, rbP, bkR, rbR, nrow, outap, rout):
                        tA, rA, tB, rB = rtmp[rtk["k"] % 2]
                        rtk["k"] += 1
                        op("dve", lambda e: e.tensor_tensor(out=tA[0:nrow, :], in0=bkP[0:nrow, :],
                                                            in1=rope_t[b][0:nrow, 0, :], op=ALU.mult),
                           reads=[rbP, r_rope[b]], writes=[rA])
                        op("dve", lambda e: e.tensor_tensor(out=tB[0:nrow, :], in0=bkR[0:nrow, :],
                                                            in1=rope_t[b][0:nrow, 1, :], op=ALU.mult),
                           reads=[rbR, r_rope[b]], writes=[rB])
                        op("pool", lambda e: e.tensor_tensor(out=outap, in0=tA[0:nrow, :], in1=tB[0:nrow, :],
                                                             op=ALU.add),
                           reads=[rA, rB], writes=[rout])

                    bkP, rbP = proj(384, 32)
                    bkR, rbR = proj(416, 32)
                    rope_comb(bkP, rbP, bkR, rbR, 32, o_kp[:, :], r_kp)

                    chk("A3")
                    for m in range(4):
                        bk, rb = proj(448 + m * 128, 128)
                        evac(o_mq[:, m, :], bk[:, :], [rb], [r_mq[m]])
                    for m in range(4):
                        bk, rb = proj(960 + m * 128, 128)
                        op("dve", lambda e, m=m, bk=bk: e.tensor_copy(out=o_mk[:, m, :], in_=bk[:, :]),
                           reads=[rb], writes=[r_mk[m]])
                        op("dve", lambda e, m=m, bk=bk: e.tensor_reduce(
                            out=kmsum[:, m, 2 * t:2 * t + 2], in_=bk[:, :].rearrange("p (b l) -> p b l", b=2),
                            op=ALU.add, axis=AX.X), reads=[rb], writes=[r_km])
                    for hp in range(2):
                        op("dve", lambda e, hp=hp: e.tensor_scalar(
                            out=kmT[64 * hp:64 * hp + 64, :, 16 * hp + 2 * t:16 * hp + 2 * t + 2],
                            in0=kmsum[64 * hp:64 * hp + 64, :, 2 * t:2 * t + 2],
                            scalar1=1.0 / 256.0, scalar2=None, op0=ALU.mult), reads=[r_km], writes=[r_km])
                    for a in range(4):
                        bk, rb = bank()
                        for c in range(8):
                            op("pe", lambda e, c=c, a=a, bk=bk: e.matmul(
                                bk[:, :], lhsT=xT[:, c, a * 128:(a + 1) * 128], rhs=win[:, c, 1472:1984],
                                start=(c == 0), stop=(c == 7)), reads=[r_w, r_xT[c]], writes=[rb])
                        evac(o_mv[:, a, :], bk[:, :], [rb], [r_mv[a]])

                    if t >= 1:
                        gate_finish(t - 1)
                    chk("A4")
                    for m in range(4):
                        bk, rb = bank()
                        for kc in range(2):
                            op("pe", lambda e, kc=kc, m=m, bk=bk: e.matmul(
                                bk[:, :], lhsT=wuq[:, kc, m * 128:(m + 1) * 128], rhs=cqn[:, kc, :],
                                start=(kc == 0), stop=(kc == 1)), reads=[r_w, r_cqn[kc]], writes=[rb])
                        evac(o_qn[:, m, :], bk[:, :], [rb], [r_qn[m]])
                    for m in range(2):
                        pr = []
                        for off in (512, 768):
                            bk, rb = bank()
                            for kc in range(2):
                                op("pe", lambda e, kc=kc, m=m, bk=bk, off=off: e.matmul(
                                    bk[:, :], lhsT=wuq[:, kc, off + m * 128:off + (m + 1) * 128], rhs=cqn[:, kc, :],
                                    start=(kc == 0), stop=(kc == 1)), reads=[r_w, r_cqn[kc]], writes=[rb])
                            pr.append((bk, rb))
                        rope_comb(pr[0][0], pr[0][1], pr[1][0], pr[1][1], 128, o_qp[:, m, :], r_qp[m])
                    for m in range(4):
                        bk, rb = bank()
                        op("pe", lambda e, m=m, bk=bk: e.matmul(bk[:, :], lhsT=wukv[:, m * 128:(m + 1) * 128],
                                                                rhs=cqn[:, 2, :], start=True, stop=True),
                           reads=[r_w, r_cqn[2]], writes=[rb])
                        evac(o_kn[:, m, :], bk[:, :], [rb], [r_kn[m]])
                    for a in range(4):
                        bk, rb = bank()
                        op("pe", lambda e, a=a, bk=bk: e.matmul(bk[:, :], lhsT=cqn[:, 2, a * 128:(a + 1) * 128],
                                                                rhs=wukv[:, 512:1024], start=True, stop=True),
                           reads=[r_w, r_cqn[2]], writes=[rb])
                        evac(o_va[:, a, :], bk[:, :], [rb], [r_va[a]])

                    chk("A5")
                    for a in range(4):
                        own = 2 * t + a // 2
                        bk, rb = bank()
                        for m in range(4):
                            op("pe", lambda e, m=m, a=a, bk=bk: e.matmul(
                                bk[:, 32 * m:32 * m + 32], lhsT=o_mq[:, m, a * 128:(a + 1) * 128],
                                rhs=kmT[:, m, :], start=True, stop=True),
                               reads=[r_mq[m], r_km], writes=[rb])
                        op("dve", lambda e, bk=bk, own=own: e.tensor_tensor(
                            out=gm[:], in0=bk[:, 0:128].rearrange("p (h n) -> p h n", h=8),
                            in1=pastm[:, own:own + 1, :].broadcast_to([128, 8, 16]), op=ALU.add),
                           reads=[rb, r_pm], writes=[r_gm])
                        seln, r_seln = selns[a], r_selns[a]
                        gm2 = seln[:].rearrange("p (h n) -> p h n", h=8)
                        op("dve", lambda e: e.tensor_copy(out=gm2, in_=gm[:]), reads=[r_gm], writes=[r_seln])
                        for rnd in range(3):
                            op("dve", lambda e: e.tensor_reduce(out=m8[:, :, 0:1], in_=gm2, op=ALU.max, axis=AX.X),
                               reads=[r_seln], writes=[r_m8])
                            op("dve", lambda e: e.tensor_tensor(out=m8[:, :, 1:2].broadcast_to([128, 8, 16]) if False else tmpg[:],
                                                                in0=gm2, in1=m8[:, :, 0:1].broadcast_to([128, 8, 16]),
                                                                op=ALU.is_ge), reads=[r_seln, r_m8], writes=[r_tmpg])
                            op("dve", lambda e: e.scalar_tensor_tensor(out=gm2, in0=tmpg[:], scalar=-2e30, in1=gm2,
                                                                       op0=ALU.mult, op1=ALU.add),
                               reads=[r_tmpg, r_seln], writes=[r_seln])
                        op("dve", lambda e: e.tensor_reduce(out=m8[:, :, 3:4], in_=gm2, op=ALU.max, axis=AX.X),
                           reads=[r_seln], writes=[r_m8])
                        op("dve", lambda e: e.tensor_tensor(
                            out=seln[:].rearrange("p (h n) -> p h n", h=8), in0=gm[:],
                            in1=m8[:, :, 3:4].broadcast_to([128, 8, 16]), op=ALU.is_lt),
                           reads=[r_gm, r_m8], writes=[r_seln])

                    chk("A6")
                    def store(dst, src, rsrc):
                        SC.dma(dst, src, d_stA, reads=(rsrc if isinstance(rsrc, list) else [rsrc]),
                               writes=[r_scrA], live=True)
                    store(sc["qn"][:, :, tsl].rearrange("m p t -> p m t"), o_qn[:], r_qn)
                    store(sc["qp"][:, :, tsl].rearrange("m p t -> p m t"), o_qp[:], r_qp)
                    store(sc["kn"][:, :, tsl].rearrange("m p t -> p m t"), o_kn[:], r_kn)
                    store(sc["kp"][:, tsl], o_kp[:], r_kp)
                    store(sc["va"][tsl, :].rearrange("(a p) c -> p a c", p=128), o_va[:], r_va)
                    store(sc["mq"][:, :, tsl].rearrange("m p t -> p m t"), o_mq[:], r_mq)
                    store(sc["mk"][:, :, tsl].rearrange("m p t -> p m t"), o_mk[:], r_mk)
                    store(sc["mv"][tsl, :].rearrange("(a p) c -> p a c", p=128), o_mv[:], r_mv)
                    if s == 0:
                        for _ in range(per_tile):
                            pro_job()
                gate_finish(NT - 1)
                SC.flush()

            if stop == "A":
                return nc
            with ExitStack() as st:
                QT = [sb(st, f"QT{i}", [128, S], BF16) for i in range(2)]
                KT = [sb(st, f"KT{i}", [128, S], BF16) for i in range(2)]
                VA = [sb(st, f"VA{i}", [128, NKT, 128], BF16) for i in range(2)]
                r_QT = [Res(), Res()]
                r_KT = [Res(), Res()]
                r_VA = [Res(), Res()]
                d_QT = [SC.dsem(), SC.dsem()]
                d_KT = [SC.dsem(), SC.dsem()]
                d_VA = [SC.dsem(), SC.dsem()]
                PT = [sb(st, f"PT{i}", [128, 512], BF16) for i in range(4)]
                r_PT = [Res() for _ in range(4)]
                aT = [sb(st, f"aT{i}", [128, S], BF16) for i in range(2)]
                r_aT = [Res(), Res()]
                rd = sb(st, "rd", [128, 512], F32)
                r_rd = Res()
                cmask = sb(st, "cmask", [128, 4, 512], BF16)
                abias = sb(st, "abias", [128, 8, 35], F32)
                r_cB = Res()
                d_cB = SC.dsem()
                SC.dma(cmask[:].rearrange("p a n -> p (a n)"), c_cmask, d_cB, writes=[r_cB])
                SC.dma(abias[:].rearrange("p a n -> p (a n)"), c_abias, d_cB, writes=[r_cB], nodeps=True)
                op("pool", lambda e: e.memset(VA[0][:, :, 64:128], 1.0), writes=[r_VA[0]])
                op("pool", lambda e: e.memset(VA[1][:, :, 0:64], 1.0), writes=[r_VA[1]])

                sbank = [(banks[i], bres[i]) for i in range(4)]
                obank = [(banks[4 + i], bres[4 + i]) for i in range(2)]

                def load_head(hh):
                    b = hh % 2
                    h = hh % 8
                    if hh < 8:
                        R = 96
                        SC.dma(QT[b][0:64, :], sc["qn"][h // 2, 64 * (h % 2):64 * (h % 2) + 64, :], d_QT[b],
                               reads=[r_scrA], writes=[r_QT[b]])
                        SC.dma(QT[b][64:96, :], sc["qp"][h // 4, 32 * (h % 4):32 * (h % 4) + 32, :], d_QT[b],
                               reads=[r_scrA], writes=[r_QT[b]], nodeps=True)
                        SC.dma(KT[b][0:64, :], sc["kn"][h // 2, 64 * (h % 2):64 * (h % 2) + 64, :], d_KT[b],
                               reads=[r_scrA], writes=[r_KT[b]])
                        SC.dma(KT[b][64:96, :], sc["kp"][:, :], d_KT[b], reads=[r_scrA], writes=[r_KT[b]], nodeps=True)
                        vsrc = sc["va"]
                    else:
                        SC.dma(QT[b][0:64, :], sc["mq"][h // 2, 64 * (h % 2):64 * (h % 2) + 64, :], d_QT[b],
                               reads=[r_scrA], writes=[r_QT[b]])
                        SC.dma(QT[b][64:66, :], c_qalibi[h], d_QT[b], writes=[r_QT[b]], nodeps=True)
                        SC.dma(QT[b][66:82, :], sc["ms"][16 * h:16 * h + 16, :], d_QT[b], writes=[r_QT[b]], nodeps=True)
                        SC.dma(KT[b][0:64, :], sc["mk"][h // 2, 64 * (h % 2):64 * (h % 2) + 64, :], d_KT[b],
                               reads=[r_scrA], writes=[r_KT[b]])
                        SC.dma(KT[b][64:82, :], c_kconst, d_KT[b], writes=[r_KT[b]], nodeps=True)
                        vsrc = sc["mv"]
                    c0 = 0 if b == 0 else 64
                    for k0 in range(0, NKT, 8):
                        SC.dma(VA[b][:, k0:k0 + 8, c0:c0 + 64],
                               vsrc[k0 * 128:(k0 + 8) * 128, 64 * h:64 * h + 64].rearrange("(k p) d -> p k d", p=128),
                               d_VA[b], reads=[r_scrA], writes=[r_VA[b]], nodeps=(k0 > 0))

                steps = []
                for hh in range(16):
                    for j in range(NT):
                        nk = 4 * j + 4
                        for kt in range(nk):
                            steps.append((hh, j, kt, kt == 0, kt == nk - 1))

                def emit_score(i):
                    hh, j, kt, first, last = steps[i]
                    b = hh % 2
                    R = 96 if hh < 8 else 82
                    r = kt - 4 * j
                    q0 = 128 * r if r > 0 else 0
                    bk, rb = sbank[i % 4]
                    diag = r >= 0
                    op("pe", lambda e: e.matmul(bk[:, q0:512], lhsT=KT[b][0:R, kt * 128:(kt + 1) * 128],
                                                rhs=QT[b][0:R, j * 512 + q0:(j + 1) * 512], start=True, stop=not diag),
                       reads=[r_KT[b], r_QT[b]], writes=[rb])
                    if diag:
                        op("pe", lambda e: e.matmul(bk[:, q0:512], lhsT=identb[:], rhs=cmask[:, r, q0:512],
                                                    start=False, stop=True), reads=[r_cB, r_const], writes=[rb])

                def emit_rest(i):
                    hh, j, kt, first, last = steps[i]
                    b = hh % 2
                    h = hh % 8
                    r = kt - 4 * j
                    q0 = 128 * r if r > 0 else 0
                    bk, rb = sbank[i % 4]
                    pt, rpt = PT[i % 4], r_PT[i % 4]
                    ob, rob = obank[(hh * NT + j) % 2]
                    if hh < 8:
                        op("act", lambda e: e.activation(out=pt[:, q0:512], in_=bk[:, q0:512], func=AF.Exp,
                                                         scale=SC_MLA), reads=[rb], writes=[rpt])
                    else:
                        di = (512 * j - 128 * kt + 384) // 128
                        op("act", lambda e: e.activation(out=pt[:, q0:512], in_=bk[:, q0:512], func=AF.Exp,
                                                         scale=SC_MOBA, bias=abias[:, h, di:di + 1]),
                           reads=[rb, r_cB], writes=[rpt])
                    op("pe", lambda e: e.matmul(ob[:, q0:512], lhsT=VA[b][:, kt, :], rhs=pt[:, q0:512],
                                                start=first, stop=last), reads=[r_VA[b], rpt], writes=[rob])
                    if last:
                        u0, d0 = (0, 64) if b == 0 else (64, 0)
                        pair = hh // 2
                        ab = pair % 2
                        op("dve", lambda e: e.reciprocal(out=rd[u0:u0 + 64, :], in_=ob[d0:d0 + 64, :]),
                           reads=[rob], writes=[r_rd])
                        op("dve", lambda e: e.tensor_tensor(out=aT[ab][u0:u0 + 64, j * 512:(j + 1) * 512],
                                                            in0=ob[u0:u0 + 64, :], in1=rd[u0:u0 + 64, :], op=ALU.mult),
                           reads=[rob, r_rd], writes=[r_aT[ab]])
                        if j == NT - 1 and b == 1:
                            SC.dma(sc["at"][pair], aT[ab][:], d_stB, reads=[r_aT[ab]], writes=[r_scrB], live=True)

                load_head(0)
                LOOK = 3
                nst = len(steps)
                for i in range(min(LOOK, nst)):
                    emit_score(i)
                for i in range(nst):
                    hh, j, kt, first, last = steps[i]
                    if first and j == 0 and hh + 1 < 16:
                        load_head(hh + 1)
                    if i + LOOK < nst:
                        emit_score(i + LOOK)
                    emit_rest(i)
                SC.flush()

            if stop == "B":
                return nc
            with ExitStack() as st:
                wo = sb(st, "wo", [128, 8, 1024], BF16)
                wdn = sb(st, "wdn", [128, NPAIR, 1024], BF16)
                lnt = sb(st, "lnt", [128, 4, 1024], F32)
                cvp = sb(st, "cvp", [128, 44, 4], F32)
                r_cw = Res()
                d_cw = SC.dsem()
                SC.dma(wo[:].rearrange("p a n -> p (a n)"), wo_s, d_cw, reads=[r_wscr], writes=[r_cw])
                SC.dma(wdn[:].rearrange("p a n -> p (a n)"), wdn_s, d_cw, writes=[r_cw], nodeps=True)
                SC.dma(lnt[:].rearrange("p a n -> p (a n)"), lnp, d_cw, writes=[r_cw], nodeps=True)
                SC.dma(cvp[:].rearrange("p a n -> p (a n)"), convp, d_cw, writes=[r_cw], nodeps=True)
                aTt = sb(st, "aTt", [128, 8, 512], BF16)
                r_aTt = Res()
                d_aTt = SC.dsem()
                xr = [sb(st, f"xr{i}", [128, D], F32) for i in range(2)]
                r_xr = [Res(), Res()]
                d_xr = [SC.dsem(), SC.dsem()]
                ybs = [sb(st, f"yb{i}", [128, D], F32) for i in range(2)]
                r_ybs = [Res(), Res()]
                lnk = {"k": 0}
                x1f2 = [sb(st, f"x1f{i}", [128, 4, D], F32) for i in range(2)]
                r_x1f2 = [[Res() for _ in range(4)] for _ in range(2)]
                x1T = sb(st, "x1T", [128, 8, 512], BF16)
                r_x1T = [Res() for _ in range(8)]
                statss = [sb(st, f"stats{i}", [128, 2, 6], F32) for i in range(2)]
                mvs = [sb(st, f"mv_{i}", [128, 2], F32) for i in range(2)]
                lnss = [sb(st, f"lns{i}", [128, 4], F32) for i in range(2)]
                r_lns = [Res(), Res()]
                wupt = [sb(st, f"wupt{i}", [128, 8, 256], BF16) for i in range(3)]
                r_wupt = [Res() for _ in range(3)]
                d_wupt = [SC.dsem() for _ in range(3)]
                hraw = [sb(st, f"hraw{i}", [128, 514], F32) for i in range(4)]
                r_hraw = [Res() for _ in range(4)]
                cacc = [sb(st, f"cacc{i}", [128, 512], F32) for i in range(4)]
                r_cacc = [Res() for _ in range(4)]
                sgs = [sb(st, f"sg{i}", [128, 512], F32) for i in range(2)]
                r_sgs = [Res(), Res()]
                carry = sb(st, "carry", [128, 44, 2], F32)
                r_carry = Res()
                actT = sb(st, "actT", [128, NPAIR, 512], BF16)
                r_actT = [Res() for _ in range(NPAIR)]
                ot = [sb(st, f"ot{i}", [128, D], F32) for i in range(2)]
                r_ot = [Res(), Res()]
                op("pool", lambda e: e.memset(carry[:], 0.0), writes=[r_carry])

                def layer_norm(k, gi, dst, rdst):
                    src, rsrc = ybs[k], r_ybs[k]
                    stats, mv_, lns, r_ln = statss[k], mvs[k], lnss[k], r_lns[k]
                    for hf in range(2):
                        op("dve", lambda e, hf=hf: e.bn_stats(out=stats[:, hf, :], in_=src[:, hf * 512:(hf + 1) * 512]),
                           reads=[rsrc], writes=[r_ln])
                    op("dve", lambda e: e.bn_aggr(out=mv_[:], in_=stats[:].rearrange("p a n -> p (a n)")),
                       reads=[r_ln], writes=[r_ln])
                    op("dve", lambda e: e.tensor_scalar(out=lns[:, 0:1], in0=mv_[:, 1:2], scalar1=EPS, scalar2=None,
                                                        op0=ALU.add), reads=[r_ln], writes=[r_ln])
                    op("act", lambda e: e.activation(out=lns[:, 1:2], in_=lns[:, 0:1], func=AF.Sqrt),
                       reads=[r_ln], writes=[r_ln])
                    op("dve", lambda e: e.reciprocal(out=lns[:, 2:3], in_=lns[:, 1:2]), reads=[r_ln], writes=[r_ln])
                    op("dve", lambda e: e.tensor_scalar(out=lns[:, 3:4], in0=mv_[:, 0:1], scalar1=-1.0,
                                                        scalar2=lns[:, 2:3], op0=ALU.mult, op1=ALU.mult),
                       reads=[r_ln], writes=[r_ln])
                    op("act", lambda e: e.activation(out=src[:], in_=src[:], func=AF.Identity, scale=lns[:, 2:3],
                                                     bias=lns[:, 3:4]), reads=[rsrc, r_ln], writes=[rsrc])
                    op("pool", lambda e: e.tensor_tensor(out=src[:], in0=src[:], in1=lnt[:, gi, :], op=ALU.mult),
                       reads=[rsrc, r_cw], writes=[rsrc])
                    op("pool", lambda e: e.tensor_tensor(out=dst, in0=src[:], in1=lnt[:, gi + 1, :], op=ALU.add),
                       reads=[rsrc, r_cw], writes=[rdst])

                def load_x(t, a):
                    k = (t * 4 + a) % 2
                    SC.dma(xr[k][:], x[s, t * 512 + a * 128:t * 512 + (a + 1) * 128, :], d_xr[k], writes=[r_xr[k]])

                wk = {"k": 0}

                def load_wup(p):
                    k = wk["k"] % 3
                    wk["k"] += 1
                    SC.dma(wupt[k][:].rearrange("p a n -> p (a n)"), wup_s[p], d_wupt[k], reads=[r_wscr],
                           writes=[r_wupt[k]])
                    return k

                wqd = {}

                def stage_wo(t):
                    tsl = slice(t * 512, (t + 1) * 512)
                    SC.dma(aTt[:], sc["at"][:, :, tsl].rearrange("c p t -> p c t"), d_aTt, reads=[r_scrB],
                           writes=[r_aTt])
                    load_x(t, 0)
                    wqd[t] = [load_wup(0), load_wup(1)]
                    for a in range(4):
                        k = (t * 4 + a) % 2
                        if a + 1 < 4:
                            load_x(t, a + 1)
                        bks = []
                        for n in range(2):
                            bk, rb = bank()
                            for c in range(8):
                                op("pe", lambda e, c=c, n=n, bk=bk: e.matmul(
                                    bk[:, :], lhsT=aTt[:, c, a * 128:(a + 1) * 128], rhs=wo[:, c, n * 512:(n + 1) * 512],
                                    start=(c == 0), stop=(c == 7)), reads=[r_aTt, r_cw], writes=[rb])
                            bks.append((bk, rb))
                        ky = lnk["k"] % 2
                        lnk["k"] += 1
                        for n in range(2):
                            bk, rb = bks[n]
                            op("dve", lambda e, n=n, bk=bk: e.scalar_tensor_tensor(
                                out=ybs[ky][:, n * 512:(n + 1) * 512], in0=xr[k][:, n * 512:(n + 1) * 512], scalar=ALPHA,
                                in1=bk[:, :], op0=ALU.mult, op1=ALU.add), reads=[r_xr[k], rb], writes=[r_ybs[ky]])
                        layer_norm(ky, 0, x1f2[t % 2][:, a, :], r_x1f2[t % 2][a])

                def stage_tr(t):
                    for c in range(8):
                        bk, rb = bank()
                        for a in range(4):
                            op("pe", lambda e, a=a, c=c, bk=bk: e.transpose(
                                out=bk[:, a * 128:(a + 1) * 128], in_=x1f2[t % 2][:, a, c * 128:(c + 1) * 128],
                                identity=ident[:]), reads=[r_x1f2[t % 2][a], r_const], writes=[rb])
                        if c % 2 == 0:
                            op("act", lambda e, c=c, bk=bk: e.activation(out=x1T[:, c, :], in_=bk[:, :], func=AF.Copy),
                               reads=[rb], writes=[r_x1T[c]])
                        else:
                            op("dve", lambda e, c=c, bk=bk: e.tensor_copy(out=x1T[:, c, :], in_=bk[:, :]),
                               reads=[rb], writes=[r_x1T[c]])

                def stage_p1(t):
                    wq = wqd.pop(t)
                    def gate_mul(p):
                        bg, bu = 2 * (p % 2), 2 * (p % 2) + 1
                        sg, r_sg = sgs[p % 2], r_sgs[p % 2]
                        op("act", lambda e: e.activation(out=sg[:], in_=cacc[bg][:], func=AF.Silu),
                           reads=[r_cacc[bg]], writes=[r_sg])
                        op("pool", lambda e: e.tensor_tensor(out=actT[:, p, :], in0=sg[:], in1=cacc[bu][:], op=ALU.mult),
                           reads=[r_sg, r_cacc[bu]], writes=[r_actT[p]])

                    for p in range(NPAIR):
                        kw = wq.pop(0)
                        if p + 2 < NPAIR:
                            wq.append(load_wup(p + 2))
                        for gu in range(2):
                            ch = p + 22 * gu
                            bk, rb = bank()
                            for c in range(8):
                                op("pe", lambda e, c=c, bk=bk, gu=gu: e.matmul(
                                    bk[:, :], lhsT=wupt[kw][:, c, gu * 128:(gu + 1) * 128], rhs=x1T[:, c, :],
                                    start=(c == 0), stop=(c == 7)), reads=[r_wupt[kw], r_x1T[c]], writes=[rb])
                            bi = 2 * (p % 2) + gu
                            hr, rhr = hraw[bi], r_hraw[bi]
                            ca, rca = cacc[bi], r_cacc[bi]
                            op("pool", lambda e, hr=hr, ch=ch: e.tensor_copy(out=hr[:, 0:2], in_=carry[:, ch, :]),
                               reads=[r_carry], writes=[rhr])
                            op("act", lambda e, hr=hr, bk=bk: e.activation(out=hr[:, 2:514], in_=bk[:, :], func=AF.Copy),
                               reads=[rb], writes=[rhr])
                            op("act", lambda e, ca=ca, bk=bk, ch=ch: e.activation(
                                out=ca[:], in_=bk[:, :], func=AF.Identity, scale=cvp[:, ch, 2:3], bias=cvp[:, ch, 3:4]),
                               reads=[rb, r_cw], writes=[rca])
                            op("pool", lambda e, hr=hr, ch=ch: e.tensor_copy(out=carry[:, ch, :], in_=hr[:, 512:514]),
                               reads=[rhr], writes=[r_carry])
                            op("dve", lambda e, ca=ca, hr=hr, ch=ch: e.scalar_tensor_tensor(
                                out=ca[:], in0=hr[:, 1:513], scalar=cvp[:, ch, 1:2], in1=ca[:],
                                op0=ALU.mult, op1=ALU.add), reads=[rhr, rca, r_cw], writes=[rca])
                            op("dve", lambda e, ca=ca, hr=hr, ch=ch: e.scalar_tensor_tensor(
                                out=ca[:], in0=hr[:, 0:512], scalar=cvp[:, ch, 0:1], in1=ca[:],
                                op0=ALU.mult, op1=ALU.add), reads=[rhr, rca, r_cw], writes=[rca])
                        if p >= 1:
                            gate_mul(p - 1)
                    gate_mul(NPAIR - 1)

                def stage_p2(t):
                    for a in range(4):
                        ko = (t * 4 + a) % 2
                        bks = []
                        for n in range(2):
                            bk, rb = bank()
                            for p in range(NPAIR):
                                op("pe", lambda e, p=p, n=n, bk=bk: e.matmul(
                                    bk[:, :], lhsT=actT[:, p, a * 128:(a + 1) * 128], rhs=wdn[:, p, n * 512:(n + 1) * 512],
                                    start=(p == 0), stop=(p == NPAIR - 1)), reads=[r_actT[p], r_cw], writes=[rb])
                            bks.append((bk, rb))
                        ky = lnk["k"] % 2
                        lnk["k"] += 1
                        for n in range(2):
                            bk, rb = bks[n]
                            op("dve", lambda e, n=n, bk=bk: e.scalar_tensor_tensor(
                                out=ybs[ky][:, n * 512:(n + 1) * 512], in0=x1f2[t % 2][:, a, n * 512:(n + 1) * 512], scalar=ALPHA,
                                in1=bk[:, :], op0=ALU.mult, op1=ALU.add), reads=[r_x1f2[t % 2][a], rb], writes=[r_ybs[ky]])
                        layer_norm(ky, 2, ot[ko][:], r_ot[ko])
                        SC.dma(out[s, t * 512 + a * 128:t * 512 + (a + 1) * 128, :], ot[ko][:], d_out,
                               reads=[r_ot[ko]], writes=[r_out], live=True, q="pool")

                stage_wo(0)
                stage_tr(0)
                for t in range(NT):
                    stage_p1(t)
                    if t + 1 < NT:
                        stage_wo(t + 1)
                    stage_p2(t)
                    if t + 1 < NT:
                        stage_tr(t + 1)
                SC.flush()

        SC._wait("sp", ("d", d_out, d_out.cnt))
        SC.flush()
    return nc


def _bf(a):
    return np.ascontiguousarray(a.astype(ml_dtypes.bfloat16))


def make_consts(S):
    NT = S // 512
    c = {}
    c["c_ident"] = np.eye(128, dtype=np.float32)
    c["c_identb"] = _bf(np.eye(128, dtype=np.float32))
    k = np.arange(128)[:, None, None]
    r = np.arange(4)[None, :, None]
    q = np.arange(512)[None, None, :]
    c["c_cmask"] = _bf(np.where(128 * r + k > q, NEGBIG, 0.0).astype(np.float32).reshape(128, 2048))
    half = 16
    inv = (np.float32(10000.0) ** (-np.arange(half, dtype=np.float32) / np.float32(half))).astype(np.float32)
    pos = np.arange(S, dtype=np.float32)
    ang = (pos[:, None] * inv[None, :]).astype(np.float32)
    cos = np.cos(ang).astype(np.float32)
    sin = np.sin(ang).astype(np.float32)
    d = np.arange(128) % 32
    cosT = cos[:, d % 16].T
    sinT = sin[:, d % 16].T * np.where(d < 16, -1.0, 1.0)[:, None]
    rope = np.stack([cosT.reshape(128, NT, 512), sinT.reshape(128, NT, 512)], axis=2)
    c["c_rope"] = np.ascontiguousarray(rope.transpose(1, 0, 2, 3).reshape(NT, 128, 1024).astype(np.float32))
    kc = np.zeros((18, S), np.float32)
    kc[0:2] = 1.0
    blk = np.arange(S) // 256
    for n in range(16):
        kc[2 + n] = np.where(blk == n, NEGBIG, 0.0)
    c["c_kconst"] = _bf(kc)
    slopes = (2.0 ** (-8.0 * np.arange(1, 9, dtype=np.float32) / 8.0)).astype(np.float32)
    dq = (np.arange(S) % 512).astype(np.float32)
    v = (-slopes[:, None] * dq[None, :] / np.float32(SC_MOBA)).astype(np.float32)
    hi = v.astype(ml_dtypes.bfloat16)
    lo = (v - hi.astype(np.float32)).astype(ml_dtypes.bfloat16)
    c["c_qalibi"] = np.ascontiguousarray(np.stack([hi, lo], axis=1))
    i = np.arange(35)
    Dd = (128 * i - 384).astype(np.float32)
    kk = np.arange(128, dtype=np.float32)
    ab = -slopes[None, :, None] * (Dd[None, None, :] - kk[:, None, None])
    c["c_abias"] = np.ascontiguousarray(ab.astype(np.float32).reshape(128, 8 * 35))
    own = np.arange(16)[:, None]
    n = np.arange(16)[None, :]
    pm = np.where(n < own, 0.0, np.where(n == own, 1e30, -1e30)).astype(np.float32)
    c["c_pastm"] = np.ascontiguousarray(np.broadcast_to(pm.reshape(1, 256), (128, 256)))
    return c


def prep_weights(w_in, q_norm_g, w_uq, kv_norm_g, w_ukv, w_o, ln1_g, ln1_b, w_up, conv_w, conv_b, w_down,
                 ln2_g, ln2_b):
    f = lambda a: np.ascontiguousarray(np.asarray(a, dtype=np.float32))
    w_in, w_uq, w_ukv, w_o, w_up, w_down = (f(a[0]) for a in (w_in, w_uq, w_ukv, w_o, w_up, w_down))
    r = np.arange(32)
    kr_sw = w_in[:, 384 + ((r + 16) % 32)]
    win = np.concatenate([w_in[:, 0:416], kr_sw, w_in[:, 416:1952]], axis=1)
    win = win.reshape(8, 128, WIN_COLS).transpose(1, 0, 2).reshape(128, 8 * WIN_COLS)
    h = np.arange(8)
    nope = (h[:, None] * 96 + np.arange(64)[None, :]).reshape(-1)
    pe = (h[:, None] * 96 + 64 + r[None, :]).reshape(-1)
    pesw = (h[:, None] * 96 + 64 + ((r + 16) % 32)[None, :]).reshape(-1)
    wuq = w_uq[:, np.concatenate([nope, pe, pesw])]
    wuq = wuq.reshape(2, 128, 1024).transpose(1, 0, 2).reshape(128, 2048)
    kcols = (h[:, None] * 128 + np.arange(64)[None, :]).reshape(-1)
    vcols = kcols + 64
    wukv = w_ukv[:, np.concatenate([kcols, vcols])]
    wo = w_o.reshape(8, 128, 1024).transpose(1, 0, 2).reshape(128, 8192)
    wdn = w_down.reshape(NPAIR, 128, 1024).transpose(1, 0, 2).reshape(128, NPAIR * 1024)
    wu = w_up.reshape(8, 128, 2, NPAIR, 128)
    wu = wu.transpose(3, 1, 0, 2, 4).reshape(NPAIR, 128, 8 * 256)
    lnp = np.stack([f(ln1_g[0]), f(ln1_b[0]), f(ln2_g[0]), f(ln2_b[0])], axis=0).reshape(1, 4096)
    lnp = np.broadcast_to(lnp, (128, 4096))
    cw = f(conv_w[0])
    cb = f(conv_b[0])
    cv = np.concatenate([cw, cb[None, :]], axis=0)
    cv = cv.reshape(4, 44, 128).transpose(2, 1, 0).reshape(128, 176)
    return {
        "w_in": f(win), "w_uq": f(wuq), "w_ukv": f(wukv), "w_o": f(wo), "w_dn": f(wdn), "w_up": f(wu),
        "qg": f(f(q_norm_g[0]).reshape(2, 128).T), "kvg": f(f(kv_norm_g[0]).reshape(128, 1)),
        "lnp": f(lnp), "convp": f(cv),
    }


_NC_CACHE = {}


def run(x, params, ncores, debug=False, stop=None):
    B, S, _ = x.shape
    nseq = B // ncores
    key = (nseq, S, debug)
    if key not in _NC_CACHE:
        _NC_CACHE[key] = build(nseq, S, debug, stop)
    nc = _NC_CACHE[key]
    shared = dict(prep_weights(**params))
    shared.update(make_consts(S))
    xs = np.ascontiguousarray(np.asarray(x, dtype=np.float32)).reshape(ncores, nseq, S, D)
    in_maps = [dict(shared, x=xs[i]) for i in range(ncores)]
    res = run_bass_kernel_spmd(nc, in_maps, core_ids=list(range(ncores)))
    return res


def kernel(x, w_in, q_norm_g, w_uq, kv_norm_g, w_ukv, w_o, ln1_g, ln1_b, w_up, conv_w, conv_b, w_down,
           ln2_g, ln2_b):
    params = dict(w_in=w_in, q_norm_g=q_norm_g, w_uq=w_uq, kv_norm_g=kv_norm_g, w_ukv=w_ukv, w_o=w_o,
                  ln1_g=ln1_g, ln1_b=ln1_b, w_up=w_up, conv_w=conv_w, conv_b=conv_b, w_down=w_down,
                  ln2_g=ln2_g, ln2_b=ln2_b)
    x = np.asarray(x)
    res = run(x, params, NCORES)
    outs = [np.asarray(r["out"]) for r in res.results]
    return np.concatenate(outs, axis=0).astype(np.float32)
```

```python
import math
from contextlib import ExitStack

import numpy as np
import ml_dtypes
import concourse.bass as bass
import concourse.mybir as mybir
from concourse.bass_utils import run_bass_kernel_spmd

F32 = mybir.dt.float32
BF16 = mybir.dt.bfloat16
AF = mybir.ActivationFunctionType
ALU = mybir.AluOpType
AX = mybir.AxisListType

D = 1024
NCORES = 8
EPS = 1e-5
NEGBIG = -30000.0
ALPHA = 2.0 ** 0.25
SC_MLA = 96.0 ** -0.5
SC_MOBA = 0.125
DFF = 2816
NPAIR = 22
WIN_COLS = 1984
import os as _os
ALL_INC = _os.environ.get("ALL_INC", "0") == "1"


class Res:
    __slots__ = ("w", "r", "scratch")

    def __init__(self, scratch=False):
        self.w = None
        self.r = []
        self.scratch = scratch


class DSem:
    def __init__(self, sem):
        self.sem = sem
        self.cnt = 0


class _Rec:
    def __getattr__(self, name):
        def f(*a, **k):
            self.call = (name, a, k)
        return f


class Sched:
    ENGS = ("pe", "act", "dve", "pool", "sp")
    CENGS = ("pe", "act", "dve", "pool")

    def __init__(self, nc, stack):
        self.nc = nc
        self.stack = stack
        self.prog = {e: [] for e in self.ENGS}
        self.sem = {}
        self.cnt = {}
        self.targets = {e: set() for e in self.CENGS}
        for e in self.CENGS:
            self.sem[e] = stack.enter_context(nc.semaphore("s_" + e))
            self.cnt[e] = 0
        self.seen = {e: {} for e in self.ENGS}
        self.dsems = []
        self.rank = {e: {} for e in self.CENGS}
        self.flushed = {e: 0 for e in self.CENGS}

    def dsem(self):
        s = self.stack.enter_context(self.nc.semaphore(f"d{len(self.dsems)}"))
        d = DSem(s)
        self.dsems.append(d)
        return d

    def _wait(self, eng, ev):
        if ev is None:
            return
        kind, obj, val = ev
        if kind == "e":
            if obj == eng and eng == "pe":
                return
            key = obj
        else:
            key = id(obj)
            if val is None:
                val = obj.cnt
        if val <= 0 or self.seen[eng].get(key, 0) >= val:
            return
        self.seen[eng][key] = val
        if kind == "e":
            assert val > self.flushed[obj] or val in self.rank[obj], (eng, obj, val)
            self.targets[obj].add(val)
        self.prog[eng].append(("w", kind, obj, val))

    def _deps(self, eng, reads, writes):
        for r in reads:
            self._wait(eng, r.w)
        for w in writes:
            if w.scratch:
                continue
            self._wait(eng, w.w)
            for ev in w.r:
                self._wait(eng, ev)

    def _commit(self, ev, reads, writes):
        for r in reads:
            if not r.scratch:
                r.r.append(ev)
        for w in writes:
            w.w = ev
            w.r = []

    def op(self, eng, fn, reads=(), writes=()):
        self._deps(eng, reads, writes)
        self.cnt[eng] += 1
        ev = ("e", eng, self.cnt[eng])
        rec = _Rec()
        fn(rec)
        if ALL_INC:
            self.targets[eng].add(self.cnt[eng])
        self.prog[eng].append(("i", rec.call, self.cnt[eng]))
        self._commit(ev, reads, writes)
        return ev

    def dma(self, out, in_, ds, reads=(), writes=(), live=False, nodeps=False, q="sp"):
        if not nodeps:
            self._deps(q, reads, writes)
        ds.cnt += 16
        assert ds.cnt < 65000
        ev = ("d", ds, None if live else ds.cnt)
        self.prog[q].append(("d", out, in_, ds.sem))
        self._commit(ev, reads, writes)
        return ev

    def barrier(self):
        for e in self.ENGS:
            for o in self.CENGS:
                self._wait(e, ("e", o, self.cnt[o]))
            for d in self.dsems:
                self._wait(e, ("d", d, d.cnt))

    def flush(self):
        self.barrier()
        rank = self.rank
        for e in self.CENGS:
            new = sorted(v for v in self.targets[e] if v not in rank[e])
            base = len(rank[e])
            for i, v in enumerate(new):
                assert v > self.flushed[e]
                rank[e][v] = base + i + 1
            assert len(rank[e]) < 65000, (e, len(rank[e]))

        def replay(name):
            def body(eng):
                for it in self.prog[name]:
                    if it[0] == "w":
                        _, kind, obj, val = it
                        if kind == "e":
                            eng.wait_ge(self.sem[obj], rank[obj][val])
                        else:
                            eng.wait_ge(obj.sem, val)
                    elif it[0] == "i":
                        nm, a, k = it[1]
                        ins = getattr(eng, nm)(*a, **k)
                        if it[2] in rank[name]:
                            ins.then_inc(self.sem[name], 1)
                    else:
                        eng.dma_start(out=it[1], in_=it[2]).then_inc(it[3], 16)
            return body

        with self.nc.Block() as block:
            block.tensor(replay("pe"))
            block.scalar(replay("act"))
            block.vector(replay("dve"))
            block.gpsimd(replay("pool"))
            block.sync(replay("sp"))
        for e in self.ENGS:
            self.prog[e] = []
        for e in self.CENGS:
            self.flushed[e] = self.cnt[e]


class _Stop(Exception):
    pass


def build(NSEQ, S, debug=False, stop=None):
    st_ = {}
    try:
        return _build(NSEQ, S, debug, stop, st_)
    except _Stop:
        return st_["nc"]


def _build(NSEQ, S, debug, stop, st_):
    assert S % 512 == 0
    NT = S // 512
    NKT = S // 128
    nc = bass.Bass("TRN2", target_bir_lowering=False)
    st_["nc"] = nc

    def din(name, shape, dt=F32):
        return nc.dram_tensor(name, list(shape), dt, kind="ExternalInput").ap()

    def dscr(name, shape, dt=BF16):
        kind = "ExternalOutput" if debug else "Internal"
        return nc.dram_tensor(name, list(shape), dt, kind=kind).ap()

    x = din("x", [NSEQ, S, D])
    out = nc.dram_tensor("out", [NSEQ, S, D], F32, kind="ExternalOutput").ap()
    w_in = din("w_in", [128, 8 * WIN_COLS])
    w_uq = din("w_uq", [128, 2 * 1024])
    w_ukv = din("w_ukv", [128, 1024])
    qg = din("qg", [128, 2])
    kvg = din("kvg", [128, 1])
    w_o = din("w_o", [128, 8 * 1024])
    w_dn = din("w_dn", [128, NPAIR * 1024])
    w_up = din("w_up", [NPAIR, 128, 2048])
    lnp = din("lnp", [128, 4 * 1024])
    convp = din("convp", [128, 44 * 4])
    c_ident = din("c_ident", [128, 128])
    c_identb = din("c_identb", [128, 128], BF16)
    c_cmask = din("c_cmask", [128, 4 * 512], BF16)
    c_rope = din("c_rope", [NT, 128, 2 * 512])
    c_kconst = din("c_kconst", [18, S], BF16)
    c_qalibi = din("c_qalibi", [8, 2, S], BF16)
    c_abias = din("c_abias", [128, 8 * 35])
    c_pastm = din("c_pastm", [128, 16 * 16])

    wo_s = dscr("wo_s", [128, 8 * 1024])
    wdn_s = dscr("wdn_s", [128, NPAIR * 1024])
    wup_s = dscr("wup_s", [NPAIR, 128, 2048])
    scr = []
    for s in range(NSEQ):
        scr.append(dict(
            qn=dscr(f"qn{s}", [4, 128, S]), qp=dscr(f"qp{s}", [2, 128, S]),
            kn=dscr(f"kn{s}", [4, 128, S]), kp=dscr(f"kp{s}", [32, S]),
            va=dscr(f"va{s}", [S, 512]),
            mq=dscr(f"mq{s}", [4, 128, S]), mk=dscr(f"mk{s}", [4, 128, S]),
            mv=dscr(f"mv{s}", [S, 512]), ms=dscr(f"ms{s}", [128, S]),
            at=dscr(f"at{s}", [8, 128, S]),
        ))

    with ExitStack() as top:
        SC = Sched(nc, top)
        op = SC.op
        uniq = {"n": 0}

        def sb(st, name, shape, dt):
            uniq["n"] += 1
            return st.enter_context(nc.sbuf_tensor(f"{name}_{uniq['n']}", list(shape), dt))

        banks = [top.enter_context(nc.psum_tensor(f"bank{i}", [128, 512], F32)) for i in range(8)]
        bres = [Res() for _ in range(8)]
        bstate = {"i": 0}

        def bank():
            i = bstate["i"]
            bstate["i"] = (i + 1) % 8
            return banks[i], bres[i]

        ident = sb(top, "ident", [128, 128], F32)
        identb = sb(top, "identb", [128, 128], BF16)
        ones = sb(top, "ones", [128, 128], BF16)
        r_const = Res()
        d_const = SC.dsem()
        SC.dma(ident[:], c_ident, d_const, writes=[r_const])
        SC.dma(identb[:], c_identb, d_const, writes=[r_const], nodeps=True)
        op("pool", lambda e: e.memset(ones[:], 1.0), writes=[r_const])

        d_wscr = SC.dsem()
        r_wscr = Res(scratch=True)

        def chk(tag):
            if stop == tag:
                SC.flush()
                raise _Stop()

        def make_stager(st, piece=2048):
            return dict(stgs=[sb(st, f"stg{i}", [128, piece], F32) for i in range(2)], rs=[Res(), Res()],
                        ds=[SC.dsem(), SC.dsem()], k=0, piece=piece)

        def cast_stream(sg_, src, ncols, dst_fn, piece=2048):
            for c0 in range(0, ncols, piece):
                c1 = min(ncols, c0 + piece)
                k = sg_["k"]
                sg_["k"] += 1
                b = k % 2
                SC.dma(sg_["stgs"][b][:, 0:c1 - c0], src[:, c0:c1], sg_["ds"][b], writes=[sg_["rs"][b]])
                dst_fn(c0, c1, sg_["stgs"][b][:, 0:c1 - c0], sg_["rs"][b], k)

        pro_jobs = []
        for c0 in range(0, 8 * 1024, 2048):
            pro_jobs.append((w_o[:, c0:c0 + 2048], wo_s[:, c0:c0 + 2048]))
        for c0 in range(0, NPAIR * 1024, 2048):
            pro_jobs.append((w_dn[:, c0:c0 + 2048], wdn_s[:, c0:c0 + 2048]))
        for p in range(NPAIR):
            pro_jobs.append((w_up[p], wup_s[p]))

        d_out = SC.dsem()
        r_out = Res(scratch=True)

        for s in range(NSEQ):
            sc = scr[s]
            d_stA = SC.dsem()
            r_scrA = Res(scratch=True)
            d_stB = SC.dsem()
            r_scrB = Res(scratch=True)

            with ExitStack() as st:
                win = sb(st, "win", [128, 8, WIN_COLS], BF16)
                wuq = sb(st, "wuq", [128, 2, 1024], BF16)
                wukv = sb(st, "wukv", [128, 1024], BF16)
                qg_t = sb(st, "qg_t", [128, 2], F32)
                kvg_t = sb(st, "kvg_t", [128, 1], F32)
                r_w = Res()
                d_w = SC.dsem()
                SC.dma(qg_t[:], qg, d_w, writes=[r_w])
                SC.dma(kvg_t[:], kvg, d_w, writes=[r_w], nodeps=True)
                with ExitStack() as st2:
                    winf = win[:].rearrange("p c n -> p (c n)")
                    stager2 = make_stager(st2)

                    def f_win(c0, c1, stg, rstg, k):
                        op("act" if k % 2 == 0 else "dve",
                           (lambda e: e.activation(out=winf[:, c0:c1], in_=stg, func=AF.Copy)) if k % 2 == 0 else
                           (lambda e: e.tensor_copy(out=winf[:, c0:c1], in_=stg)),
                           reads=[rstg], writes=[r_w])
                    cast_stream(stager2, w_in, 8 * WIN_COLS, f_win)

                    def f_wuq(c0, c1, stg, rstg, k):
                        kc = c0 // 1024
                        op("dve", lambda e: e.tensor_scalar(out=wuq[:, kc, :], in0=stg, scalar1=qg_t[:, kc:kc + 1],
                                                            scalar2=None, op0=ALU.mult),
                           reads=[rstg, r_w], writes=[r_w])
                    cast_stream(stager2, w_uq, 2048, f_wuq, piece=1024)

                    def f_wukv(c0, c1, stg, rstg, k):
                        op("dve", lambda e: e.tensor_scalar(out=wukv[:, :], in0=stg, scalar1=kvg_t[:, 0:1],
                                                            scalar2=None, op0=ALU.mult),
                           reads=[rstg, r_w], writes=[r_w])
                    cast_stream(stager2, w_ukv, 1024, f_wukv, piece=1024)
                    SC.flush()

                xt = [sb(st, f"xt{i}", [128, 4, D], F32) for i in range(2)]
                r_xt = [Res(), Res()]
                d_xt = [SC.dsem(), SC.dsem()]
                rope_t = [sb(st, f"rope{i}", [128, 2, 512], F32) for i in range(2)]
                r_rope = [Res(), Res()]
                d_rope = [SC.dsem(), SC.dsem()]
                xT = sb(st, "xT", [128, 8, 512], BF16)
                r_xT = [Res() for _ in range(8)]
                cqf = sb(st, "cqf", [128, 3, 512], F32)
                r_cqf = [Res() for _ in range(3)]
                sq = sb(st, "sq", [128, 3, 512], BF16)
                r_sq = [Res() for _ in range(3)]
                rstd = sb(st, "rstd", [128, 2, 512], F32)
                r_rstd = [Res(), Res()]
                cqn = sb(st, "cqn", [128, 3, 512], BF16)
                r_cqn = [Res() for _ in range(3)]
                tmp1 = sb(st, "tmp1", [128, 512], F32)
                r_tmp1 = Res()
                tmp2 = sb(st, "tmp2", [128, 512], F32)
                r_tmp2 = Res()
                rtmp = [(sb(st, f"rtA{i}", [128, 512], F32), Res(), sb(st, f"rtB{i}", [128, 512], F32), Res())
                        for i in range(2)]
                rtk = {"k": 0}
                pastm = sb(st, "pastm", [128, 16, 16], F32)
                kmsum = sb(st, "kmsum", [128, 4, 16], F32)
                kmT = sb(st, "kmT", [128, 4, 32], BF16)
                r_km = Res()
                gm = sb(st, "gm", [128, 8, 16], F32)
                r_gm = Res()
                m8 = sb(st, "m8", [128, 8, 8], F32)
                r_m8 = Res()
                selns = [sb(st, f"seln{i}", [128, 128], F32) for i in range(4)]
                r_selns = [Res() for _ in range(4)]
                tmpg = sb(st, "tmpg", [128, 8, 16], F32)
                r_tmpg = Res()
                d_pm = SC.dsem()
                r_pm = Res()
                SC.dma(pastm[:].rearrange("p a b -> p (a b)"), c_pastm, d_pm, writes=[r_pm])
                op("pool", lambda e: e.memset(kmT[:], 0.0), writes=[r_km])
                op("pool", lambda e: e.memset(kmsum[:], 0.0), writes=[r_km])

                def obuf(name, shape, n=1):
                    return sb(st, name, shape, BF16), ([Res() for _ in range(n)] if n > 1 else Res())
                o_qn, r_qn = obuf("o_qn", [128, 4, 512], 4)
                o_qp, r_qp = obuf("o_qp", [128, 2, 512], 2)
                o_kn, r_kn = obuf("o_kn", [128, 4, 512], 4)
                o_kp, r_kp = obuf("o_kp", [32, 512])
                o_va, r_va = obuf("o_va", [128, 4, 512], 4)
                o_mq, r_mq = obuf("o_mq", [128, 4, 512], 4)
                o_mk, r_mk = obuf("o_mk", [128, 4, 512], 4)
                o_mv, r_mv = obuf("o_mv", [128, 4, 512], 4)
                o_ms, r_ms = obuf("o_ms", [128, 512])

                if s == 0:
                    pstg = [sb(st, f"pstg{i}", [128, 2048], F32) for i in range(2)]
                    pstb = [sb(st, f"pstb{i}", [128, 2048], BF16) for i in range(2)]
                    r_pstg = [Res(), Res()]
                    r_pstb = [Res(), Res()]
                    d_pstg = [SC.dsem(), SC.dsem()]
                    pjk = {"k": 0}
                    per_tile = -(-len(pro_jobs) // NT)

                    def pro_job():
                        k = pjk["k"]
                        if k >= len(pro_jobs):
                            return
                        pjk["k"] += 1
                        src, dst = pro_jobs[k]
                        b = k % 2
                        SC.dma(pstg[b][:], src, d_pstg[b], writes=[r_pstg[b]])
                        op("pool", lambda e: e.tensor_copy(out=pstb[b][:], in_=pstg[b][:]),
                           reads=[r_pstg[b]], writes=[r_pstb[b]])
                        SC.dma(dst, pstb[b][:], d_wscr, reads=[r_pstb[b]], writes=[r_wscr], live=True)

                def gate_finish(tp):
                    bkT, rbT = bank()
                    for a in range(4):
                        op("pe", lambda e, a=a: e.transpose(out=bkT[:, a * 128:(a + 1) * 128], in_=selns[a][:],
                                                            identity=ident[:]),
                           reads=[r_selns[a], r_const], writes=[rbT])
                    op("act", lambda e: e.activation(out=o_ms[:, :], in_=bkT[:, :], func=AF.Copy),
                       reads=[rbT], writes=[r_ms])
                    SC.dma(sc["ms"][:, tp * 512:(tp + 1) * 512], o_ms[:], d_stA, reads=[r_ms], writes=[r_scrA],
                           live=True)

                evk = {"k": 0}

                def evac(outap, inap, reads, writes):
                    evk["k"] += 1
                    if evk["k"] % 8 != 0:
                        op("act", lambda e: e.activation(out=outap, in_=inap, func=AF.Copy), reads, writes)
                    else:
                        op("dve", lambda e: e.tensor_copy(out=outap, in_=inap), reads, writes)

                def load_tile(t):
                    b = t % 2
                    SC.dma(xt[b][:], x[s, t * 512:(t + 1) * 512, :].rearrange("(a p) d -> p a d", p=128),
                           d_xt[b], writes=[r_xt[b]])
                    SC.dma(rope_t[b][:].rearrange("p a n -> p (a n)"), c_rope[t], d_rope[b], writes=[r_rope[b]])

                load_tile(0)
                for t in range(NT):
                    b = t % 2
                    tsl = slice(t * 512, (t + 1) * 512)
                    if t + 1 < NT:
                        load_tile(t + 1)
                    for c in range(8):
                        bk, rb = bank()
                        for a in range(4):
                            op("pe", lambda e, a=a, c=c, bk=bk: e.transpose(
                                out=bk[:, a * 128:(a + 1) * 128], in_=xt[b][:, a, c * 128:(c + 1) * 128],
                                identity=ident[:]), reads=[r_xt[b], r_const], writes=[rb])
                        evac(xT[:, c, :], bk[:, :], [rb], [r_xT[c]])

                    chk("A1")

                    def proj(col0, m, rhs_t=None):
                        bk, rb = bank()
                        for c in range(8):
                            op("pe", lambda e, c=c, bk=bk: e.matmul(bk[0:m, :], lhsT=win[:, c, col0:col0 + m],
                                                                     rhs=xT[:, c, :], start=(c == 0), stop=(c == 7)),
                               reads=[r_w, r_xT[c]], writes=[rb])
                        return bk, rb

                    for m in range(3):
                        bk, rb = proj(m * 128, 128)
                        op("dve", lambda e, m=m, bk=bk: e.tensor_copy(out=cqf[:, m, :], in_=bk[:, :]),
                           reads=[rb], writes=[r_cqf[m]])
                        op("act", lambda e, m=m: e.activation(out=sq[:, m, :], in_=cqf[:, m, :], func=AF.Square),
                           reads=[r_cqf[m]], writes=[r_sq[m]])
                    chk("A1b")
                    for g, (chs, n) in enumerate((((0, 1), 256.0), ((2,), 128.0))):
                        bk, rb = bank()
                        for i, m in enumerate(chs):
                            op("pe", lambda e, m=m, bk=bk, i=i, chs=chs: e.matmul(
                                bk[:, :], lhsT=ones[:], rhs=sq[:, m, :], start=(i == 0), stop=(i == len(chs) - 1)),
                               reads=[r_sq[m], r_const], writes=[rb])
                        op("dve", lambda e, bk=bk, n=n: e.tensor_scalar(out=tmp1[:], in0=bk[:, :], scalar1=1.0 / n,
                                                                        scalar2=EPS, op0=ALU.mult, op1=ALU.add),
                           reads=[rb], writes=[r_tmp1])
                        op("act", lambda e: e.activation(out=tmp2[:], in_=tmp1[:], func=AF.Sqrt),
                           reads=[r_tmp1], writes=[r_tmp2])
                        op("dve", lambda e, g=g: e.reciprocal(out=rstd[:, g, :], in_=tmp2[:]),
                           reads=[r_tmp2], writes=[r_rstd[g]])
                        chk("A1c")
                        for m in chs:
                            op("pool", lambda e, m=m, g=g: e.tensor_tensor(out=cqn[:, m, :], in0=cqf[:, m, :],
                                                                           in1=rstd[:, g, :], op=ALU.mult),
                               reads=[r_cqf[m], r_rstd[g]], writes=[r_cqn[m]])

                    chk("A2")

                    def rope_comb(bkP, rbP, bkR, rbR, nrow, outap, rout):
                        tA, rA, tB, rB = rtmp[rtk["k"] % 2]
                        rtk["k"] += 1
                        op("dve", lambda e: e.tensor_tensor(out=tA[0:nrow, :], in0=bkP[0:nrow, :],
                                                            in1=rope_t[b][0:nrow, 0, :], op=ALU.mult),
                           reads=[rbP, r_rope[b]], writes=[rA])
                        op("dve", lambda e: e.tensor_tensor(out=tB[0:nrow, :], in0=bkR[0:nrow, :],
                                                            in1=rope_t[b][0:nrow, 1, :], op=ALU.mult),
                           reads=[rbR, r_rope[b]], writes=[rB])
                        op("pool", lambda e: e.tensor_tensor(out=outap, in0=tA[0:nrow, :], in1=tB[0:nrow, :],
                                                             op=ALU.add),
                           reads=[rA, rB], writes=[rout])

                    bkP, rbP = proj(384, 32)
                    bkR, rbR = proj(416, 32)
                    rope_comb(bkP, rbP, bkR, rbR, 32, o_kp[:, :], r_kp)

                    chk("A3")
                    for m in range(4):
                        bk, rb = proj(448 + m * 128, 128)
                        evac(o_mq[:, m, :], bk[:, :], [rb], [r_mq[m]])
                    for m in range(4):
                        bk, rb = proj(960 + m * 128, 128)
                        op("dve", lambda e, m=m, bk=bk: e.tensor_copy(out=o_mk[:, m, :], in_=bk[:, :]),
                           reads=[rb], writes=[r_mk[m]])
                        op("dve", lambda e, m=m, bk=bk: e.tensor_reduce(
                            out=kmsum[:, m, 2 * t:2 * t + 2], in_=bk[:, :].rearrange("p (b l) -> p b l", b=2),
                            op=ALU.add, axis=AX.X), reads=[rb], writes=[r_km])
                    for hp in range(2):
                        op("dve", lambda e, hp=hp: e.tensor_scalar(
                            out=kmT[64 * hp:64 * hp + 64, :, 16 * hp + 2 * t:16 * hp + 2 * t + 2],
                            in0=kmsum[64 * hp:64 * hp + 64, :, 2 * t:2 * t + 2],
                            scalar1=1.0 / 256.0, scalar2=None, op0=ALU.mult), reads=[r_km], writes=[r_km])
                    for a in range(4):
                        bk, rb = bank()
                        for c in range(8):
                            op("pe", lambda e, c=c, a=a, bk=bk: e.matmul(
                                bk[:, :], lhsT=xT[:, c, a * 128:(a + 1) * 128], rhs=win[:, c, 1472:1984],
                                start=(c == 0), stop=(c == 7)), reads=[r_w, r_xT[c]], writes=[rb])
                        evac(o_mv[:, a, :], bk[:, :], [rb], [r_mv[a]])

                    if t >= 1:
                        gate_finish(t - 1)
                    chk("A4")
                    for m in range(4):
                        bk, rb = bank()
                        for kc in range(2):
                            op("pe", lambda e, kc=kc, m=m, bk=bk: e.matmul(
                                bk[:, :], lhsT=wuq[:, kc, m * 128:(m + 1) * 128], rhs=cqn[:, kc, :],
                                start=(kc == 0), stop=(kc == 1)), reads=[r_w, r_cqn[kc]], writes=[rb])
                        evac(o_qn[:, m, :], bk[:, :], [rb], [r_qn[m]])
                    for m in range(2):
                        pr = []
                        for off in (512, 768):
                            bk, rb = bank()
                            for kc in range(2):
                                op("pe", lambda e, kc=kc, m=m, bk=bk, off=off: e.matmul(
                                    bk[:, :], lhsT=wuq[:, kc, off + m * 128:off + (m + 1) * 128], rhs=cqn[:, kc, :],
                                    start=(kc == 0), stop=(kc == 1)), reads=[r_w, r_cqn[kc]], writes=[rb])
                            pr.append((bk, rb))
                        rope_comb(pr[0][0], pr[0][1], pr[1][0], pr[1][1], 128, o_qp[:, m, :], r_qp[m])
                    for m in range(4):
                        bk, rb = bank()
                        op("pe", lambda e, m=m, bk=bk: e.matmul(bk[:, :], lhsT=wukv[:, m * 128:(m + 1) * 128],
                                                                rhs=cqn[:, 2, :], start=True, stop=True),
                           reads=[r_w, r_cqn[2]], writes=[rb])
                        evac(o_kn[:, m, :], bk[:, :], [rb], [r_kn[m]])
                    for a in range(4):
                        bk, rb = bank()
                        op("pe", lambda e, a=a, bk=bk: e.matmul(bk[:, :], lhsT=cqn[:, 2, a * 128:(a + 1) * 128],
                                                                rhs=wukv[:, 512:1024], start=True, stop=True),
                           reads=[r_w, r_cqn[2]], writes=[rb])
                        evac(o_va[:, a, :], bk[:, :], [rb], [r_va[a]])

                    chk("A5")
                    for a in range(4):
                        own = 2 * t + a // 2
                        bk, rb = bank()
                        for m in range(4):
                            op("pe", lambda e, m=m, a=a, bk=bk: e.matmul(
                                bk[:, 32 * m:32 * m + 32], lhsT=o_mq[:, m, a * 128:(a + 1) * 128],
                                rhs=kmT[:, m, :], start=True, stop=True),
                               reads=[r_mq[m], r_km], writes=[rb])
                        op("dve", lambda e, bk=bk, own=own: e.tensor_tensor(
                            out=gm[:], in0=bk[:, 0:128].rearrange("p (h n) -> p h n", h=8),
                            in1=pastm[:, own:own + 1, :].broadcast_to([128, 8, 16]), op=ALU.add),
                           reads=[rb, r_pm], writes=[r_gm])
                        seln, r_seln = selns[a], r_selns[a]
                        gm2 = seln[:].rearrange("p (h n) -> p h n", h=8)
                        op("dve", lambda e: e.tensor_copy(out=gm2, in_=gm[:]), reads=[r_gm], writes=[r_seln])
                        for rnd in range(3):
                            op("dve", lambda e: e.tensor_reduce(out=m8[:, :, 0:1], in_=gm2, op=ALU.max, axis=AX.X),
                               reads=[r_seln], writes=[r_m8])
                            op("dve", lambda e: e.tensor_tensor(out=m8[:, :, 1:2].broadcast_to([128, 8, 16]) if False else tmpg[:],
                                                                in0=gm2, in1=m8[:, :, 0:1].broadcast_to([128, 8, 16]),
                                                                op=ALU.is_ge), reads=[r_seln, r_m8], writes=[r_tmpg])
                            op("dve", lambda e: e.scalar_tensor_tensor(out=gm2, in0=tmpg[:], scalar=-2e30, in1=gm2,
                                                                       op0=ALU.mult, op1=ALU.add),
                               reads=[r_tmpg, r_seln], writes=[r_seln])
                        op("dve", lambda e: e.tensor_reduce(out=m8[:, :, 3:4], in_=gm2, op=ALU.max, axis=AX.X),
                           reads=[r_seln], writes=[r_m8])
                        op("dve", lambda e: e.tensor_tensor(
                            out=seln[:].rearrange("p (h n) -> p h n", h=8), in0=gm[:],
                            in1=m8[:, :, 3:4].broadcast_to([128, 8, 16]), op=ALU.is_lt),
                           reads=[r_gm, r_m8], writes=[r_seln])

                    chk("A6")
                    def store(dst, src, rsrc):
                        SC.dma(dst, src, d_stA, reads=(rsrc if isinstance(rsrc, list) else [rsrc]),
                               writes=[r_scrA], live=True)
                    store(sc["qn"][:, :, tsl].rearrange("m p t -> p m t"), o_qn[:], r_qn)
                    store(sc["qp"][:, :, tsl].rearrange("m p t -> p m t"), o_qp[:], r_qp)
                    store(sc["kn"][:, :, tsl].rearrange("m p t -> p m t"), o_kn[:], r_kn)
                    store(sc["kp"][:, tsl], o_kp[:], r_kp)
                    store(sc["va"][tsl, :].rearrange("(a p) c -> p a c", p=128), o_va[:], r_va)
                    store(sc["mq"][:, :, tsl].rearrange("m p t -> p m t"), o_mq[:], r_mq)
                    store(sc["mk"][:, :, tsl].rearrange("m p t -> p m t"), o_mk[:], r_mk)
                    store(sc["mv"][tsl, :].rearrange("(a p) c -> p a c", p=128), o_mv[:], r_mv)
                    if s == 0:
                        for _ in range(per_tile):
                            pro_job()
                gate_finish(NT - 1)
                SC.flush()

            if stop == "A":
                return nc
            with ExitStack() as st:
                QT = [sb(st, f"QT{i}", [128, S], BF16) for i in range(2)]
                KT = [sb(st, f"KT{i}", [128, S], BF16) for i in range(2)]
                VA = [sb(st, f"VA{i}", [128, NKT, 128], BF16) for i in range(2)]
                r_QT = [Res(), Res()]
                r_KT = [Res(), Res()]
                r_VA = [Res(), Res()]
                d_QT = [SC.dsem(), SC.dsem()]
                d_KT = [SC.dsem(), SC.dsem()]
                d_VA = [SC.dsem(), SC.dsem()]
                PT = [sb(st, f"PT{i}", [128, 512], BF16) for i in range(4)]
                r_PT = [Res() for _ in range(4)]
                aT = [sb(st, f"aT{i}", [128, S], BF16) for i in range(2)]
                r_aT = [Res(), Res()]
                rd = sb(st, "rd", [128, 512], F32)
                r_rd = Res()
                cmask = sb(st, "cmask", [128, 4, 512], BF16)
                abias = sb(st, "abias", [128, 8, 35], F32)
                r_cB = Res()
                d_cB = SC.dsem()
                SC.dma(cmask[:].rearrange("p a n -> p (a n)"), c_cmask, d_cB, writes=[r_cB])
                SC.dma(abias[:].rearrange("p a n -> p (a n)"), c_abias, d_cB, writes=[r_cB], nodeps=True)
                op("pool", lambda e: e.memset(VA[0][:, :, 64:128], 1.0), writes=[r_VA[0]])
                op("pool", lambda e: e.memset(VA[1][:, :, 0:64], 1.0), writes=[r_VA[1]])

                sbank = [(banks[i], bres[i]) for i in range(4)]
                obank = [(banks[4 + i], bres[4 + i]) for i in range(2)]

                def load_head(hh):
                    b = hh % 2
                    h = hh % 8
                    if hh < 8:
                        R = 96
                        SC.dma(QT[b][0:64, :], sc["qn"][h // 2, 64 * (h % 2):64 * (h % 2) + 64, :], d_QT[b],
                               reads=[r_scrA], writes=[r_QT[b]])
                        SC.dma(QT[b][64:96, :], sc["qp"][h // 4, 32 * (h % 4):32 * (h % 4) + 32, :], d_QT[b],
                               reads=[r_scrA], writes=[r_QT[b]], nodeps=True)
                        SC.dma(KT[b][0:64, :], sc["kn"][h // 2, 64 * (h % 2):64 * (h % 2) + 64, :], d_KT[b],
                               reads=[r_scrA], writes=[r_KT[b]])
                        SC.dma(KT[b][64:96, :], sc["kp"][:, :], d_KT[b], reads=[r_scrA], writes=[r_KT[b]], nodeps=True)
                        vsrc = sc["va"]
                    else:
                        SC.dma(QT[b][0:64, :], sc["mq"][h // 2, 64 * (h % 2):64 * (h % 2) + 64, :], d_QT[b],
                               reads=[r_scrA], writes=[r_QT[b]])
                        SC.dma(QT[b][64:66, :], c_qalibi[h], d_QT[b], writes=[r_QT[b]], nodeps=True)
                        SC.dma(QT[b][66:82, :], sc["ms"][16 * h:16 * h + 16, :], d_QT[b], writes=[r_QT[b]], nodeps=True)
                        SC.dma(KT[b][0:64, :], sc["mk"][h // 2, 64 * (h % 2):64 * (h % 2) + 64, :], d_KT[b],
                               reads=[r_scrA], writes=[r_KT[b]])
                        SC.dma(KT[b][64:82, :], c_kconst, d_KT[b], writes=[r_KT[b]], nodeps=True)
                        vsrc = sc["mv"]
                    c0 = 0 if b == 0 else 64
                    for k0 in range(0, NKT, 8):
                        SC.dma(VA[b][:, k0:k0 + 8, c0:c0 + 64],
                               vsrc[k0 * 128:(k0 + 8) * 128, 64 * h:64 * h + 64].rearrange("(k p) d -> p k d", p=128),
                               d_VA[b], reads=[r_scrA], writes=[r_VA[b]], nodeps=(k0 > 0))

                steps = []
                for hh in range(16):
                    for j in range(NT):
                        nk = 4 * j + 4
                        for kt in range(nk):
                            steps.append((hh, j, kt, kt == 0, kt == nk - 1))

                def emit_score(i):
                    hh, j, kt, first, last = steps[i]
                    b = hh % 2
                    R = 96 if hh < 8 else 82
                    r = kt - 4 * j
                    q0 = 128 * r if r > 0 else 0
                    bk, rb = sbank[i % 4]
                    diag = r >= 0
                    op("pe", lambda e: e.matmul(bk[:, q0:512], lhsT=KT[b][0:R, kt * 128:(kt + 1) * 128],
                                                rhs=QT[b][0:R, j * 512 + q0:(j + 1) * 512], start=True, stop=not diag),
                       reads=[r_KT[b], r_QT[b]], writes=[rb])
                    if diag:
                        op("pe", lambda e: e.matmul(bk[:, q0:512], lhsT=identb[:], rhs=cmask[:, r, q0:512],
                                                    start=False, stop=True), reads=[r_cB, r_const], writes=[rb])

                def emit_rest(i):
                    hh, j, kt, first, last = steps[i]
                    b = hh % 2
                    h = hh % 8
                    r = kt - 4 * j
                    q0 = 128 * r if r > 0 else 0
                    bk, rb = sbank[i % 4]
                    pt, rpt = PT[i % 4], r_PT[i % 4]
                    ob, rob = obank[(hh * NT + j) % 2]
                    if hh < 8:
                        op("act", lambda e: e.activation(out=pt[:, q0:512], in_=bk[:, q0:512], func=AF.Exp,
                                                         scale=SC_MLA), reads=[rb], writes=[rpt])
                    else:
                        di = (512 * j - 128 * kt + 384) // 128
                        op("act", lambda e: e.activation(out=pt[:, q0:512], in_=bk[:, q0:512], func=AF.Exp,
                                                         scale=SC_MOBA, bias=abias[:, h, di:di + 1]),
                           reads=[rb, r_cB], writes=[rpt])
                    op("pe", lambda e: e.matmul(ob[:, q0:512], lhsT=VA[b][:, kt, :], rhs=pt[:, q0:512],
                                                start=first, stop=last), reads=[r_VA[b], rpt], writes=[rob])
                    if last:
                        u0, d0 = (0, 64) if b == 0 else (64, 0)
                        pair = hh // 2
                        ab = pair % 2
                        op("dve", lambda e: e.reciprocal(out=rd[u0:u0 + 64, :], in_=ob[d0:d0 + 64, :]),
                           reads=[rob], writes=[r_rd])
                        op("dve", lambda e: e.tensor_tensor(out=aT[ab][u0:u0 + 64, j * 512:(j + 1) * 512],
                                                            in0=ob[u0:u0 + 64, :], in1=rd[u0:u0 + 64, :], op=ALU.mult),
                           reads=[rob, r_rd], writes=[r_aT[ab]])
                        if j == NT - 1 and b == 1:
                            SC.dma(sc["at"][pair], aT[ab][:], d_stB, reads=[r_aT[ab]], writes=[r_scrB], live=True)

                load_head(0)
                LOOK = 3
                nst = len(steps)
                for i in range(min(LOOK, nst)):
                    emit_score(i)
                for i in range(nst):
                    hh, j, kt, first, last = steps[i]
                    if first and j == 0 and hh + 1 < 16:
                        load_head(hh + 1)
                    if i + LOOK < nst:
                        emit_score(i + LOOK)
                    emit_rest(i)
                SC.flush()

            if stop == "B":
                return nc
            with ExitStack() as st:
                wo = sb(st, "wo", [128, 8, 1024], BF16)
                wdn = sb(st, "wdn", [128, NPAIR, 1024], BF16)
                lnt = sb(st, "lnt", [128, 4, 1024], F32)
                cvp = sb(st, "cvp", [128, 44, 4], F32)
                r_cw = Res()
                d_cw = SC.dsem()
                SC.dma(wo[:].rearrange("p a n -> p (a n)"), wo_s, d_cw, reads=[r_wscr], writes=[r_cw])
                SC.dma(wdn[:].rearrange("p a n -> p (a n)"), wdn_s, d_cw, writes=[r_cw], nodeps=True)
                SC.dma(lnt[:].rearrange("p a n -> p (a n)"), lnp, d_cw, writes=[r_cw], nodeps=True)
                SC.dma(cvp[:].rearrange("p a n -> p (a n)"), convp, d_cw, writes=[r_cw], nodeps=True)
                aTt = sb(st, "aTt", [128, 8, 512], BF16)
                r_aTt = Res()
                d_aTt = SC.dsem()
                xr = [sb(st, f"xr{i}", [128, D], F32) for i in range(2)]
                r_xr = [Res(), Res()]
                d_xr = [SC.dsem(), SC.dsem()]
                ybs = [sb(st, f"yb{i}", [128, D], F32) for i in range(2)]
                r_ybs = [Res(), Res()]
                lnk = {"k": 0}
                x1f2 = [sb(st, f"x1f{i}", [128, 4, D], F32) for i in range(2)]
                r_x1f2 = [[Res() for _ in range(4)] for _ in range(2)]
                x1T = sb(st, "x1T", [128, 8, 512], BF16)
                r_x1T = [Res() for _ in range(8)]
                statss = [sb(st, f"stats{i}", [128, 2, 6], F32) for i in range(2)]
                mvs = [sb(st, f"mv_{i}", [128, 2], F32) for i in range(2)]
                lnss = [sb(st, f"lns{i}", [128, 4], F32) for i in range(2)]
                r_lns = [Res(), Res()]
                wupt = [sb(st, f"wupt{i}", [128, 8, 256], BF16) for i in range(3)]
                r_wupt = [Res() for _ in range(3)]
                d_wupt = [SC.dsem() for _ in range(3)]
                hraw = [sb(st, f"hraw{i}", [128, 514], F32) for i in range(4)]
                r_hraw = [Res() for _ in range(4)]
                cacc = [sb(st, f"cacc{i}", [128, 512], F32) for i in range(4)]
                r_cacc = [Res() for _ in range(4)]
                sgs = [sb(st, f"sg{i}", [128, 512], F32) for i in range(2)]
                r_sgs = [Res(), Res()]
                carry = sb(st, "carry", [128, 44, 2], F32)
                r_carry = Res()
                actT = sb(st, "actT", [128, NPAIR, 512], BF16)
                r_actT = [Res() for _ in range(NPAIR)]
                ot = [sb(st, f"ot{i}", [128, D], F32) for i in range(2)]
                r_ot = [Res(), Res()]
                op("pool", lambda e: e.memset(carry[:], 0.0), writes=[r_carry])

                def layer_norm(k, gi, dst, rdst):
                    src, rsrc = ybs[k], r_ybs[k]
                    stats, mv_, lns, r_ln = statss[k], mvs[k], lnss[k], r_lns[k]
                    for hf in range(2):
                        op("dve", lambda e, hf=hf: e.bn_stats(out=stats[:, hf, :], in_=src[:, hf * 512:(hf + 1) * 512]),
                           reads=[rsrc], writes=[r_ln])
                    op("dve", lambda e: e.bn_aggr(out=mv_[:], in_=stats[:].rearrange("p a n -> p (a n)")),
                       reads=[r_ln], writes=[r_ln])
                    op("dve", lambda e: e.tensor_scalar(out=lns[:, 0:1], in0=mv_[:, 1:2], scalar1=EPS, scalar2=None,
                                                        op0=ALU.add), reads=[r_ln], writes=[r_ln])
                    op("act", lambda e: e.activation(out=lns[:, 1:2], in_=lns[:, 0:1], func=AF.Sqrt),
                       reads=[r_ln], writes=[r_ln])
                    op("dve", lambda e: e.reciprocal(out=lns[:, 2:3], in_=lns[:, 1:2]), reads=[r_ln], writes=[r_ln])
                    op("dve", lambda e: e.tensor_scalar(out=lns[:, 3:4], in0=mv_[:, 0:1], scalar1=-1.0,
                                                        scalar2=lns[:, 2:3], op0=ALU.mult, op1=ALU.mult),
                       reads=[r_ln], writes=[r_ln])
                    op("act", lambda e: e.activation(out=src[:], in_=src[:], func=AF.Identity, scale=lns[:, 2:3],
                                                     bias=lns[:, 3:4]), reads=[rsrc, r_ln], writes=[rsrc])
                    op("pool", lambda e: e.tensor_tensor(out=src[:], in0=src[:], in1=lnt[:, gi, :], op=ALU.mult),
                       reads=[rsrc, r_cw], writes=[rsrc])
                    op("pool", lambda e: e.tensor_tensor(out=dst, in0=src[:], in1=lnt[:, gi + 1, :], op=ALU.add),
                       reads=[rsrc, r_cw], writes=[rdst])

                def load_x(t, a):
                    k = (t * 4 + a) % 2
                    SC.dma(xr[k][:], x[s, t * 512 + a * 128:t * 512 + (a + 1) * 128, :], d_xr[k], writes=[r_xr[k]])

                wk = {"k": 0}

                def load_wup(p):
                    k = wk["k"] % 3
                    wk["k"] += 1
                    SC.dma(wupt[k][:].rearrange("p a n -> p (a n)"), wup_s[p], d_wupt[k], reads=[r_wscr],
                           writes=[r_wupt[k]])
                    return k

                wqd = {}

                def stage_wo(t):
                    tsl = slice(t * 512, (t + 1) * 512)
                    SC.dma(aTt[:], sc["at"][:, :, tsl].rearrange("c p t -> p c t"), d_aTt, reads=[r_scrB],
                           writes=[r_aTt])
                    load_x(t, 0)
                    wqd[t] = [load_wup(0), load_wup(1)]
                    for a in range(4):
                        k = (t * 4 + a) % 2
                        if a + 1 < 4:
                            load_x(t, a + 1)
                        bks = []
                        for n in range(2):
                            bk, rb = bank()
                            for c in range(8):
                                op("pe", lambda e, c=c, n=n, bk=bk: e.matmul(
                                    bk[:, :], lhsT=aTt[:, c, a * 128:(a + 1) * 128], rhs=wo[:, c, n * 512:(n + 1) * 512],
                                    start=(c == 0), stop=(c == 7)), reads=[r_aTt, r_cw], writes=[rb])
                            bks.append((bk, rb))
                        ky = lnk["k"] % 2
                        lnk["k"] += 1
                        for n in range(2):
                            bk, rb = bks[n]
                            op("dve", lambda e, n=n, bk=bk: e.scalar_tensor_tensor(
                                out=ybs[ky][:, n * 512:(n + 1) * 512], in0=xr[k][:, n * 512:(n + 1) * 512], scalar=ALPHA,
                                in1=bk[:, :], op0=ALU.mult, op1=ALU.add), reads=[r_xr[k], rb], writes=[r_ybs[ky]])
                        layer_norm(ky, 0, x1f2[t % 2][:, a, :], r_x1f2[t % 2][a])

                def stage_tr(t):
                    for c in range(8):
                        bk, rb = bank()
                        for a in range(4):
                            op("pe", lambda e, a=a, c=c, bk=bk: e.transpose(
                                out=bk[:, a * 128:(a + 1) * 128], in_=x1f2[t % 2][:, a, c * 128:(c + 1) * 128],
                                identity=ident[:]), reads=[r_x1f2[t % 2][a], r_const], writes=[rb])
                        if c % 2 == 0:
                            op("act", lambda e, c=c, bk=bk: e.activation(out=x1T[:, c, :], in_=bk[:, :], func=AF.Copy),
                               reads=[rb], writes=[r_x1T[c]])
                        else:
                            op("dve", lambda e, c=c, bk=bk: e.tensor_copy(out=x1T[:, c, :], in_=bk[:, :]),
                               reads=[rb], writes=[r_x1T[c]])

                def stage_p1(t):
                    wq = wqd.pop(t)
                    def gate_mul(p):
                        bg, bu = 2 * (p % 2), 2 * (p % 2) + 1
                        sg, r_sg = sgs[p % 2], r_sgs[p % 2]
                        op("act", lambda e: e.activation(out=sg[:], in_=cacc[bg][:], func=AF.Silu),
                           reads=[r_cacc[bg]], writes=[r_sg])
                        op("pool", lambda e: e.tensor_tensor(out=actT[:, p, :], in0=sg[:], in1=cacc[bu][:], op=ALU.mult),
                           reads=[r_sg, r_cacc[bu]], writes=[r_actT[p]])

                    for p in range(NPAIR):
                        kw = wq.pop(0)
                        if p + 2 < NPAIR:
                            wq.append(load_wup(p + 2))
                        for gu in range(2):
                            ch = p + 22 * gu
                            bk, rb = bank()
                            for c in range(8):
                                op("pe", lambda e, c=c, bk=bk, gu=gu: e.matmul(
                                    bk[:, :], lhsT=wupt[kw][:, c, gu * 128:(gu + 1) * 128], rhs=x1T[:, c, :],
                                    start=(c == 0), stop=(c == 7)), reads=[r_wupt[kw], r_x1T[c]], writes=[rb])
                            bi = 2 * (p % 2) + gu
                            hr, rhr = hraw[bi], r_hraw[bi]
                            ca, rca = cacc[bi], r_cacc[bi]
                            op("pool", lambda e, hr=hr, ch=ch: e.tensor_copy(out=hr[:, 0:2], in_=carry[:, ch, :]),
                               reads=[r_carry], writes=[rhr])
                            op("act", lambda e, hr=hr, bk=bk: e.activation(out=hr[:, 2:514], in_=bk[:, :], func=AF.Copy),
                               reads=[rb], writes=[rhr])
                            op("act", lambda e, ca=ca, bk=bk, ch=ch: e.activation(
                                out=ca[:], in_=bk[:, :], func=AF.Identity, scale=cvp[:, ch, 2:3], bias=cvp[:, ch, 3:4]),
                               reads=[rb, r_cw], writes=[rca])
                            op("pool", lambda e, hr=hr, ch=ch: e.tensor_copy(out=carry[:, ch, :], in_=hr[:, 512:514]),
                               reads=[rhr], writes=[r_carry])
                            op("dve", lambda e, ca=ca, hr=hr, ch=ch: e.scalar_tensor_tensor(
                                out=ca[:], in0=hr[:, 1:513], scalar=cvp[:, ch, 1:2], in1=ca[:],
                                op0=ALU.mult, op1=ALU.add), reads=[rhr, rca, r_cw], writes=[rca])
                            op("dve", lambda e, ca=ca, hr=hr, ch=ch: e.scalar_tensor_tensor(
                                out=ca[:], in0=hr[:, 0:512], scalar=cvp[:, ch, 0:1], in1=ca[:],
                                op0=ALU.mult, op1=ALU.add), reads=[rhr, rca, r_cw], writes=[rca])
                        if p >= 1:
                            gate_mul(p - 1)
                    gate_mul(NPAIR - 1)

                def stage_p2(t):
                    for a in range(4):
                        ko = (t * 4 + a) % 2
                        bks = []
                        for n in range(2):
                            bk, rb = bank()
                            for p in range(NPAIR):
                                op("pe", lambda e, p=p, n=n, bk=bk: e.matmul(
                                    bk[:, :], lhsT=actT[:, p, a * 128:(a + 1) * 128], rhs=wdn[:, p, n * 512:(n + 1) * 512],
                                    start=(p == 0), stop=(p == NPAIR - 1)), reads=[r_actT[p], r_cw], writes=[rb])
                            bks.append((bk, rb))
                        ky = lnk["k"] % 2
                        lnk["k"] += 1
                        for n in range(2):
                            bk, rb = bks[n]
                            op("dve", lambda e, n=n, bk=bk: e.scalar_tensor_tensor(
                                out=ybs[ky][:, n * 512:(n + 1) * 512], in0=x1f2[t % 2][:, a, n * 512:(n + 1) * 512], scalar=ALPHA,
                                in1=bk[:, :], op0=ALU.mult, op1=ALU.add), reads=[r_x1f2[t % 2][a], rb], writes=[r_ybs[ky]])
                        layer_norm(ky, 2, ot[ko][:], r_ot[ko])
                        SC.dma(out[s, t * 512 + a * 128:t * 512 + (a + 1) * 128, :], ot[ko][:], d_out,
                               reads=[r_ot[ko]], writes=[r_out], live=True, q="pool")

                stage_wo(0)
                stage_tr(0)
                for t in range(NT):
                    stage_p1(t)
                    if t + 1 < NT:
                        stage_wo(t + 1)
                    stage_p2(t)
                    if t + 1 < NT:
                        stage_tr(t + 1)
                SC.flush()

        SC._wait("sp", ("d", d_out, d_out.cnt))
        SC.flush()
    return nc


def _bf(a):
    return np.ascontiguousarray(a.astype(ml_dtypes.bfloat16))


def make_consts(S):
    NT = S // 512
    c = {}
    c["c_ident"] = np.eye(128, dtype=np.float32)
    c["c_identb"] = _bf(np.eye(128, dtype=np.float32))
    k = np.arange(128)[:, None, None]
    r = np.arange(4)[None, :, None]
    q = np.arange(512)[None, None, :]
    c["c_cmask"] = _bf(np.where(128 * r + k > q, NEGBIG, 0.0).astype(np.float32).reshape(128, 2048))
    half = 16
    inv = (np.float32(10000.0) ** (-np.arange(half, dtype=np.float32) / np.float32(half))).astype(np.float32)
    pos = np.arange(S, dtype=np.float32)
    ang = (pos[:, None] * inv[None, :]).astype(np.float32)
    cos = np.cos(ang).astype(np.float32)
    sin = np.sin(ang).astype(np.float32)
    d = np.arange(128) % 32
    cosT = cos[:, d % 16].T
    sinT = sin[:, d % 16].T * np.where(d < 16, -1.0, 1.0)[:, None]
    rope = np.stack([cosT.reshape(128, NT, 512), sinT.reshape(128, NT, 512)], axis=2)
    c["c_rope"] = np.ascontiguousarray(rope.transpose(1, 0, 2, 3).reshape(NT, 128, 1024).astype(np.float32))
    kc = np.zeros((18, S), np.float32)
    kc[0:2] = 1.0
    blk = np.arange(S) // 256
    for n in range(16):
        kc[2 + n] = np.where(blk == n, NEGBIG, 0.0)
    c["c_kconst"] = _bf(kc)
    slopes = (2.0 ** (-8.0 * np.arange(1, 9, dtype=np.float32) / 8.0)).astype(np.float32)
    dq = (np.arange(S) % 512).astype(np.float32)
    v = (-slopes[:, None] * dq[None, :] / np.float32(SC_MOBA)).astype(np.float32)
    hi = v.astype(ml_dtypes.bfloat16)
    lo = (v - hi.astype(np.float32)).astype(ml_dtypes.bfloat16)
    c["c_qalibi"] = np.ascontiguousarray(np.stack([hi, lo], axis=1))
    i = np.arange(35)
    Dd = (128 * i - 384).astype(np.float32)
    kk = np.arange(128, dtype=np.float32)
    ab = -slopes[None, :, None] * (Dd[None, None, :] - kk[:, None, None])
    c["c_abias"] = np.ascontiguousarray(ab.astype(np.float32).reshape(128, 8 * 35))
    own = np.arange(16)[:, None]
    n = np.arange(16)[None, :]
    pm = np.where(n < own, 0.0, np.where(n == own, 1e30, -1e30)).astype(np.float32)
    c["c_pastm"] = np.ascontiguousarray(np.broadcast_to(pm.reshape(1, 256), (128, 256)))
    return c


def prep_weights(w_in, q_norm_g, w_uq, kv_norm_g, w_ukv, w_o, ln1_g, ln1_b, w_up, conv_w, conv_b, w_down,
                 ln2_g, ln2_b):
    f = lambda a: np.ascontiguousarray(np.asarray(a, dtype=np.float32))
    w_in, w_uq, w_ukv, w_o, w_up, w_down = (f(a[0]) for a in (w_in, w_uq, w_ukv, w_o, w_up, w_down))
    r = np.arange(32)
    kr_sw = w_in[:, 384 + ((r + 16) % 32)]
    win = np.concatenate([w_in[:, 0:416], kr_sw, w_in[:, 416:1952]], axis=1)
    win = win.reshape(8, 128, WIN_COLS).transpose(1, 0, 2).reshape(128, 8 * WIN_COLS)
    h = np.arange(8)
    nope = (h[:, None] * 96 + np.arange(64)[None, :]).reshape(-1)
    pe = (h[:, None] * 96 + 64 + r[None, :]).reshape(-1)
    pesw = (h[:, None] * 96 + 64 + ((r + 16) % 32)[None, :]).reshape(-1)
    wuq = w_uq[:, np.concatenate([nope, pe, pesw])]
    wuq = wuq.reshape(2, 128, 1024).transpose(1, 0, 2).reshape(128, 2048)
    kcols = (h[:, None] * 128 + np.arange(64)[None, :]).reshape(-1)
    vcols = kcols + 64
    wukv = w_ukv[:, np.concatenate([kcols, vcols])]
    wo = w_o.reshape(8, 128, 1024).transpose(1, 0, 2).reshape(128, 8192)
    wdn = w_down.reshape(NPAIR, 128, 1024).transpose(1, 0, 2).reshape(128, NPAIR * 1024)
    wu = w_up.reshape(8, 128, 2, NPAIR, 128)
    wu = wu.transpose(3, 1, 0, 2, 4).reshape(NPAIR, 128, 8 * 256)
    lnp = np.stack([f(ln1_g[0]), f(ln1_b[0]), f(ln2_g[0]), f(ln2_b[0])], axis=0).reshape(1, 4096)
    lnp = np.broadcast_to(lnp, (128, 4096))
    cw = f(conv_w[0])
    cb = f(conv_b[0])
    cv = np.concatenate([cw, cb[None, :]], axis=0)
    cv = cv.reshape(4, 44, 128).transpose(2, 1, 0).reshape(128, 176)
    return {
        "w_in": f(win), "w_uq": f(wuq), "w_ukv": f(wukv), "w_o": f(wo), "w_dn": f(wdn), "w_up": f(wu),
        "qg": f(f(q_norm_g[0]).reshape(2, 128).T), "kvg": f(f(kv_norm_g[0]).reshape(128, 1)),
        "lnp": f(lnp), "convp": f(cv),
    }


_NC_CACHE = {}


def run(x, params, ncores, debug=False, stop=None):
    B, S, _ = x.shape
    nseq = B // ncores
    key = (nseq, S, debug)
    if key not in _NC_CACHE:
        _NC_CACHE[key] = build(nseq, S, debug, stop)
    nc = _NC_CACHE[key]
    shared = dict(prep_weights(**params))
    shared.update(make_consts(S))
    xs = np.ascontiguousarray(np.asarray(x, dtype=np.float32)).reshape(ncores, nseq, S, D)
    in_maps = [dict(shared, x=xs[i]) for i in range(ncores)]
    res = run_bass_kernel_spmd(nc, in_maps, core_ids=list(range(ncores)))
    return res


def kernel(x, w_in, q_norm_g, w_uq, kv_norm_g, w_ukv, w_o, ln1_g, ln1_b, w_up, conv_w, conv_b, w_down,
           ln2_g, ln2_b):
    params = dict(w_in=w_in, q_norm_g=q_norm_g, w_uq=w_uq, kv_norm_g=kv_norm_g, w_ukv=w_ukv, w_o=w_o,
                  ln1_g=ln1_g, ln1_b=ln1_b, w_up=w_up, conv_w=conv_w, conv_b=conv_b, w_down=w_down,
                  ln2_g=ln2_g, ln2_b=ln2_b)
    x = np.asarray(x)
    res = run(x, params, NCORES)
    outs = [np.asarray(r["out"]) for r in res.results]
    return np.concatenate(outs, axis=0).astype(np.float32)
```
